# Optimizing a Trainium2 kernel written in Bass

```python
import jax, jax.numpy as jnp
from jax import lax
import numpy as np

D_MODEL = 4096
BATCH = 2
SEQ = 8192
DEPTH = 4

GRID_W = 64
CTX_LEN = 256
EPS = 1e-6

RET_HEADS = 8
RET_DK = D_MODEL // (4 * RET_HEADS)
RET_DV = 2 * RET_DK
RET_QK = RET_HEADS * RET_DK
RET_V = RET_HEADS * RET_DV
RET_CHUNK = 128
ROPE_BASE = 10000.0

NA_HEADS = 8
NA_HD = D_MODEL // (4 * NA_HEADS)
NA_W = NA_HEADS * NA_HD
NA_ROWS_MAX = 8
NA_COLS = 16

CONV_W = D_MODEL // 4
CONV_K = 31

IN_COLS = (
    ("ret_q", RET_QK), ("ret_k", RET_QK), ("ret_v", RET_V), ("ret_g", RET_V),
    ("na_q", NA_W), ("na_k", NA_W), ("na_v", NA_W), ("na_g", NA_W),
    ("cv_a", CONV_W), ("cv_b", CONV_W), ("cv_g", CONV_W),
    ("gate_ret", D_MODEL), ("gate_na", D_MODEL), ("gate_cv", D_MODEL),
)
N_IN = sum(n for _, n in IN_COLS)
CTX_KV_COLS = ("ret_k", "ret_v", "na_k", "na_v")

kernel_name = "hybrid_retention_natten_conformer_dit"


def rms_norm(x, g):
    xf = x.astype(jnp.float32)
    y = xf * lax.rsqrt(jnp.mean(xf * xf, axis=-1, keepdims=True) + EPS)
    return (y * g.astype(jnp.float32)).astype(x.dtype)


def layer_norm(x, g, b):
    xf = x.astype(jnp.float32)
    mu = jnp.mean(xf, axis=-1, keepdims=True)
    xc = xf - mu
    var = jnp.mean(xc * xc, axis=-1, keepdims=True)
    return (xc * lax.rsqrt(var + EPS) * g.astype(jnp.float32) + b.astype(jnp.float32)).astype(x.dtype)


def _head_rms(o):
    o = o.astype(jnp.float32)
    return o * lax.rsqrt(jnp.mean(o * o, axis=-1, keepdims=True) + EPS)


def _heads(a, n):
    return a.reshape(*a.shape[:-1], n, a.shape[-1] // n)


def _flip(a):
    return jnp.flip(a, axis=1)


def _split_cols(p):
    out, off = {}, 0
    for name, n in IN_COLS:
        out[name] = p[..., off:off + n]
        off += n
    return out


def _project_subset(h, w_in, names):
    out, off = {}, 0
    for name, n in IN_COLS:
        if name in names:
            out[name] = h @ w_in[:, off:off + n]
        off += n
    return out


def axial_rope(n_tokens, head_dim):
    t = jnp.arange(n_tokens)
    row = (t // GRID_W).astype(jnp.float32)
    col = (t % GRID_W).astype(jnp.float32)
    n_freq = head_dim // 4
    inv = ROPE_BASE ** (-jnp.arange(n_freq, dtype=jnp.float32) / n_freq)
    ang = jnp.concatenate([row[:, None] * inv, col[:, None] * inv], axis=-1)
    return jnp.cos(ang), jnp.sin(ang)


def apply_rope(x, cos, sin):
    x1, x2 = jnp.split(x, 2, axis=-1)
    cs = cos[None, :, None, :].astype(x.dtype)
    sn = sin[None, :, None, :].astype(x.dtype)
    return jnp.concatenate([x1 * cs - x2 * sn, x1 * sn + x2 * cs], axis=-1)


def retention_scan(q, k, v, log_g, s0):
    B, T, H, _ = q.shape
    dv = v.shape[-1]
    n_chunks = T // RET_CHUNK

    def chunks(a):
        a = a.astype(jnp.float32).reshape(B, n_chunks, RET_CHUNK, H, a.shape[-1])
        return a.transpose(1, 0, 3, 2, 4)

    idx = jnp.arange(RET_CHUNK, dtype=jnp.float32)
    rel = idx[:, None] - idx[None, :]
    intra = jnp.where(rel >= 0, jnp.exp(log_g[:, None, None] * jnp.maximum(rel, 0.0)), 0.0)
    q_dec = jnp.exp(log_g[:, None] * (idx + 1.0))[None, :, :, None]
    k_dec = jnp.exp(log_g[:, None] * (RET_CHUNK - 1.0 - idx))[None, :, :, None]
    chunk_dec = jnp.exp(log_g * RET_CHUNK)[None, :, None, None]

    def step(s, inp):
        qb, kb, vb = inp
        scores = jnp.einsum('bhid,bhjd->bhij', qb, kb) * intra
        o = jnp.einsum('bhij,bhjv->bhiv', scores, vb) + jnp.einsum('bhid,bhdv->bhiv', qb * q_dec, s)
        s = s * chunk_dec + jnp.einsum('bhjd,bhjv->bhdv', kb * k_dec, vb)
        return s, o

    s, o = lax.scan(step, s0, (chunks(q), chunks(k), chunks(v)))
    return o.transpose(1, 0, 3, 2, 4).reshape(B, T, H, dv), s


def retention_final_state(k, v, log_g):
    T = k.shape[1]
    age = (T - 1 - jnp.arange(T)).astype(jnp.float32)
    w = jnp.exp(age[:, None] * log_g[None, :])
    return jnp.einsum('bthd,th,bthv->bhdv', k.astype(jnp.float32), w, v.astype(jnp.float32))


def neighbourhood_attention(q, k, v, k_ctx, v_ctx, rpb):
    B, S, H, d = q.shape
    rows = S // GRID_W
    kr = min(NA_ROWS_MAX, rows)
    n_loc = kr * NA_COLS
    qg = q.reshape(B, rows, GRID_W, H, d)
    kg = k.reshape(B, rows, GRID_W, H, d)
    vg = v.reshape(B, rows, GRID_W, H, d)
    cols = jnp.arange(GRID_W)
    col_start = jnp.clip(cols - NA_COLS // 2, 0, GRID_W - NA_COLS)
    col_idx = col_start[:, None] + jnp.arange(NA_COLS)[None, :]
    col_bias_idx = col_idx - cols[:, None] + (NA_COLS - 1)

    def row_block(r):
        rs = jnp.clip(r - kr // 2, 0, rows - kr)
        kb = lax.dynamic_slice_in_dim(kg, rs, kr, axis=1)
        vb = lax.dynamic_slice_in_dim(vg, rs, kr, axis=1)
        kw = kb[:, :, col_idx]
        vw = vb[:, :, col_idx]
        qr = lax.dynamic_index_in_dim(qg, r, axis=1, keepdims=False)
        row_bias_idx = rs + jnp.arange(kr) - r + (NA_ROWS_MAX - 1)
        bias = rpb[:, row_bias_idx][:, :, col_bias_idx]
        s_loc = jnp.einsum('bqhd,brqchd->bhqrc', qr, kw).astype(jnp.float32)
        s_loc = s_loc + bias.transpose(0, 2, 1, 3)[None].astype(jnp.float32)
        s_ctx = jnp.einsum('bqhd,bchd->bhqc', qr, k_ctx).astype(jnp.float32)
        s_all = jnp.concatenate([s_loc.reshape(B, H, GRID_W, n_loc), s_ctx], axis=-1)
        p = jax.nn.softmax(s_all, axis=-1).astype(v.dtype)
        p_loc = p[..., :n_loc].reshape(B, H, GRID_W, kr, NA_COLS)
        p_ctx = p[..., n_loc:]
        return (jnp.einsum('bhqrc,brqchd->bqhd', p_loc, vw)
                + jnp.einsum('bhqc,bchd->bqhd', p_ctx, v_ctx))

    out = lax.map(row_block, jnp.arange(rows))
    return out.transpose(1, 0, 2, 3, 4).reshape(B, S, H, d)


def context_attention(q, k, v):
    s = jnp.einsum('bqhd,bkhd->bhqk', q, k).astype(jnp.float32)
    p = jax.nn.softmax(s, axis=-1).astype(v.dtype)
    return jnp.einsum('bhqk,bkhd->bqhd', p, v)


def conformer_conv(a, b, dw, db, ln_g, ln_b):
    u = a * jax.nn.sigmoid(b)
    u = lax.conv_general_dilated(
        u, dw[:, None, :].astype(u.dtype), window_strides=(1,),
        padding=((CONV_K // 2, CONV_K // 2),),
        dimension_numbers=('NWC', 'WIO', 'NWC'),
        feature_group_count=u.shape[-1]) + db
    return jax.nn.silu(layer_norm(u, ln_g, ln_b))


def _gated_out(o, g, w):
    o = o.reshape(*o.shape[:2], -1).astype(g.dtype)
    return (o * jax.nn.silu(g)) @ w


def _merge(p, y_ret, y_na, y_cv):
    return (jax.nn.sigmoid(p["gate_ret"]) * y_ret + jax.nn.sigmoid(p["gate_na"]) * y_na
            + jax.nn.sigmoid(p["gate_cv"]) * y_cv)


def hybrid_layer(x, ctx, c, c_ctx, w_ada, b_ada, norm_g, w_in, ret_decay_f, ret_decay_b, w_ret_o,
                 na_q_gain, na_k_gain, na_rpb, w_na_o, cv_dw, cv_db, cv_ln_g, cv_ln_b, w_cv_o,
                 w_out, update_ctx):
    B, S, _ = x.shape
    shift, scale, gate = jnp.split(jax.nn.silu(c) @ w_ada + b_ada, 3, axis=-1)
    shift_c, scale_c, gate_c = jnp.split(jax.nn.silu(c_ctx) @ w_ada + b_ada, 3, axis=-1)
    hx = rms_norm(x, norm_g) * (1.0 + scale[:, None]) + shift[:, None]
    hc = rms_norm(ctx, norm_g) * (1.0 + scale_c) + shift_c
    px = _split_cols(hx @ w_in)
    pc = _split_cols(hc @ w_in) if update_ctx else _project_subset(hc, w_in, CTX_KV_COLS)

    lg_f = jax.nn.log_sigmoid(ret_decay_f.astype(jnp.float32))
    lg_b = jax.nn.log_sigmoid(ret_decay_b.astype(jnp.float32))
    cos, sin = axial_rope(S, RET_DK)
    k_scale = RET_DK ** -0.5
    rq = apply_rope(_heads(px["ret_q"], RET_HEADS), cos, sin)
    rk = apply_rope(_heads(px["ret_k"], RET_HEADS), cos, sin) * k_scale
    rv = _heads(px["ret_v"], RET_HEADS)
    rk_c = _heads(pc["ret_k"], RET_HEADS) * k_scale
    rv_c = _heads(pc["ret_v"], RET_HEADS)
    if update_ctx:
        rq_c = _heads(pc["ret_q"], RET_HEADS)
        zero = jnp.zeros((B, RET_HEADS, RET_DK, RET_DV), jnp.float32)
        oc_f, s_f = retention_scan(rq_c, rk_c, rv_c, lg_f, zero)
        oc_b, s_b = retention_scan(_flip(rq_c), _flip(rk_c), _flip(rv_c), lg_b, zero)
        yc_ret = _gated_out(_head_rms(oc_f + _flip(oc_b)), pc["ret_g"], w_ret_o)
    else:
        s_f = retention_final_state(rk_c, rv_c, lg_f)
        s_b = retention_final_state(_flip(rk_c), _flip(rv_c), lg_b)
    ol_f, _ = retention_scan(rq, rk, rv, lg_f, s_f)
    ol_b, _ = retention_scan(_flip(rq), _flip(rk), _flip(rv), lg_b, s_b)
    y_ret = _gated_out(_head_rms(ol_f + _flip(ol_b)), px["ret_g"], w_ret_o)

    nq = rms_norm(_heads(px["na_q"], NA_HEADS), na_q_gain) * NA_HD ** -0.5
    nk = rms_norm(_heads(px["na_k"], NA_HEADS), na_k_gain)
    nv = _heads(px["na_v"], NA_HEADS)
    nk_c = rms_norm(_heads(pc["na_k"], NA_HEADS), na_k_gain)
    nv_c = _heads(pc["na_v"], NA_HEADS)
    y_na = _gated_out(neighbourhood_attention(nq, nk, nv, nk_c, nv_c, na_rpb), px["na_g"], w_na_o)

    y_cv = _gated_out(conformer_conv(px["cv_a"], px["cv_b"], cv_dw, cv_db, cv_ln_g, cv_ln_b),
                      px["cv_g"], w_cv_o)

    x = x + gate[:, None] * (_merge(px, y_ret, y_na, y_cv) @ w_out)

    if update_ctx:
        nq_c = rms_norm(_heads(pc["na_q"], NA_HEADS), na_q_gain) * NA_HD ** -0.5
        yc_na = _gated_out(context_attention(nq_c, nk_c, nv_c), pc["na_g"], w_na_o)
        yc_cv = _gated_out(conformer_conv(pc["cv_a"], pc["cv_b"], cv_dw, cv_db, cv_ln_g, cv_ln_b),
                           pc["cv_g"], w_cv_o)
        ctx = ctx + gate_c * (_merge(pc, yc_ret, yc_na, yc_cv) @ w_out)
    return x, ctx


def setup_inputs(seed: int = 0) -> dict:
    key = jax.random.key(seed)
    ks = iter(jax.random.split(key, 32))
    f32 = jnp.float32
    L, D = DEPTH, D_MODEL

    def nrm(shape, s):
        return jax.random.normal(next(ks), shape, f32) * s

    ret_base = jnp.log(2.0 ** (5.0 + jnp.arange(RET_HEADS, dtype=f32)) - 1.0)
    return {
        "x": nrm((BATCH, SEQ, D), 1.0),
        "c": nrm((BATCH, D), 1.0),
        "ctx": nrm((BATCH, CTX_LEN, D), 1.0),
        "c_ctx": nrm((D,), 1.0),
        "w_ada": nrm((L, D, 3 * D), D ** -0.5),
        "b_ada": nrm((L, 3 * D), 0.01),
        "norm_g": 1.0 + nrm((L, D), 0.02),
        "w_in": nrm((L, D, N_IN), D ** -0.5),
        "ret_decay_f": ret_base[None, :] + nrm((L, RET_HEADS), 0.1),
        "ret_decay_b": ret_base[None, :] + nrm((L, RET_HEADS), 0.1),
        "w_ret_o": nrm((L, RET_V, D), RET_V ** -0.5),
        "na_q_gain": 1.0 + nrm((L, NA_HD), 0.02),
        "na_k_gain": 1.0 + nrm((L, NA_HD), 0.02),
        "na_rpb": nrm((L, NA_HEADS, 2 * NA_ROWS_MAX - 1, 2 * NA_COLS - 1), 0.02),
        "w_na_o": nrm((L, NA_W, D), NA_W ** -0.5),
        "cv_dw": nrm((L, CONV_K, CONV_W), CONV_K ** -0.5),
        "cv_db": nrm((L, CONV_W), 0.01),
        "cv_ln_g": 1.0 + nrm((L, CONV_W), 0.02),
        "cv_ln_b": nrm((L, CONV_W), 0.01),
        "w_cv_o": nrm((L, CONV_W, D), CONV_W ** -0.5),
        "w_out": nrm((L, D, D), D ** -0.5),
    }


def reference(x, c, ctx, c_ctx, w_ada, b_ada, norm_g, w_in, ret_decay_f, ret_decay_b, w_ret_o,
              na_q_gain, na_k_gain, na_rpb, w_na_o, cv_dw, cv_db, cv_ln_g, cv_ln_b, w_cv_o, w_out):
    h, hc = x, ctx
    for l in range(DEPTH):
        h, hc = hybrid_layer(
            h, hc, c, c_ctx, w_ada[l], b_ada[l], norm_g[l], w_in[l], ret_decay_f[l], ret_decay_b[l],
            w_ret_o[l], na_q_gain[l], na_k_gain[l], na_rpb[l], w_na_o[l], cv_dw[l], cv_db[l],
            cv_ln_g[l], cv_ln_b[l], w_cv_o[l], w_out[l], update_ctx=(l < DEPTH - 1))
    return h
```

```python
import contextlib
import numpy as np
import ml_dtypes
import concourse.bass as bass
import concourse.mybir as mybir
from concourse.bass_utils import run_bass_kernel_spmd

F32 = mybir.dt.float32
BF16 = mybir.dt.bfloat16
AF = mybir.ActivationFunctionType
ALU = mybir.AluOpType
NPBF = ml_dtypes.bfloat16

GRID_W = 64
EPS = 1e-6
CONV_K = 31
BIGD = 1.0e7
NEG = -30000.0


class Cfg:
    def __init__(self, D=4096, H=8, SEQ=8192, CTX=256, DEPTH=4, B=2, NQ=4):
        self.D, self.H, self.SEQ, self.CTX, self.DEPTH, self.B = D, H, SEQ, CTX, DEPTH, B
        self.NQ = NQ
        self.TL = SEQ // self.NQ
        self.T = self.TL + CTX
        self.KC = D // 128
        self.RQK = H * 128
        self.RV = H * 256
        self.NAW = H * 128
        self.CW = H * 128
        self.NP = self.TL // 128
        self.ROWS = SEQ // GRID_W
        self.LR = self.TL // GRID_W
        o = 0
        self.off = {}
        for name, n in (("ret_q", self.RQK), ("ret_k", self.RQK), ("ret_v", self.RV), ("ret_g", self.RV),
                        ("na_q", self.NAW), ("na_k", self.NAW), ("na_v", self.NAW), ("na_g", self.NAW),
                        ("cv_a", self.CW), ("cv_b", self.CW), ("cv_g", self.CW),
                        ("gate_ret", D), ("gate_na", D), ("gate_cv", D)):
            self.off[name] = (o, n)
            o += n
        self.N_IN = o
        self.KBR = self.RV + self.NAW + self.CW


class Buf:
    __slots__ = ("name", "w", "r")

    def __init__(self, name=""):
        self.name = name
        self.w = None
        self.r = []


class Prog:
    ENG = ("pe", "act", "dve", "pool", "sp")
    NDMA = 24

    def __init__(self, nc):
        self.nc = nc
        self.ops = {e: [] for e in self.ENG}
        self.cnt = {}
        self.known = {e: {} for e in self.ENG}
        self.dma_rr = {e: 0 for e in self.ENG}
        self.n_ops = 0

    def _need(self, eng, ticket, waits):
        if ticket is None:
            return
        k, v = ticket
        if k == ("e", eng) and eng == "pe":
            return
        if self.known[eng].get(k, 0) >= v:
            return
        if waits.get(k, 0) < v:
            waits[k] = v

    def _deps(self, eng, reads, writes):
        waits = {}
        for b in reads:
            self._need(eng, b.w, waits)
        for b in writes:
            self._need(eng, b.w, waits)
            for t in b.r:
                self._need(eng, t, waits)
        return waits

    def _commit(self, eng, waits):
        for k, v in waits.items():
            self.known[eng][k] = v
        return list(waits.items())

    def _mark(self, ticket, reads, writes):
        for b in reads:
            if b not in writes:
                b.r.append(ticket)
                if len(b.r) > 48:
                    b.r = b.r[-48:]
        for b in writes:
            b.w = ticket
            b.r = []

    def op(self, eng, fn, reads=(), writes=()):
        reads = list(reads); writes = list(writes)
        waits = self._commit(eng, self._deps(eng, reads, writes))
        k = ("e", eng)
        v = self.cnt.get(k, 0) + 1
        self.cnt[k] = v
        self.ops[eng].append((waits, fn, k, 1))
        self._mark((k, v), reads, writes)
        self.n_ops += 1

    def dma(self, q, out, in_, reads=(), writes=(), **kw):
        reads = list(reads); writes = list(writes)
        waits = self._deps(q, reads, writes)
        i = self.dma_rr[q]
        self.dma_rr[q] = (i + 1) % self.NDMA
        k = ("d", q, i)
        prev = self.cnt.get(k, 0)
        if prev > 0 and self.known[q].get(k, 0) < prev:
            waits[k] = max(waits.get(k, 0), prev)
        waits = self._commit(q, waits)
        self.cnt[k] = prev + 16

        def fn(e, out=out, in_=in_, kw=kw):
            return e.dma_start(out=out, in_=in_, **kw)
        self.ops[q].append((waits, fn, k, 16))
        self._mark((k, prev + 16), reads, writes)
        self.n_ops += 1

    def barrier(self):
        for eng in self.ENG:
            waits = {}
            for k, v in self.cnt.items():
                if k == ("e", eng):
                    continue
                if self.known[eng].get(k, 0) < v:
                    waits[k] = v
            waits = self._commit(eng, waits)
            if waits:
                self.ops[eng].append((waits, None, None, 0))

    def emit(self):
        nc = self.nc
        self.barrier()
        keys = sorted(self.cnt.keys(), key=str)
        with contextlib.ExitStack() as st:
            sems = {}
            for k in keys:
                sems[k] = st.enter_context(nc.semaphore("s_" + "_".join(str(x) for x in k)))
            block = st.enter_context(nc.Block())

            def run(name, e):
                for waits, fn, k, inc in self.ops[name]:
                    for wk, wv in waits:
                        e.wait_ge(sems[wk], wv)
                    if fn is not None:
                        fn(e).then_inc(sems[k], inc)

            @block.tensor
            def _(e):
                run("pe", e)

            @block.scalar
            def _(e):
                run("act", e)

            @block.vector
            def _(e):
                run("dve", e)

            @block.gpsimd
            def _(e):
                run("pool", e)

            @block.sync
            def _(e):
                run("sp", e)


DBG_NAMES = ("hxT", "ogT", "mT", "qT", "kT", "nqT", "nkT")
DBGOUT = {}


class Ctx:
    def __init__(self, nc, cfg):
        self.nc = nc
        self.cfg = cfg
        self.P = Prog(nc)
        self.uid = 0
        self.bufs = {}

    def name(self, s):
        self.uid += 1
        return f"{s}_{self.uid}"

    def sb(self, st, name, shape, dt):
        t = st.enter_context(self.nc.sbuf_tensor(self.name(name), list(shape), dt))
        return t, Buf(name)

    def dram(self, name, shape, dt, kind="Internal"):
        if getattr(self.cfg, "debug", False) and name in DBG_NAMES:
            kind = "ExternalOutput"
        t = self.nc.dram_tensor(name, list(shape), dt, kind=kind).ap()
        b = Buf(name)
        self.bufs[name] = b
        return t, b


def bc(ap, shape, axis):
    return ap.unsqueeze(axis).broadcast_to(list(shape))


class Layer:
    pass


def setup_common(C, st, ins):
    cfg, P, nc = C.cfg, C.P, C.nc
    L = Layer()
    KC, H = cfg.KC, cfg.H
    L.ps = []
    for i in range(7):
        t = st.enter_context(nc.psum_tensor(C.name("ps"), [128, 512], F32))
        L.ps.append((t, Buf(f"ps{i}")))
    t = st.enter_context(nc.psum_tensor(C.name("psb"), [128, 1024], BF16))
    L.psb = (t, Buf("psb"))
    L.ones_bf, b1 = C.sb(st, "ones_bf", [128, 128], BF16)
    L.ones_f, b2 = C.sb(st, "ones_f", [128, 128], F32)
    L.ident, b3 = C.sb(st, "ident", [128, 128], BF16)
    L.Bconst = Buf("const")
    P.op("pool", lambda e: e.memset(L.ones_bf[:], 1.0), writes=[L.Bconst])
    P.op("pool", lambda e: e.memset(L.ones_f[:], 1.0), writes=[L.Bconst])
    P.dma("sp", L.ident[:], ins["ident"][:, :], writes=[L.Bconst])
    L.mods, _ = C.sb(st, "mods", [128, KC, 6], F32)
    L.ng, _ = C.sb(st, "ng", [128, KC], F32)
    L.G, _ = C.sb(st, "G", [128, 2, KC], F32)
    L.SH, _ = C.sb(st, "SH", [128, 2, KC], F32)
    L.GT, _ = C.sb(st, "GT", [128, 2, KC], F32)
    L.Bpar = Buf("par")
    P.dma("sp", L.mods[:], ins["mods"][:, :, :], writes=[L.Bpar])
    P.dma("sp", L.ng[:], ins["norm_g"][:, :], writes=[L.Bpar])
    for s in range(2):
        P.op("dve", lambda e, s=s: e.scalar_tensor_tensor(out=L.G[:, s, :], in0=L.mods[:, :, 3 * s + 1], scalar=1.0,
                                                          in1=L.ng[:], op0=ALU.add, op1=ALU.mult),
             reads=[L.Bpar], writes=[L.Bpar])
        P.op("dve", lambda e, s=s: e.tensor_copy(out=L.SH[:, s, :], in_=L.mods[:, :, 3 * s]), reads=[L.Bpar], writes=[L.Bpar])
        P.op("dve", lambda e, s=s: e.tensor_copy(out=L.GT[:, s, :], in_=L.mods[:, :, 3 * s + 2]), reads=[L.Bpar], writes=[L.Bpar])
    L.lg, _ = C.sb(st, "lg", [128, 2 * H], F32)
    tmp, _ = C.sb(st, "lgt", [128, 2 * H], F32)
    tmp2, _ = C.sb(st, "lgt2", [128, 2 * H], F32)
    L.Blg = Buf("lg")
    P.dma("sp", L.lg[:], ins["decay"][:, :], writes=[L.Blg])
    P.op("act", lambda e: e.activation(out=tmp[:], in_=L.lg[:], func=AF.Exp, scale=-1.0), reads=[L.Blg], writes=[L.Blg])
    P.op("dve", lambda e: e.tensor_scalar(out=tmp2[:], in0=tmp[:], scalar1=0.2, scalar2=-0.25, op0=ALU.mult, op1=ALU.add),
         reads=[L.Blg], writes=[L.Blg])
    for cst in (1.0 / 3.0, -0.5, 1.0):
        P.op("dve", lambda e: e.tensor_tensor(out=tmp2[:], in0=tmp2[:], in1=tmp[:], op=ALU.mult), reads=[L.Blg], writes=[L.Blg])
        P.op("dve", lambda e, cst=cst: e.tensor_scalar(out=tmp2[:], in0=tmp2[:], scalar1=cst, scalar2=None, op0=ALU.add),
             reads=[L.Blg], writes=[L.Blg])
    P.op("dve", lambda e: e.tensor_tensor(out=tmp2[:], in0=tmp2[:], in1=tmp[:], op=ALU.mult), reads=[L.Blg], writes=[L.Blg])
    P.op("dve", lambda e: e.tensor_scalar(out=L.lg[:], in0=tmp2[:], scalar1=-1.0, scalar2=None, op0=ALU.mult),
         reads=[L.Blg], writes=[L.Blg])
    return L


def stage_norm(C, L, xT, Bx, cT, Bc, hxT, Bhx, do_ctx=True):
    cfg, P, nc = C.cfg, C.P, C.nc
    KC, D = cfg.KC, cfg.D
    BLK = 256
    with contextlib.ExitStack() as st:
        xs = [C.sb(st, "nx", [128, KC, BLK], F32) for _ in range(2)]
        sq = [C.sb(st, "nsq", [128, KC, BLK], BF16) for _ in range(2)]
        ho = [C.sb(st, "nho", [128, KC, BLK], BF16) for _ in range(2)]
        rs = [C.sb(st, "nrs", [128, BLK], F32) for _ in range(2)]
        blocks = [(0, t0, xT, Bx, t0) for t0 in range(0, cfg.TL, BLK)]
        if do_ctx:
            blocks += [(1, t0, cT, Bc, cfg.TL + t0) for t0 in range(0, cfg.CTX, BLK)]
        for bi, (s, t0, src, Bsrc, o0) in enumerate(blocks):
            (x, bx), (q, bq), (h, bh), (r, br) = xs[bi % 2], sq[bi % 2], ho[bi % 2], rs[bi % 2]
            ps, bps = L.ps[bi % 2]
            P.dma("sp", x[:], src.rearrange("(c p) t -> p c t", p=128)[:, :, t0:t0 + BLK], reads=[Bsrc], writes=[bx])
            P.op("act", lambda e, x=x, q=q: e.activation(out=q[:], in_=x[:], func=AF.Square), reads=[bx], writes=[bq])

            def mm(e, q=q, ps=ps):
                for kc in range(KC):
                    ins_ = e.matmul(ps[:, 0:BLK], lhsT=L.ones_bf[:], rhs=q[:, kc, :], start=(kc == 0), stop=(kc == KC - 1))
                return ins_
            P.op("pe", mm, reads=[bq, L.Bconst], writes=[bps])
            P.op("act", lambda e, r=r, ps=ps: e.activation(out=r[:], in_=ps[:, 0:BLK], func=AF.Sqrt, bias=EPS, scale=1.0 / D),
                 reads=[bps], writes=[br])
            P.op("dve", lambda e, r=r: e.reciprocal(out=r[:], in_=r[:]), reads=[br], writes=[br])
            P.op("dve", lambda e, x=x, r=r: e.tensor_tensor(out=x[:], in0=x[:], in1=bc(r[:], [128, KC, BLK], 1), op=ALU.mult),
                 reads=[bx, br], writes=[bx])
            def modul(e, x=x, h=h, s=s):
                for kc in range(KC):
                    ins_ = e.activation(out=h[:, kc, :], in_=x[:, kc, :], func=AF.Identity, scale=L.G[:, s, kc:kc + 1], bias=L.SH[:, s, kc:kc + 1])
                return ins_
            P.op("act", modul, reads=[bx, L.Bpar], writes=[bh])
            P.dma("sp", hxT.rearrange("(c p) t -> p c t", p=128)[:, :, o0:o0 + BLK], h[:], reads=[bh], writes=[Bhx])
        P.barrier()


def stage_gemm(C, L, aT, Ba, Ka_segs, w_ap, tiles, groups, epi_setup=None):
    cfg, P, nc = C.cfg, C.P, C.nc
    Ktot = sum(n for _, n in Ka_segs)
    KCt = Ktot // 128
    nseg = len(Ka_segs)
    GW = 256
    with contextlib.ExitStack() as st:
        maxT = max(sum(t1 - t0 for t0, t1 in tl) for tl in tiles)
        a_sb, Basb = C.sb(st, "ga", [128, KCt, maxT], BF16)
        NWB = 3
        wbs = [C.sb(st, "gw", [128, KCt, GW], BF16) for _ in range(NWB)]
        env = epi_setup(st) if epi_setup else None
        av = aT.rearrange("(c p) t -> p c t", p=128)
        wv = w_ap.rearrange("(c p) n -> p c n", p=128)
        wi = 0
        pi = 0
        npsum = 6 if nseg == 1 else 6
        for tl in tiles:
            o = 0
            suboff = []
            for (t0, t1) in tl:
                n = t1 - t0
                half = KCt // 2 if KCt >= 2 else KCt
                P.dma("sp", a_sb[:, 0:half, o:o + n], av[:, 0:half, t0:t1], reads=[Ba], writes=[Basb])
                if half < KCt:
                    P.dma("sp", a_sb[:, half:KCt, o:o + n], av[:, half:KCt, t0:t1], reads=[Ba], writes=[Basb])
                suboff.append(o)
                o += n
            for g in groups:
                wb, bwb = wbs[wi % NWB]
                wi += 1
                co = 0
                colmap = []
                for (c0, n) in g["cols"]:
                    P.dma("pool", wb[:, :, co:co + n], wv[:, :, c0:c0 + n], writes=[bwb])
                    colmap.append((c0, n, co))
                    co += n
                if g["mode"] == "fm":
                    for (c0, n, cof) in colmap:
                        for cj in range(n // 128):
                            col0 = c0 + cj * 128
                            wo = cof + cj * 128
                            for si, (t0, t1) in enumerate(tl):
                                nt = t1 - t0
                                pss = []
                                for sg in range(nseg):
                                    pss.append(L.ps[pi % npsum]); pi += 1
                                so = suboff[si]

                                def mm(e, pss=pss, wb=wb, wo=wo, so=so, nt=nt):
                                    ins_ = None
                                    for sg, (k0, nk) in enumerate(Ka_segs):
                                        kc0 = k0 // 128
                                        nkc = nk // 128
                                        for kk in range(nkc):
                                            ins_ = e.matmul(pss[sg][0][:, 0:nt], lhsT=wb[:, kc0 + kk, wo:wo + 128],
                                                            rhs=a_sb[:, kc0 + kk, so:so + nt],
                                                            start=(kk == 0), stop=(kk == nkc - 1))
                                    return ins_
                                P.op("pe", mm, reads=[bwb, Basb], writes=[b for _, b in pss])
                                g["epi"](env, col0, (t0, t1), pss)
                else:
                    ncols = co
                    for si, (t0, t1) in enumerate(tl):
                        for tk in range(t0, t1, 128):
                            ps, bps = L.ps[pi % npsum]; pi += 1
                            so = suboff[si] + (tk - t0)

                            def mm(e, ps=ps, wb=wb, so=so, ncols=ncols):
                                for kc in range(KCt):
                                    ins_ = e.matmul(ps[:, 0:ncols], lhsT=a_sb[:, kc, so:so + 128], rhs=wb[:, kc, 0:ncols],
                                                    start=(kc == 0), stop=(kc == KCt - 1))
                                return ins_
                            P.op("pe", mm, reads=[bwb, Basb], writes=[bps])
                            g["epi"](env, colmap, tk, ps, bps)
        P.barrier()


class Rot:
    def __init__(self, items):
        self.items = items
        self.i = 0

    def next(self):
        x = self.items[self.i % len(self.items)]
        self.i += 1
        return x


def inproj_groups(C, L, S, ins, which):
    cfg, P, nc = C.cfg, C.P, C.nc
    off = cfg.off
    TL = cfg.TL

    def setup(st):
        env = {}
        env["o_bf"] = Rot([C.sb(st, "eob", [128, 512], BF16) for _ in range(4)])
        env["o_f"] = Rot([C.sb(st, "eof", [128, 512], F32) for _ in range(3)])
        env["t_f"] = Rot([C.sb(st, "etf", [128, 512], F32) for _ in range(3)])
        env["t_f2"] = Rot([C.sb(st, "etg", [128, 512], F32) for _ in range(2)])
        env["sq"] = Rot([C.sb(st, "esq", [128, 512], BF16) for _ in range(2)])
        env["ahold"] = {}
        if "ret_q" in which or "ret_k" in which:
            env["rope"], env["Brope"] = C.sb(st, "rope", [128, 2, TL], F32)
            P.dma("sp", env["rope"][:], ins["rope"].rearrange("f p t -> p f t"), writes=[env["Brope"]])
        if "na_q" in which or "na_k" in which:
            env["gains"], env["Bgains"] = C.sb(st, "gains", [128, 2], F32)
            P.dma("sp", env["gains"][:], ins["na_gain"][:, :], writes=[env["Bgains"]])
            P.op("dve", lambda e: e.tensor_scalar(out=env["gains"][:, 0:1], in0=env["gains"][:, 0:1], scalar1=128.0 ** -0.5,
                                                  scalar2=None, op0=ALU.mult), reads=[env["Bgains"]], writes=[env["Bgains"]])
        return env

    def store(env, dst, Bdst, row0, sub, tile, btile, nt):
        P.dma("sp", dst[row0:row0 + 128, sub[0]:sub[1]], tile[:, 0:nt], reads=[btile], writes=[Bdst])

    def epi_act(func, dstname, base):
        dst, Bdst = S[dstname]

        def f(env, col0, sub, pss):
            ps, bps = pss[0]
            nt = sub[1] - sub[0]
            o, bo = env["o_bf"].next()
            P.op("act", lambda e: e.activation(out=o[:, 0:nt], in_=ps[:, 0:nt], func=func), reads=[bps], writes=[bo])
            store(env, dst, Bdst, col0 - base, sub, o, bo, nt)
        return f

    def epi_rope(dstname, base, tab):
        dst, Bdst = S[dstname]

        def f(env, col0, sub, pss):
            ps, bps = pss[0]
            nt = sub[1] - sub[0]
            o, bo = env["o_bf"].next()
            sc = 1.0 if tab == 0 else 128.0 ** -0.5
            if sub[0] >= TL:
                P.op("act", lambda e: e.activation(out=o[:, 0:nt], in_=ps[:, 0:nt], func=AF.Copy, scale=sc), reads=[bps], writes=[bo])
            else:
                x, bx = env["t_f"].next()
                t1, bt1 = env["o_f"].next()
                t2, bt2 = env["t_f2"].next()
                rp = env["rope"]
                P.op("act", lambda e: e.activation(out=x[:, 0:nt], in_=ps[:, 0:nt], func=AF.Copy, scale=sc), reads=[bps], writes=[bx])
                P.op("dve", lambda e: e.tensor_tensor(out=t1[:, 0:nt], in0=x[:, 0:nt], in1=rp[:, 0, sub[0]:sub[1]], op=ALU.mult),
                     reads=[bx, env["Brope"]], writes=[bt1])
                P.op("dve", lambda e: e.tensor_tensor(out=t2[0:64, 0:nt], in0=x[64:128, 0:nt], in1=rp[64:128, 1, sub[0]:sub[1]], op=ALU.mult),
                     reads=[bx, env["Brope"]], writes=[bt2])
                P.op("dve", lambda e: e.tensor_tensor(out=t2[64:128, 0:nt], in0=x[0:64, 0:nt], in1=rp[0:64, 1, sub[0]:sub[1]], op=ALU.mult),
                     reads=[bx, env["Brope"]], writes=[bt2])
                P.op("dve", lambda e: e.tensor_tensor(out=o[:, 0:nt], in0=t1[:, 0:nt], in1=t2[:, 0:nt], op=ALU.add),
                     reads=[bt1, bt2], writes=[bo])
            store(env, dst, Bdst, col0 - base, sub, o, bo, nt)
        return f

    def epi_norm(dstname, base, gi):
        dst, Bdst = S[dstname]

        def f(env, col0, sub, pss):
            ps, bps = pss[0]
            nt = sub[1] - sub[0]
            o, bo = env["o_bf"].next()
            q, bq = env["sq"].next()
            r, br = env["t_f"].next()
            ps2, bps2 = L.ps[6]
            P.op("act", lambda e: e.activation(out=q[:, 0:nt], in_=ps[:, 0:nt], func=AF.Square), reads=[bps], writes=[bq])
            P.op("pe", lambda e: e.matmul(ps2[:, 0:nt], lhsT=L.ones_bf[:], rhs=q[:, 0:nt], start=True, stop=True),
                 reads=[bq, L.Bconst], writes=[bps2])
            P.op("act", lambda e: e.activation(out=r[:, 0:nt], in_=ps2[:, 0:nt], func=AF.Sqrt, bias=EPS, scale=1.0 / 128), reads=[bps2], writes=[br])
            P.op("dve", lambda e: e.reciprocal(out=r[:, 0:nt], in_=r[:, 0:nt]), reads=[br], writes=[br])
            P.op("dve", lambda e: e.scalar_tensor_tensor(out=o[:, 0:nt], in0=ps[:, 0:nt], scalar=env["gains"][:, gi:gi + 1], in1=r[:, 0:nt],
                                                         op0=ALU.mult, op1=ALU.mult), reads=[bps, br, env["Bgains"]], writes=[bo])
            store(env, dst, Bdst, col0 - base, sub, o, bo, nt)
        return f

    def epi_conv_ab():
        dst, Bdst = S["uT"]
        a0 = off["cv_a"][0]
        b0 = off["cv_b"][0]

        def f(env, col0, sub, pss):
            ps, bps = pss[0]
            nt = sub[1] - sub[0]
            if col0 < b0:
                key = (col0 - a0, sub)
                x, bx = env["o_f"].next()
                env["ahold"][key] = (x, bx)
                P.op("act", lambda e: e.activation(out=x[:, 0:nt], in_=ps[:, 0:nt], func=AF.Copy), reads=[bps], writes=[bx])
            else:
                x, bx = env["ahold"].pop((col0 - b0, sub))
                sg, bsg = env["t_f"].next()
                P.op("act", lambda e: e.activation(out=sg[:, 0:nt], in_=ps[:, 0:nt], func=AF.Sigmoid), reads=[bps], writes=[bsg])
                P.op("dve", lambda e: e.tensor_tensor(out=sg[:, 0:nt], in0=sg[:, 0:nt], in1=x[:, 0:nt], op=ALU.mult),
                     reads=[bsg, bx], writes=[bsg])
                P.dma("sp", dst[col0 - b0:col0 - b0 + 128, sub[0]:sub[1]], sg[:, 0:nt], reads=[bsg], writes=[Bdst])
        return f

    def epi_tm(dstname, base):
        dst, Bdst = S[dstname]

        def f(env, colmap, tk, ps, bps):
            for (c0, n, cof) in colmap:
                o, bo = env["o_bf"].next()
                P.op("act", lambda e, o=o, cof=cof, n=n: e.activation(out=o[:, 0:n], in_=ps[:, cof:cof + n], func=AF.Copy), reads=[bps], writes=[bo])
                P.dma("sp", dst[tk:tk + 128, c0 - base:c0 - base + n], o[:, 0:n], reads=[bo], writes=[Bdst])
        return f

    groups = []

    def add_fm(name, epi):
        c0, n = off[name]
        for c in range(c0, c0 + n, 256):
            groups.append(dict(cols=[(c, min(256, c0 + n - c))], mode="fm", epi=epi))

    def add_tm(name, epi):
        c0, n = off[name]
        for c in range(c0, c0 + n, 256):
            groups.append(dict(cols=[(c, min(256, c0 + n - c))], mode="tm", epi=epi))

    if "ret_q" in which:
        add_fm("ret_q", epi_rope("qT", off["ret_q"][0], 0))
    if "ret_k" in which:
        add_fm("ret_k", epi_rope("kT", off["ret_k"][0], 2))
    if "ret_v" in which:
        add_tm("ret_v", epi_tm("vtm", off["ret_v"][0]))
    if "na_q" in which:
        add_fm("na_q", epi_norm("nqT", off["na_q"][0], 0))
    if "na_k" in which:
        add_fm("na_k", epi_norm("nkT", off["na_k"][0], 1))
    if "na_v" in which:
        add_tm("na_v", epi_tm("nvtm", off["na_v"][0]))
    if "cv_ab" in which:
        e = epi_conv_ab()
        for i in range(cfg.CW // 128):
            groups.append(dict(cols=[(off["cv_a"][0] + i * 128, 128), (off["cv_b"][0] + i * 128, 128)], mode="fm", epi=e))
    if "silu" in which:
        add_fm("ret_g", epi_act(AF.Silu, "rgT", off["ret_g"][0]))
        add_fm("na_g", epi_act(AF.Silu, "ngT", off["na_g"][0]))
        add_fm("cv_g", epi_act(AF.Silu, "cgT", off["cv_g"][0]))
    if "gates" in which:
        g0 = off["gate_ret"][0]
        for nm in ("gate_ret", "gate_na", "gate_cv"):
            add_fm(nm, epi_act(AF.Sigmoid, "sgT", g0))
    return groups, setup


def ret_tables(C, L, st, ins):
    cfg, P = C.cfg, C.P
    H = cfg.H
    NCL = cfg.TL // 128
    R = {}
    R["B"] = Buf("rtab")
    cst, bcst = C.sb(st, "rcst", [128, 6, 128], F32)
    P.dma("sp", cst[:], ins["rconst"].rearrange("f p i -> p f i"), writes=[bcst])
    cv, bcv = C.sb(st, "rcv", [128, 2 + 2 * NCL + 10], F32)
    P.dma("sp", cv[:], ins["rvec"][:, :], writes=[bcv])
    R["mask"], _ = C.sb(st, "rmask", [128, H, 128], F32)
    R["qdf"], _ = C.sb(st, "rqdf", [128, H, 128], BF16)
    R["qdb"], _ = C.sb(st, "rqdb", [128, H, 128], BF16)
    R["vec"], _ = C.sb(st, "rvecs", [128, H, 2, 2 + 2 * NCL + 10], F32)
    R["gC"], _ = C.sb(st, "rgC", [128, H, 2], F32)
    tmpa, bta = C.sb(st, "rtmpa", [128, 128], F32)
    tmpb, btb = C.sb(st, "rtmpb", [128, 128], F32)
    c128, bc128 = C.sb(st, "rc128", [128, 1], F32)
    P.op("pool", lambda e: e.memset(c128[:], 128.0), writes=[bc128])
    for h in range(H):
        lf = L.lg[:, h:h + 1]
        lb = L.lg[:, H + h:H + h + 1]
        P.op("act", lambda e, lf=lf: e.activation(out=tmpa[:], in_=cst[:, 0, :], func=AF.Exp, scale=lf), reads=[bcst, L.Blg], writes=[bta])
        P.op("dve", lambda e: e.tensor_tensor(out=tmpa[:], in0=tmpa[:], in1=cst[:, 2, :], op=ALU.mult), reads=[bta, bcst], writes=[bta])
        P.op("act", lambda e, lb=lb: e.activation(out=tmpb[:], in_=cst[:, 1, :], func=AF.Exp, scale=lb), reads=[bcst, L.Blg], writes=[btb])
        P.op("dve", lambda e: e.tensor_tensor(out=tmpb[:], in0=tmpb[:], in1=cst[:, 3, :], op=ALU.mult), reads=[btb, bcst], writes=[btb])
        P.op("dve", lambda e, h=h: e.tensor_tensor(out=R["mask"][:, h, :], in0=tmpa[:], in1=tmpb[:], op=ALU.add), reads=[bta, btb], writes=[R["B"]])
        P.op("act", lambda e, h=h, lf=lf: e.activation(out=R["qdf"][:, h, :], in_=cst[:, 4, :], func=AF.Exp, scale=lf), reads=[bcst, L.Blg], writes=[R["B"]])
        P.op("act", lambda e, h=h, lb=lb: e.activation(out=R["qdb"][:, h, :], in_=cst[:, 5, :], func=AF.Exp, scale=lb), reads=[bcst, L.Blg], writes=[R["B"]])
        P.op("act", lambda e, h=h, lf=lf: e.activation(out=R["vec"][:, h, 0, :], in_=cv[:], func=AF.Exp, scale=lf), reads=[bcv, L.Blg], writes=[R["B"]])
        P.op("act", lambda e, h=h, lb=lb: e.activation(out=R["vec"][:, h, 1, :], in_=cv[:], func=AF.Exp, scale=lb), reads=[bcv, L.Blg], writes=[R["B"]])
        P.op("act", lambda e, h=h, lf=lf: e.activation(out=R["gC"][:, h, 0:1], in_=c128[:], func=AF.Exp, scale=lf), reads=[bc128, L.Blg], writes=[R["B"]])
        P.op("act", lambda e, h=h, lb=lb: e.activation(out=R["gC"][:, h, 1:2], in_=c128[:], func=AF.Exp, scale=lb), reads=[bc128, L.Blg], writes=[R["B"]])
    R["NCL"] = NCL
    return R


def k_tokmajor(C, L, kT_sb, bk, c0, nch, outs):
    P = C.P
    psb, bpsb = L.psb
    for g0 in range(0, nch, 4):
        n = min(4, nch - g0)

        def tr(e, g0=g0, n=n):
            for i in range(n):
                c = c0 + g0 + i
                ins_ = e.transpose(out=psb[:, i * 128:(i + 1) * 128], in_=kT_sb[:, c * 128:(c + 1) * 128], identity=L.ident[:])
            return ins_
        P.op("pe", tr, reads=[bk, L.Bconst], writes=[bpsb])
        for oi, (dst, bd, scf) in enumerate(outs):
            for i in range(n):
                c = c0 + g0 + i
                eng = "act" if (oi + i) % 2 == 0 else "dve"
                if eng == "act":
                    P.op("act", lambda e, dst=dst, c=c, i=i, scf=scf: e.activation(out=dst[:, c, :], in_=psb[:, i * 128:(i + 1) * 128], func=AF.Identity, scale=scf(c)),
                         reads=[bpsb], writes=[bd])
                else:
                    P.op("dve", lambda e, dst=dst, c=c, i=i, scf=scf: e.tensor_scalar(out=dst[:, c, :], in0=psb[:, i * 128:(i + 1) * 128], scalar1=scf(c), scalar2=None, op0=ALU.mult),
                         reads=[bpsb], writes=[bd])


def stage_ret_passA(C, L, S, ins, Fout, BFout):
    cfg, P = C.cfg, C.P
    H, TL = cfg.H, cfg.TL
    NCL = TL // 128
    with contextlib.ExitStack() as st:
        R = ret_tables(C, L, st, ins)
        kT, BkT = S["kT"]
        vtm, Bvtm = S["vtm"]
        ks = [C.sb(st, "pak", [128, TL], BF16) for _ in range(2)]
        vs = [C.sb(st, "pav", [128, NCL, 256], BF16) for _ in range(2)]
        kf = [C.sb(st, "pakf", [128, NCL, 128], BF16) for _ in range(2)]
        kb = [C.sb(st, "pakb", [128, NCL, 128], BF16) for _ in range(2)]
        so = [C.sb(st, "paso", [128, 2, 256], F32) for _ in range(2)]
        for h in range(H):
            (k, bk), (v, bv), (kfx, bkf), (kbx, bkb), (o, bo) = ks[h % 2], vs[h % 2], kf[h % 2], kb[h % 2], so[h % 2]
            P.dma("sp", k[:], kT[h * 128:(h + 1) * 128, 0:TL], reads=[BkT], writes=[bk])
            P.dma("sp", v[:], vtm[0:TL, h * 256:(h + 1) * 256].rearrange("(c p) v -> p c v", p=128), reads=[Bvtm], writes=[bv])
            k_tokmajor(C, L, k, bk, 0, NCL, [
                (kfx, bkf, lambda c, h=h: R["vec"][:, h, 0, 2 + c:3 + c]),
                (kbx, bkb, lambda c, h=h: R["vec"][:, h, 1, 2 + NCL + c:3 + NCL + c])])
            for d, (kx, bkx) in enumerate(((kfx, bkf), (kbx, bkb))):
                ps, bps = L.ps[(2 * h + d) % 4]

                def mm(e, kx=kx, v=v, ps=ps):
                    for c in range(NCL):
                        ins_ = e.matmul(ps[:, 0:256], lhsT=kx[:, c, :], rhs=v[:, c, :], start=(c == 0), stop=(c == NCL - 1))
                    return ins_
                P.op("pe", mm, reads=[bkx, bv, R["B"]], writes=[bps])
                P.op("act", lambda e, o=o, d=d, ps=ps: e.activation(out=o[:, d, :], in_=ps[:, 0:256], func=AF.Copy), reads=[bps], writes=[bo])
            P.dma("sp", Fout[:, h, :, :].rearrange("d p v -> p d v"), o[:], reads=[bo], writes=[BFout])
        P.barrier()


def stage_ret(C, L, S, ins, Sall, BSall):
    cfg, P = C.cfg, C.P
    H, TL, T = cfg.H, cfg.TL, cfg.T
    NCL = TL // 128
    NCC = cfg.CTX // 128
    NCH = NCL + NCC
    with contextlib.ExitStack() as st:
        R = ret_tables(C, L, st, ins)
        VD = 2 + 2 * NCL
        qT, BqT = S["qT"]; kT, BkT = S["kT"]; vtm, Bvtm = S["vtm"]; rgT, BrgT = S["rgT"]; ogT, BogT = S["ogT"]
        NB = 2
        qs = [C.sb(st, "rq", [128, T], BF16) for _ in range(NB)]
        ks = [C.sb(st, "rk", [128, T], BF16) for _ in range(NB)]
        vs = [C.sb(st, "rv", [128, NCH, 256], BF16) for _ in range(NB)]
        gs = [C.sb(st, "rg", [128, 2, T], BF16) for _ in range(NB)]
        qfs = [C.sb(st, "rqf", [128, T], BF16) for _ in range(1)] * NB
        qbs = [C.sb(st, "rqb", [128, T], BF16) for _ in range(1)] * NB
        kfs = [C.sb(st, "rkf", [128, NCH, 128], BF16) for _ in range(NB)]
        kbs = [C.sb(st, "rkb", [128, NCH, 128], BF16) for _ in range(NB)]
        sbin = [C.sb(st, "rsbin", [128, NCH, 256], BF16) for _ in range(1)] * NB
        ogs = [C.sb(st, "rog", [128, 2, T], BF16) for _ in range(1)] * NB
        oraw, boraw = C.sb(st, "roraw", [128, 2, T], F32)
        sqb, bsqb = C.sb(st, "rsqb", [128, 2, T], BF16)
        rsb, brsb = C.sb(st, "rrsb", [128, T], F32)
        sall, bsall = C.sb(st, "rsall", [128, 4, 2, 256], F32)
        sfP = [C.sb(st, "rsf", [128, 256], F32) for _ in range(2)]
        sbP = [C.sb(st, "rsb", [128, 256], F32) for _ in range(2)]
        sfbR = Rot([C.sb(st, "rsfb", [128, 256], BF16) for _ in range(3)])
        sctx, bsctx = C.sb(st, "rsctx", [128, 2, 256], F32)
        smr = Rot([C.sb(st, "rsm", [128, 128], BF16) for _ in range(3)])
        psr = Rot([L.ps[i] for i in range(6)])
        for h in range(H):
            i2 = h % NB
            (q, bq), (k, bk), (v, bv), (g, bg) = qs[i2], ks[i2], vs[i2], gs[i2]
            (qf, bqf), (qb, bqb), (kf, bkf), (kb, bkb) = qfs[i2], qbs[i2], kfs[i2], kbs[i2]
            (sbi, bsbi), (og, bog) = sbin[i2], ogs[i2]
            P.dma("sp", q[:], qT[h * 128:(h + 1) * 128, :], reads=[BqT], writes=[bq])
            P.dma("sp", k[:], kT[h * 128:(h + 1) * 128, :], reads=[BkT], writes=[bk])
            P.dma("sp", v[:], vtm[:, h * 256:(h + 1) * 256].rearrange("(c p) v -> p c v", p=128), reads=[Bvtm], writes=[bv])
            P.dma("sp", g[:], rgT[h * 256:(h + 1) * 256, :].rearrange("(a p) t -> p a t", p=128), reads=[BrgT], writes=[bg])
            P.dma("sp", sall[:], Sall[:, :, h, :, :].rearrange("r d p v -> p r d v"), reads=[BSall], writes=[bsall])
            P.op("dve", lambda e, q=q, qf=qf, h=h: e.tensor_tensor(out=qf[:].rearrange("p (c i) -> p c i", i=128), in0=q[:].rearrange("p (c i) -> p c i", i=128),
                                                                   in1=bc(R["qdf"][:, h, :], [128, NCH, 128], 1), op=ALU.mult), reads=[bq, R["B"]], writes=[bqf])
            P.op("pool", lambda e, q=q, qb=qb, h=h: e.tensor_tensor(out=qb[:].rearrange("p (c i) -> p c i", i=128), in0=q[:].rearrange("p (c i) -> p c i", i=128),
                                                                    in1=bc(R["qdb"][:, h, :], [128, NCH, 128], 1), op=ALU.mult), reads=[bq, R["B"]], writes=[bqb])
            k_tokmajor(C, L, k, bk, 0, NCH, [
                (kf, bkf, lambda c, h=h: R["vec"][:, h, 0, 0:1]),
                (kb, bkb, lambda c, h=h: R["vec"][:, h, 1, 1:2])])
            gCf = R["gC"][:, h, 0:1]
            gCb = R["gC"][:, h, 1:2]
            for seq in ("ctx", "lat"):
                cs = list(range(NCL, NCH)) if seq == "ctx" else list(range(0, NCL))
                (sf, bsf), (sbk, bsb) = sfP[0], sbP[0]
                if seq == "ctx":
                    P.op("pool", lambda e, sf=sf: e.memset(sf[:], 0.0), writes=[bsf])
                    P.op("pool", lambda e, sbk=sbk: e.memset(sbk[:], 0.0), writes=[bsb])
                else:
                    for d, (sx, bsx) in enumerate(((sf, bsf), (sbk, bsb))):
                        vo = VD + 5 * d
                        P.op("dve", lambda e, sx=sx, d=d, vo=vo, h=h: e.tensor_scalar(out=sx[:], in0=sctx[:, d, :], scalar1=R["vec"][:, h, d, vo + 4:vo + 5],
                                                                                      scalar2=None, op0=ALU.mult), reads=[bsctx, R["B"]], writes=[bsx])
                        for r in range(4):
                            P.op("dve", lambda e, sx=sx, d=d, r=r, vo=vo, h=h: e.scalar_tensor_tensor(out=sx[:], in0=sall[:, r, d, :], scalar=R["vec"][:, h, d, vo + r:vo + r + 1],
                                                                                                      in1=sx[:], op0=ALU.mult, op1=ALU.add), reads=[bsall, R["B"]], writes=[bsx])
                pp = 0
                for c in reversed(cs):
                    (sbk, bsb), (sbn, bsbn) = sbP[pp], sbP[1 - pp]
                    pp = 1 - pp
                    P.op("act", lambda e, c=c, sbi=sbi, sbk=sbk: e.activation(out=sbi[:, c, :], in_=sbk[:], func=AF.Copy), reads=[bsb], writes=[bsbi])
                    ps, bps = psr.next()
                    P.op("pe", lambda e, c=c, ps=ps, kb=kb, v=v: e.matmul(ps[:, 0:256], lhsT=kb[:, c, :], rhs=v[:, c, :], start=True, stop=True),
                         reads=[bkb, bv], writes=[bps])
                    P.op("dve", lambda e, ps=ps, gCb=gCb, sbk=sbk, sbn=sbn: e.scalar_tensor_tensor(out=sbn[:], in0=sbk[:], scalar=gCb, in1=ps[:, 0:256], op0=ALU.mult, op1=ALU.add),
                         reads=[bps, R["B"], bsb], writes=[bsbn])
                (sbk, bsb) = sbP[pp]
                if seq == "ctx":
                    P.op("act", lambda e, sbk=sbk: e.activation(out=sctx[:, 1, :], in_=sbk[:], func=AF.Copy), reads=[bsb], writes=[bsctx])
                pf = 0
                for c in cs:
                    tsl = slice(c * 128, (c + 1) * 128)
                    (sf, bsf), (sfn, bsfn) = sfP[pf], sfP[1 - pf]
                    pf = 1 - pf
                    sfb, bsfb = sfbR.next()
                    P.op("act", lambda e, sfb=sfb, sf=sf: e.activation(out=sfb[:], in_=sf[:], func=AF.Copy), reads=[bsf], writes=[bsfb])
                    ps, bps = psr.next()
                    P.op("pe", lambda e, ps=ps, k=k, q=q, tsl=tsl: e.matmul(ps[:, 0:128], lhsT=k[:, tsl], rhs=q[:, tsl], start=True, stop=True),
                         reads=[bk, bq], writes=[bps])
                    sm, bsm = smr.next()
                    P.op("dve", lambda e, ps=ps, sm=sm, h=h: e.tensor_tensor(out=sm[:], in0=ps[:, 0:128], in1=R["mask"][:, h, :], op=ALU.mult),
                         reads=[bps, R["B"]], writes=[bsm])
                    po, bpo = psr.next()

                    def mmo(e, po=po, v=v, sm=sm, qf=qf, qb=qb, sbi=sbi, c=c, tsl=tsl, sfb=sfb):
                        for hv in range(2):
                            vsl = slice(hv * 128, (hv + 1) * 128)
                            e.matmul(po[:, vsl], lhsT=v[:, c, vsl], rhs=sm[:], start=True, stop=False)
                            e.matmul(po[:, vsl], lhsT=sfb[:, vsl], rhs=qf[:, tsl], start=False, stop=False)
                            ins_ = e.matmul(po[:, vsl], lhsT=sbi[:, c, vsl], rhs=qb[:, tsl], start=False, stop=True)
                        return ins_
                    P.op("pe", mmo, reads=[bv, bsm, bsfb, bqf, bqb, bsbi], writes=[bpo])
                    pu, bpu = psr.next()
                    P.op("pe", lambda e, pu=pu, kf=kf, v=v, c=c: e.matmul(pu[:, 0:256], lhsT=kf[:, c, :], rhs=v[:, c, :], start=True, stop=True),
                         reads=[bkf, bv], writes=[bpu])
                    P.op("dve", lambda e, pu=pu, gCf=gCf, sf=sf, sfn=sfn: e.scalar_tensor_tensor(out=sfn[:], in0=sf[:], scalar=gCf, in1=pu[:, 0:256], op0=ALU.mult, op1=ALU.add),
                         reads=[bpu, R["B"], bsf], writes=[bsfn])
                    P.op("act", lambda e, po=po, tsl=tsl: e.activation(out=oraw[:, :, tsl], in_=po[:, 0:256].rearrange("p (a i) -> p a i", i=128), func=AF.Copy),
                         reads=[bpo], writes=[boraw])
                (sf, bsf) = sfP[pf]
                if seq == "ctx":
                    P.op("act", lambda e, sf=sf: e.activation(out=sctx[:, 0, :], in_=sf[:], func=AF.Copy), reads=[bsf], writes=[bsctx])
            P.op("act", lambda e: e.activation(out=sqb[:], in_=oraw[:], func=AF.Square), reads=[boraw], writes=[bsqb])
            for t0 in range(0, T, 512):
                n_ = min(512, T - t0)
                pr, bpr = psr.next()

                def mmr(e, pr=pr, t0=t0, n_=n_):
                    e.matmul(pr[:, 0:n_], lhsT=L.ones_bf[:], rhs=sqb[:, 0, t0:t0 + n_], start=True, stop=False)
                    return e.matmul(pr[:, 0:n_], lhsT=L.ones_bf[:], rhs=sqb[:, 1, t0:t0 + n_], start=False, stop=True)
                P.op("pe", mmr, reads=[bsqb, L.Bconst], writes=[bpr])
                P.op("act", lambda e, pr=pr, t0=t0, n_=n_: e.activation(out=rsb[:, t0:t0 + n_], in_=pr[:, 0:n_], func=AF.Sqrt, bias=EPS, scale=1.0 / 256),
                     reads=[bpr], writes=[brsb])
            P.op("dve", lambda e: e.reciprocal(out=rsb[:], in_=rsb[:]), reads=[brsb], writes=[brsb])
            P.op("dve", lambda e: e.tensor_tensor(out=oraw[:], in0=oraw[:], in1=bc(rsb[:], [128, 2, T], 1), op=ALU.mult), reads=[boraw, brsb], writes=[boraw])
            P.op("pool", lambda e, og=og, g=g: e.tensor_tensor(out=og[:], in0=oraw[:], in1=g[:], op=ALU.mult), reads=[boraw, bg], writes=[bog])
            P.dma("sp", ogT[h * 256:(h + 1) * 256, :].rearrange("(a p) t -> p a t", p=128), og[:], reads=[bog], writes=[BogT])
        P.barrier()


NA_SLOT_KEYS = (768, 640, 576, 576, 704)


def na_pair_info(NP, m):
    if m == 0:
        return 0, 0
    if m == 1:
        return 1, 1
    if m == NP - 1:
        return 4, m - 1
    if m == NP - 2:
        return 3, m
    return 2, m


def stage_na(C, L, S, ins):
    cfg, P = C.cfg, C.P
    H, TL, T, NP = cfg.H, cfg.TL, cfg.T, cfg.NP
    TE = TL + 512
    NCE = TE // 128
    NCC = cfg.CTX // 128
    with contextlib.ExitStack() as st:
        nqT, BnqT = S["nqT"]; nkT, BnkT = S["nkT"]; nvtm, Bnvtm = S["nvtm"]; ngT, BngT = S["ngT"]; ogT, BogT = S["ogT"]
        NB = 2
        qs = [C.sb(st, "nq", [128, T], BF16) for _ in range(NB)]
        ke = [C.sb(st, "nke", [128, TE + cfg.CTX], BF16) for _ in range(NB)]
        ve = [C.sb(st, "nve", [128, NCE + NCC, 128], BF16) for _ in range(NB)]
        gs = [C.sb(st, "ngs", [128, T], BF16) for _ in range(NB)]
        ogs = [C.sb(st, "nog", [128, T], BF16) for _ in range(NB)]
        bias = [C.sb(st, "nbias", [128, 5, 6, 128], F32) for _ in range(NB)]
        er = Rot([C.sb(st, "ne", [128, 4, 128], BF16) for _ in range(4)])
        tr = Rot([C.sb(st, "nt", [128, 4, 128], F32) for _ in range(4)])
        onums = [C.sb(st, "nonum", [128, T], F32) for _ in range(NB)]
        odens = [C.sb(st, "noden", [128, T], F32) for _ in range(NB)]
        pss = Rot([L.ps[i] for i in range(3)])
        pacc = Rot([(L.ps[3], L.ps[4]), (L.ps[5], L.ps[6])])
        for h in range(H):
            i2 = h % NB
            (q, bq), (k, bk), (v, bv), (g, bg), (og, bog), (bi, bbi) = qs[i2], ke[i2], ve[i2], gs[i2], ogs[i2], bias[i2]
            (onum, bonum), (oden, boden) = onums[i2], odens[i2]
            hs = slice(h * 128, (h + 1) * 128)
            P.dma("sp", q[:], nqT[hs, :], reads=[BnqT], writes=[bq])
            P.dma("sp", g[:], ngT[hs, :], reads=[BngT], writes=[bg])
            P.dma("sp", k[:, 256:256 + TL], nkT[hs, 0:TL], reads=[BnkT], writes=[bk])
            P.dma("sp", k[:, TE:TE + cfg.CTX], nkT[hs, TL:T], reads=[BnkT], writes=[bk])
            hal = ins.get("halo")
            if hal is None:
                P.dma("sp", k[:, 0:256], ins["nk_halo"][hs, 0:256], writes=[bk])
                P.dma("sp", k[:, 256 + TL:TE], ins["nk_halo"][hs, 256:512], writes=[bk])
            else:
                for key, dsl in (("kb", slice(0, 256)), ("ka", slice(256 + TL, TE))):
                    if hal[key] is None:
                        P.op("pool", lambda e, k=k, dsl=dsl: e.memset(k[:, dsl], 0.0), writes=[bk])
                    else:
                        P.dma("sp", k[:, dsl], hal[key][0][hs, :], reads=[hal[key][1]], writes=[bk])
            P.dma("sp", v[:, 2:2 + TL // 128, :], nvtm[0:TL, hs].rearrange("(c p) d -> p c d", p=128), reads=[Bnvtm], writes=[bv])
            P.dma("sp", v[:, NCE:NCE + NCC, :], nvtm[TL:T, hs].rearrange("(c p) d -> p c d", p=128), reads=[Bnvtm], writes=[bv])
            if hal is None:
                P.dma("sp", v[:, 0:2, :], ins["nv_halo"][0:256, hs].rearrange("(c p) d -> p c d", p=128), writes=[bv])
                P.dma("sp", v[:, 2 + TL // 128:NCE, :], ins["nv_halo"][256:512, hs].rearrange("(c p) d -> p c d", p=128), writes=[bv])
            else:
                for key, c0_ in (("vb", 0), ("va", 2 + TL // 128)):
                    if hal[key] is None:
                        P.op("pool", lambda e, v=v, c0_=c0_: e.memset(v[:, c0_:c0_ + 2, :], 0.0), writes=[bv])
                    else:
                        P.dma("sp", v[:, c0_:c0_ + 2, :], hal[key][0][:, hs].rearrange("(c p) d -> p c d", p=128), reads=[hal[key][1]], writes=[bv])
            for s_ in range(5):
                P.dma("sp", bi[:, s_, :, :], ins["na_bias"][s_, h, :, :, :].rearrange("c p q -> p c q"), writes=[bbi])
            units = [("lat", m) for m in range(NP)] + [("ctx", j) for j in range(NCC)]
            for kind, m in units:
                if kind == "lat":
                    qsl = slice(m * 128, (m + 1) * 128)
                    slot, sc = na_pair_info(NP, m)
                    nk = NA_SLOT_KEYS[slot]
                    chunks = []
                    for j in range((nk + 127) // 128):
                        n = min(128, nk - j * 128)
                        chunks.append(((sc + j) * 128, sc + j, n, j))
                else:
                    qsl = slice(TL + m * 128, TL + (m + 1) * 128)
                    chunks = []
                for j in range(NCC):
                    chunks.append((TE + j * 128, NCE + j, 128, None))
                (pn, bpn), (pd, bpd) = pacc.next()
                nchunks = len(chunks)
                gps = []
                for b0 in range(0, nchunks, 4):
                    grp = chunks[b0:b0 + 4]
                    ps, bps = pss.next()

                    def mms(e, ps=ps, grp=grp, k=k, q=q, qsl=qsl):
                        for i, (kc0, vc, n, bj) in enumerate(grp):
                            ins_ = e.matmul(ps[0:n, i * 128:(i + 1) * 128], lhsT=k[:, kc0:kc0 + n], rhs=q[:, qsl], start=True, stop=True)
                        return ins_
                    P.op("pe", mms, reads=[bk, bq], writes=[bps])
                    gps.append((grp, ps, bps))
                gex = []
                for grp, ps, bps in gps:
                    ex, bex = er.next()
                    i = 0
                    while i < len(grp):
                        j = i
                        while j + 1 < len(grp) and (grp[j + 1][3] is None) == (grp[i][3] is None) and grp[j + 1][2] == grp[i][2]:
                            j += 1
                        n = grp[i][2]
                        cnt = j - i + 1
                        psv = ps[0:n, i * 128:(j + 1) * 128].rearrange("p (c q) -> p c q", q=128)
                        if grp[i][3] is not None:
                            tt, btt = tr.next()
                            bj0 = grp[i][3]
                            P.op("dve", lambda e, tt=tt, psv=psv, n=n, cnt=cnt, bj0=bj0, slot=slot, bi=bi: e.tensor_tensor(
                                out=tt[0:n, 0:cnt, :], in0=psv, in1=bi[0:n, slot, bj0:bj0 + cnt, :], op=ALU.add), reads=[bps, bbi], writes=[btt])
                            P.op("act", lambda e, ex=ex, tt=tt, n=n, cnt=cnt, i=i: e.activation(out=ex[0:n, i:i + cnt, :], in_=tt[0:n, 0:cnt, :], func=AF.Exp),
                                 reads=[btt], writes=[bex])
                        else:
                            P.op("act", lambda e, ex=ex, psv=psv, n=n, cnt=cnt, i=i: e.activation(out=ex[0:n, i:i + cnt, :], in_=psv, func=AF.Exp),
                                 reads=[bps], writes=[bex])
                        i = j + 1
                    gex.append((grp, ex, bex))
                done = 0
                for grp, ex, bex in gex:
                    def mmv(e, grp=grp, ex=ex, v=v, pn=pn, pd=pd, done=done, nchunks=nchunks):
                        for i, (kc0, vc, n, bj) in enumerate(grp):
                            first = (done + i == 0)
                            last = (done + i == nchunks - 1)
                            e.matmul(pn[:, 0:128], lhsT=v[0:n, vc, :], rhs=ex[0:n, i, :], start=first, stop=last)
                            ins_ = e.matmul(pd[:, 0:128], lhsT=L.ones_bf[0:n, :], rhs=ex[0:n, i, :], start=first, stop=last)
                        return ins_
                    P.op("pe", mmv, reads=[bex, bv, L.Bconst], writes=[bpn, bpd])
                    done += len(grp)
                P.op("act", lambda e, pn=pn, onum=onum, qsl=qsl: e.activation(out=onum[:, qsl], in_=pn[:, 0:128], func=AF.Copy), reads=[bpn], writes=[bonum])
                P.op("dve", lambda e, pd=pd, oden=oden, qsl=qsl: e.tensor_copy(out=oden[:, qsl], in_=pd[:, 0:128]), reads=[bpd], writes=[boden])
            P.op("dve", lambda e, oden=oden: e.reciprocal(out=oden[:], in_=oden[:]), reads=[boden], writes=[boden])
            P.op("dve", lambda e, onum=onum, oden=oden: e.tensor_tensor(out=onum[:], in0=onum[:], in1=oden[:], op=ALU.mult), reads=[bonum, boden], writes=[bonum])
            P.op("pool", lambda e, onum=onum, og=og, g=g: e.tensor_tensor(out=og[:], in0=onum[:], in1=g[:], op=ALU.mult), reads=[bonum, bg], writes=[bog])
            P.dma("sp", ogT[cfg.RV + h * 128:cfg.RV + (h + 1) * 128, :], og[:], reads=[bog], writes=[BogT])
        P.barrier()


def stage_conv(C, L, S, ins):
    cfg, P = C.cfg, C.P
    TL, T, CW = cfg.TL, cfg.T, cfg.CW
    NCT = CW // 128
    uT, BuT = S["uT"]; cgT, BcgT = S["cgT"]; ogT, BogT = S["ogT"]
    R0 = cfg.RV + cfg.NAW
    with contextlib.ExitStack() as st:
        par, bpar = C.sb(st, "cpar", [128, NCT, CONV_K + 3], F32)
        P.dma("sp", par[:], ins["cv_par"][:, :, :], writes=[bpar])
        BLKMAX = 512
        ue = [C.sb(st, "cue", [128, NCT, BLKMAX + 30], F32) for _ in range(2)]
        acc = [C.sb(st, "cacc", [128, NCT, BLKMAX], F32) for _ in range(2)]
        sqt = [C.sb(st, "csq", [128, NCT, BLKMAX], F32) for _ in range(2)]
        mean = [C.sb(st, "cmean", [128, BLKMAX], F32) for _ in range(2)]
        rstd = [C.sb(st, "crstd", [128, BLKMAX], F32) for _ in range(2)]
        cg = [C.sb(st, "ccg", [128, NCT, BLKMAX], BF16) for _ in range(2)]
        ob = [C.sb(st, "cob", [128, NCT, BLKMAX], BF16) for _ in range(2)]
        blocks = [("lat", t0, min(t0 + BLKMAX, TL)) for t0 in range(0, TL, BLKMAX)]
        blocks += [("ctx", TL + t0, min(TL + t0 + BLKMAX, T)) for t0 in range(0, cfg.CTX, BLKMAX)]
        uv = uT.rearrange("(c p) t -> p c t", p=128)
        hal = ins.get("halo")
        if hal is None:
            hv = ins["u_halo"].rearrange("(c p) t -> p c t", p=128)
            hvb = (hv[:, :, 0:15], None); hva = (hv[:, :, 15:30], None)
        else:
            hvb = None if hal["ub"] is None else (hal["ub"][0].rearrange("(c p) t -> p c t", p=128), hal["ub"][1])
            hva = None if hal["ua"] is None else (hal["ua"][0].rearrange("(c p) t -> p c t", p=128), hal["ua"][1])
        for bi_, (kind, t0, t1) in enumerate(blocks):
            n = t1 - t0
            (u, bu), (a, ba), (sq, bsq), (mn, bmn), (rs, brs), (cgx, bcg), (o, bo) = [x[bi_ % 2] for x in (ue, acc, sqt, mean, rstd, cg, ob)]
            lo = TL if kind == "ctx" else 0
            hi = T if kind == "ctx" else TL
            s0 = max(t0 - 15, lo); s1 = min(t1 + 15, hi)
            P.dma("sp", u[:, :, 15 - (t0 - s0):15 + n + (s1 - t1)], uv[:, :, s0:s1], reads=[BuT], writes=[bu])
            if t0 - 15 < lo:
                if kind == "lat" and hvb is not None:
                    P.dma("sp", u[:, :, 0:15], hvb[0], reads=([hvb[1]] if hvb[1] is not None else []), writes=[bu])
                else:
                    P.op("pool", lambda e, u=u: e.memset(u[:, :, 0:15], 0.0), writes=[bu])
            if t1 + 15 > hi:
                if kind == "lat" and hva is not None:
                    P.dma("sp", u[:, :, 15 + n:30 + n], hva[0], reads=([hva[1]] if hva[1] is not None else []), writes=[bu])
                else:
                    P.op("pool", lambda e, u=u, n=n: e.memset(u[:, :, 15 + n:30 + n], 0.0), writes=[bu])
            P.dma("sp", cgx[:, :, 0:n], cgT.rearrange("(c p) t -> p c t", p=128)[:, :, t0:t1], reads=[BcgT], writes=[bcg])
            for ct in range(NCT):
                eng = "dve"
                P.op(eng, lambda e, ct=ct, a=a, u=u, n=n: e.tensor_scalar(out=a[:, ct, 0:n], in0=u[:, ct, 0:n], scalar1=par[:, ct, 0:1], scalar2=par[:, ct, CONV_K:CONV_K + 1],
                                                                         op0=ALU.mult, op1=ALU.add), reads=[bu, bpar], writes=[ba])
                for kk in range(1, CONV_K):
                    P.op(eng, lambda e, ct=ct, a=a, u=u, n=n, kk=kk: e.scalar_tensor_tensor(out=a[:, ct, 0:n], in0=u[:, ct, kk:kk + n], scalar=par[:, ct, kk:kk + 1],
                                                                                       in1=a[:, ct, 0:n], op0=ALU.mult, op1=ALU.add), reads=[bu, bpar], writes=[ba])
            ps, bps = L.ps[bi_ % 2]

            def mm1(e, ps=ps, a=a, n=n):
                for ct in range(NCT):
                    ins_ = e.matmul(ps[:, 0:n], lhsT=L.ones_f[:], rhs=a[:, ct, 0:n], start=(ct == 0), stop=(ct == NCT - 1))
                return ins_
            P.op("pe", mm1, reads=[ba, L.Bconst], writes=[bps])
            P.op("act", lambda e, mn=mn, ps=ps, n=n: e.activation(out=mn[:, 0:n], in_=ps[:, 0:n], func=AF.Copy, scale=1.0 / CW), reads=[bps], writes=[bmn])
            P.op("dve", lambda e, a=a, mn=mn, n=n: e.tensor_tensor(out=a[:, :, 0:n], in0=a[:, :, 0:n], in1=bc(mn[:, 0:n], [128, NCT, n], 1), op=ALU.subtract),
                 reads=[ba, bmn], writes=[ba])
            P.op("act", lambda e, sq=sq, a=a, n=n: e.activation(out=sq[:, :, 0:n], in_=a[:, :, 0:n], func=AF.Square), reads=[ba], writes=[bsq])
            ps2, bps2 = L.ps[2 + bi_ % 2]

            def mm2(e, ps2=ps2, sq=sq, n=n):
                for ct in range(NCT):
                    ins_ = e.matmul(ps2[:, 0:n], lhsT=L.ones_f[:], rhs=sq[:, ct, 0:n], start=(ct == 0), stop=(ct == NCT - 1))
                return ins_
            P.op("pe", mm2, reads=[bsq, L.Bconst], writes=[bps2])
            P.op("act", lambda e, rs=rs, ps2=ps2, n=n: e.activation(out=rs[:, 0:n], in_=ps2[:, 0:n], func=AF.Sqrt, bias=EPS, scale=1.0 / CW), reads=[bps2], writes=[brs])
            P.op("dve", lambda e, rs=rs, n=n: e.reciprocal(out=rs[:, 0:n], in_=rs[:, 0:n]), reads=[brs], writes=[brs])
            P.op("dve", lambda e, a=a, rs=rs, n=n: e.tensor_tensor(out=a[:, :, 0:n], in0=a[:, :, 0:n], in1=bc(rs[:, 0:n], [128, NCT, n], 1), op=ALU.mult),
                 reads=[ba, brs], writes=[ba])
            for ct in range(NCT):
                P.op("act", lambda e, a=a, ct=ct, n=n: e.activation(out=a[:, ct, 0:n], in_=a[:, ct, 0:n], func=AF.Silu, scale=par[:, ct, CONV_K + 1:CONV_K + 2],
                                                                    bias=par[:, ct, CONV_K + 2:CONV_K + 3]), reads=[ba, bpar], writes=[ba])
            P.op("pool", lambda e, o=o, a=a, cgx=cgx, n=n: e.tensor_tensor(out=o[:, :, 0:n], in0=a[:, :, 0:n], in1=cgx[:, :, 0:n], op=ALU.mult),
                 reads=[ba, bcg], writes=[bo])
            P.dma("sp", ogT[R0:R0 + CW, t0:t1].rearrange("(c p) t -> p c t", p=128), o[:, :, 0:n], reads=[bo], writes=[BogT])
        P.barrier()


def tok_tiles(cfg, with_ctx=True):
    subs = [(t0, min(t0 + 512, cfg.TL)) for t0 in range(0, cfg.TL, 512)]
    if with_ctx:
        subs += [(cfg.TL, cfg.T)]
    tiles = []
    cur = []
    tot = 0
    for s in subs:
        n = s[1] - s[0]
        if tot + n > 1280 and cur:
            tiles.append(cur); cur = []; tot = 0
        cur.append(s); tot += n
    if cur:
        tiles.append(cur)
    return tiles


def common_inputs(nc, cfg):
    ins = {}
    d = lambda name, shape, dt=F32: nc.dram_tensor(name, list(shape), dt, kind="ExternalInput").ap()
    NCL = cfg.TL // 128
    ins["ident"] = d("ident", [128, 128], BF16)
    ins["mods"] = d("mods", [128, cfg.KC, 6])
    ins["norm_g"] = d("norm_g", [128, cfg.KC])
    ins["decay"] = d("decay", [128, 2 * cfg.H])
    ins["rope"] = d("rope", [2, 128, cfg.TL])
    ins["na_gain"] = d("na_gain", [128, 2])
    ins["rconst"] = d("rconst", [6, 128, 128])
    ins["rvec"] = d("rvec", [128, 2 + 2 * NCL + 10])
    return ins


def build_A(cfg):
    nc = bass.Bass("TRN2", target_bir_lowering=False)
    C = Ctx(nc, cfg)
    ins = common_inputs(nc, cfg)
    TL = cfg.TL
    cols = [cfg.off["ret_k"], cfg.off["ret_v"], cfg.off["na_k"], cfg.off["na_v"], cfg.off["cv_a"], cfg.off["cv_b"]]
    ncol = sum(n for _, n in cols)
    xT = nc.dram_tensor("xT", [cfg.D, TL], F32, kind="ExternalInput").ap()
    wA = nc.dram_tensor("wA", [cfg.D, ncol], F32, kind="ExternalInput").ap()
    Fout = nc.dram_tensor("Fout", [2, cfg.H, 128, 256], F32, kind="ExternalOutput").ap()
    nk_e = nc.dram_tensor("nk_e", [cfg.NAW, 512], BF16, kind="ExternalOutput").ap()
    nv_e = nc.dram_tensor("nv_e", [512, cfg.NAW], BF16, kind="ExternalOutput").ap()
    u_e = nc.dram_tensor("u_e", [cfg.CW, 512], F32, kind="ExternalOutput").ap()
    Bx, Bw = Buf("xT"), Buf("wA")
    cfgA = Cfg(cfg.D, cfg.H, cfg.SEQ, cfg.CTX, cfg.DEPTH, cfg.B)
    o = 0
    cfgA.off = dict(cfg.off)
    for nm in ("ret_k", "ret_v", "na_k", "na_v", "cv_a", "cv_b"):
        cfgA.off[nm] = (o, cfg.off[nm][1]); o += cfg.off[nm][1]
    C.cfg = cfgA
    with contextlib.ExitStack() as st:
        L = setup_common(C, st, ins)
        S = {}
        S["hxT"] = C.dram("hxT", [cfg.D, cfg.T], BF16)
        S["kT"] = C.dram("kT", [cfg.RQK, cfg.T], BF16)
        S["vtm"] = C.dram("vtm", [cfg.T, cfg.RV], BF16)
        S["nkT"] = (nk_e, Buf("nk_e"))
        S["nvtm"] = (nv_e, Buf("nv_e"))
        S["uT"] = (u_e, Buf("u_e"))
        stage_norm(C, L, xT, Bx, None, None, S["hxT"][0], S["hxT"][1], do_ctx=False)
        groups, setup = inproj_groups(C, L, S, ins, {"ret_k", "ret_v"})
        stage_gemm(C, L, S["hxT"][0], S["hxT"][1], [(0, cfg.D)], wA, tok_tiles(cfg, False), groups, setup)
        stage_ret_passA(C, L, S, ins, Fout, Buf("Fout"))
        hx_e, Bhe = C.dram("hx_e", [cfg.D, 512], BF16)
        with contextlib.ExitStack() as st2:
            t, bt = C.sb(st2, "edge", [128, cfg.KC, 512], BF16)
            hv = S["hxT"][0].rearrange("(c p) t -> p c t", p=128)
            C.P.dma("sp", t[:, :, 0:256], hv[:, :, 0:256], reads=[S["hxT"][1]], writes=[bt])
            C.P.dma("sp", t[:, :, 256:512], hv[:, :, TL - 256:TL], reads=[S["hxT"][1]], writes=[bt])
            C.P.dma("sp", hx_e.rearrange("(c p) t -> p c t", p=128), t[:], reads=[bt], writes=[Bhe])
            C.P.barrier()
        groups, setup = inproj_groups(C, L, S, ins, {"na_k", "na_v", "cv_ab"})
        stage_gemm(C, L, hx_e, Bhe, [(0, cfg.D)], wA, [[(0, 512)]], groups, setup)
        C.P.emit()
    return nc


def build_B(cfg):
    nc = bass.Bass("TRN2", target_bir_lowering=False)
    C = Ctx(nc, cfg)
    ins = common_inputs(nc, cfg)
    TL, T, D = cfg.TL, cfg.T, cfg.D
    d = lambda name, shape, dt=F32: nc.dram_tensor(name, list(shape), dt, kind="ExternalInput").ap()
    xT = d("xT", [D, TL]); cT = d("cT", [D, cfg.CTX])
    w_in = d("w_in", [D, cfg.N_IN]); w_br = d("w_br", [cfg.KBR, D]); w_out = d("w_out", [D, D])
    ins["Sall"] = d("Sall", [4, 2, cfg.H, 128, 256])
    ins["nk_halo"] = d("nk_halo", [cfg.NAW, 512], BF16)
    ins["nv_halo"] = d("nv_halo", [512, cfg.NAW], BF16)
    ins["u_halo"] = d("u_halo", [cfg.CW, 30])
    ins["na_bias"] = d("na_bias", [5, cfg.H, 6, 128, 128])
    ins["cv_par"] = d("cv_par", [128, cfg.CW // 128, CONV_K + 3])
    xo = nc.dram_tensor("xoT", [D, TL], F32, kind="ExternalOutput").ap()
    co = nc.dram_tensor("coT", [D, cfg.CTX], F32, kind="ExternalOutput").ap()
    Bx, Bc, Bxo, Bco = Buf("xT"), Buf("cT"), Buf("xo"), Buf("co")
    with contextlib.ExitStack() as st:
        L = setup_common(C, st, ins)
        S = {}
        S["hxT"] = C.dram("hxT", [D, T], BF16)
        S["qT"] = C.dram("qT", [cfg.RQK, T], BF16)
        S["kT"] = C.dram("kT", [cfg.RQK, T], BF16)
        S["vtm"] = C.dram("vtm", [T, cfg.RV], BF16)
        S["rgT"] = C.dram("rgT", [cfg.RV, T], BF16)
        S["nqT"] = C.dram("nqT", [cfg.NAW, T], BF16)
        S["nkT"] = C.dram("nkT", [cfg.NAW, T], BF16)
        S["nvtm"] = C.dram("nvtm", [T, cfg.NAW], BF16)
        S["ngT"] = C.dram("ngT", [cfg.NAW, T], BF16)
        S["uT"] = C.dram("uT", [cfg.CW, T], F32)
        S["cgT"] = C.dram("cgT", [cfg.CW, T], BF16)
        S["sgT"] = C.dram("sgT", [3 * D, T], BF16)
        S["ogT"] = C.dram("ogT", [cfg.KBR, T], BF16)
        S["mT"] = C.dram("mT", [D, T], BF16)
        stage_norm(C, L, xT, Bx, cT, Bc, S["hxT"][0], S["hxT"][1])
        groups, setup = inproj_groups(C, L, S, ins, {"ret_q", "ret_k", "ret_v", "na_q", "na_k", "na_v", "cv_ab", "silu", "gates"})
        tiles = tok_tiles(cfg, True)
        stage_gemm(C, L, S["hxT"][0], S["hxT"][1], [(0, D)], w_in, tiles, groups, setup)
        stage_ret(C, L, S, ins, ins["Sall"], Buf("Sall"))
        stage_na(C, L, S, ins)
        stage_conv(C, L, S, ins)
        P = C.P
        sgT, BsgT = S["sgT"]; mT, BmT = S["mT"]

        def setup_m(st2):
            env = {}
            env["sg"] = Rot([C.sb(st2, "msg", [128, 3, 512], BF16) for _ in range(3)])
            env["t"] = Rot([C.sb(st2, "mt", [128, 3, 512], F32) for _ in range(2)])
            env["o"] = Rot([C.sb(st2, "mo", [128, 512], BF16) for _ in range(3)])
            return env

        def epi_m(env, col0, sub, pss):
            nt = sub[1] - sub[0]
            sg, bsg = env["sg"].next()
            t, bt = env["t"].next()
            o, bo = env["o"].next()
            P.dma("sp", sg[:, :, 0:nt], sgT.rearrange("(b d) t -> d b t", b=3)[col0:col0 + 128, :, sub[0]:sub[1]], reads=[BsgT], writes=[bsg])
            for b in range(3):
                ps, bps = pss[b]
                P.op("dve", lambda e, b=b, ps=ps: e.tensor_tensor(out=t[:, b, 0:nt], in0=ps[:, 0:nt], in1=sg[:, b, 0:nt], op=ALU.mult),
                     reads=[bps, bsg], writes=[bt])
            P.op("dve", lambda e: e.tensor_tensor(out=t[:, 0, 0:nt], in0=t[:, 0, 0:nt], in1=t[:, 1, 0:nt], op=ALU.add), reads=[bt], writes=[bt])
            P.op("dve", lambda e: e.tensor_tensor(out=o[:, 0:nt], in0=t[:, 0, 0:nt], in1=t[:, 2, 0:nt], op=ALU.add), reads=[bt], writes=[bo])
            P.dma("sp", mT[col0:col0 + 128, sub[0]:sub[1]], o[:, 0:nt], reads=[bo], writes=[BmT])
        groups = [dict(cols=[(c, 256)], mode="fm", epi=epi_m) for c in range(0, D, 256)]
        stage_gemm(C, L, S["ogT"][0], S["ogT"][1], [(0, cfg.RV), (cfg.RV, cfg.NAW), (cfg.RV + cfg.NAW, cfg.CW)], w_br, tiles, groups, setup_m)

        def setup_f(st2):
            env = {}
            env["x"] = Rot([C.sb(st2, "fx", [128, 512], F32) for _ in range(3)])
            return env

        def epi_f(env, col0, sub, pss):
            nt = sub[1] - sub[0]
            ps, bps = pss[0]
            x, bx = env["x"].next()
            kc = col0 // 128
            if sub[0] >= TL:
                src, Bsrc, dst, Bdst, s, c0 = cT, Bc, co, Bco, 1, sub[0] - TL
            else:
                src, Bsrc, dst, Bdst, s, c0 = xT, Bx, xo, Bxo, 0, sub[0]
            P.dma("sp", x[:, 0:nt], src[col0:col0 + 128, c0:c0 + nt], reads=[Bsrc], writes=[bx])
            P.op("dve", lambda e: e.scalar_tensor_tensor(out=x[:, 0:nt], in0=ps[:, 0:nt], scalar=L.GT[:, s, kc:kc + 1], in1=x[:, 0:nt],
                                                         op0=ALU.mult, op1=ALU.add), reads=[bps, bx, L.Bpar], writes=[bx])
            P.dma("sp", dst[col0:col0 + 128, c0:c0 + nt], x[:, 0:nt], reads=[bx], writes=[Bdst])
        groups = [dict(cols=[(c, 256)], mode="fm", epi=epi_f) for c in range(0, D, 256)]
        stage_gemm(C, L, S["mT"][0], S["mT"][1], [(0, D)], w_out, tiles, groups, setup_f)
        C.P.emit()
    return nc


def build_ada(cfg):
    nc = bass.Bass("TRN2", target_bir_lowering=False)
    C = Ctx(nc, cfg)
    P = C.P
    D, KC, Lyr = cfg.D, cfg.KC, cfg.DEPTH
    NCOL = 3 * D // 8
    cv = nc.dram_tensor("cv", [128, KC, 3], F32, kind="ExternalInput").ap()
    wa = nc.dram_tensor("wa", [Lyr, D, NCOL], F32, kind="ExternalInput").ap()
    ba = nc.dram_tensor("ba", [Lyr, 3, NCOL], F32, kind="ExternalInput").ap()
    out = nc.dram_tensor("out", [Lyr, 3, NCOL], F32, kind="ExternalOutput").ap()
    CB = min(512, NCOL)
    with contextlib.ExitStack() as st:
        ps = [(st.enter_context(nc.psum_tensor(C.name("ps"), [128, 512], F32)), Buf("ps")) for _ in range(2)]
        c, bcv = C.sb(st, "c", [128, KC, 3], F32)
        P.dma("sp", c[:], cv[:, :, :], writes=[bcv])
        P.op("act", lambda e: e.activation(out=c[:], in_=c[:], func=AF.Silu), reads=[bcv], writes=[bcv])
        wbs = [C.sb(st, "w", [128, KC, CB], F32) for _ in range(2)]
        obs = [C.sb(st, "o", [3, CB], F32) for _ in range(2)]
        bbs = [C.sb(st, "b", [3, CB], F32) for _ in range(2)]
        Bout = Buf("out")
        i = 0
        for l in range(Lyr):
            for c0 in range(0, NCOL, CB):
                (w, bw), (o, bo), (b, bb), (p, bp) = wbs[i % 2], obs[i % 2], bbs[i % 2], ps[i % 2]
                i += 1
                half = KC // 2
                P.dma("sp", w[:, 0:half, :], wa[l].rearrange("(c p) n -> p c n", p=128)[:, 0:half, c0:c0 + CB], writes=[bw])
                P.dma("sp", w[:, half:KC, :], wa[l].rearrange("(c p) n -> p c n", p=128)[:, half:KC, c0:c0 + CB], writes=[bw])
                P.dma("sp", b[:], ba[l, :, c0:c0 + CB], writes=[bb])

                def mm(e, w=w, p=p):
                    for kc in range(KC):
                        ins_ = e.matmul(p[0:3, 0:CB], lhsT=c[:, kc, :], rhs=w[:, kc, :], start=(kc == 0), stop=(kc == KC - 1))
                    return ins_
                P.op("pe", mm, reads=[bw, bcv], writes=[bp])
                P.op("dve", lambda e, o=o, p=p, b=b: e.tensor_tensor(out=o[:], in0=p[0:3, 0:CB], in1=b[:], op=ALU.add), reads=[bp, bb], writes=[bo])
                P.dma("sp", out[l, :, c0:c0 + CB], o[:], reads=[bo], writes=[Bout])
        P.emit()
    return nc


def rope_tables(cfg, q):
    t = np.arange(cfg.TL) + q * cfg.TL
    row = (t // GRID_W).astype(np.float32)
    col = (t % GRID_W).astype(np.float32)
    n_freq = 32
    inv = (10000.0 ** (-np.arange(n_freq, dtype=np.float32) / n_freq)).astype(np.float32)
    ang = np.concatenate([row[:, None] * inv, col[:, None] * inv], axis=-1).astype(np.float32)
    cos = np.cos(ang).T.astype(np.float32)
    sin = np.sin(ang).T.astype(np.float32)
    cosf = np.concatenate([cos, cos], 0)
    sinf = np.concatenate([sin, -sin], 0)
    return np.stack([cosf, sinf]).astype(np.float32)


def ret_consts(cfg, q):
    i = np.arange(128, dtype=np.float32)
    J, I = np.meshgrid(i, i, indexing="ij")
    relF = np.maximum(I - J, 0); relB = np.maximum(J - I, 0)
    indF = (I >= J).astype(np.float32); indB = (J >= I).astype(np.float32)
    qdf = np.broadcast_to(i[None, :] + 1.0, (128, 128)); qdb = np.broadcast_to(128.0 - i[None, :], (128, 128))
    rconst = np.stack([relF, relB, indF, indB, qdf, qdb]).astype(np.float32)
    NCL = cfg.TL // 128
    TL = cfg.TL
    vec = np.zeros((128, 2 + 2 * NCL + 10), np.float32)
    vec[:, 0] = 127.0 - i
    vec[:, 1] = i
    for c in range(NCL):
        vec[:, 2 + c] = TL - 1 - (128 * c + i)
        vec[:, 2 + NCL + c] = 128 * c + i
    o = 2 + 2 * NCL
    for r in range(4):
        vec[:, o + r] = TL * (q - 1 - r) if r < q else BIGD
        vec[:, o + 5 + r] = TL * (r - q - 1) if r > q else BIGD
    vec[:, o + 4] = TL * q
    vec[:, o + 9] = TL * (3 - q)
    return rconst, vec


def na_bias_table(cfg, q, rpb):
    NP, H = cfg.NP, cfg.H
    rows = cfg.ROWS
    out = np.full((5, H, 768, 128), NEG, np.float32)
    qi = np.arange(128)
    for m in sorted(set([0, 1, 2, NP - 2, NP - 1])):
        if m < 0 or m >= NP:
            continue
        slot, sc = na_pair_info(NP, m)
        nk = NA_SLOT_KEYS[slot]
        kk = np.arange(nk)
        ext_tok = 128 * sc + kk
        gr = q * cfg.LR + ext_tok // 64 - 4
        kc = ext_tok % 64
        r = q * cfg.LR + 2 * m + qi // 64
        c = qi % 64
        rs = np.clip(r - 4, 0, rows - 8)
        cs = np.clip(c - 8, 0, GRID_W - 16)
        valid = ((gr[:, None] >= rs[None, :]) & (gr[:, None] < rs[None, :] + 8) & (kc[:, None] >= cs[None, :]) & (kc[:, None] < cs[None, :] + 16)
                 & (gr[:, None] >= 0) & (gr[:, None] < rows))
        ri = np.clip(gr[:, None] - r[None, :] + 7, 0, 14)
        ci = np.clip(kc[:, None] - c[None, :] + 15, 0, 30)
        for h in range(H):
            vals = rpb[h][ri, ci]
            out[slot, h, :nk, :] = np.where(valid, vals, np.float32(NEG))
    return out.reshape(5, H, 6, 128, 128)


def fm(v, KC):
    return np.ascontiguousarray(v.reshape(KC, 128).T)


_CACHE = {}


def _get(name, builder, cfg):
    key = (name, cfg.D, cfg.H, cfg.SEQ, cfg.DEPTH)
    if key not in _CACHE:
        _CACHE[key] = builder(cfg)
    return _CACHE[key]


def run_model(cfg, x, c, ctx, c_ctx, w_ada, b_ada, norm_g, w_in, ret_decay_f, ret_decay_b, w_ret_o,
              na_q_gain, na_k_gain, na_rpb, w_na_o, cv_dw, cv_db, cv_ln_g, cv_ln_b, w_cv_o, w_out):
    f32 = np.float32
    D, KC, H, TL, NQ = cfg.D, cfg.KC, cfg.H, cfg.TL, cfg.NQ
    cores = list(range(8))
    x = np.asarray(x, f32); ctx = np.asarray(ctx, f32)
    w_ada = np.asarray(w_ada, f32)[:cfg.DEPTH]; b_ada = np.asarray(b_ada, f32)[:cfg.DEPTH]
    nc_ada = _get("ada", build_ada, cfg)
    cvec = np.stack([np.asarray(c[0], f32), np.asarray(c[1], f32), np.asarray(c_ctx, f32)], -1)
    cvl = np.ascontiguousarray(cvec.reshape(KC, 128, 3).transpose(1, 0, 2))
    NCOL = 3 * D // 8
    in_maps = []
    for k in cores:
        in_maps.append({"cv": cvl,
                        "wa": np.ascontiguousarray(np.asarray(w_ada, f32)[:, :, k * NCOL:(k + 1) * NCOL]),
                        "ba": np.ascontiguousarray(np.broadcast_to(np.asarray(b_ada, f32)[:, None, k * NCOL:(k + 1) * NCOL], (cfg.DEPTH, 3, NCOL)))})
    res = run_bass_kernel_spmd(nc_ada, in_maps, core_ids=cores)
    mods_all = np.concatenate([r["out"] for r in res.results], axis=-1)
    ident = np.eye(128, dtype=f32).astype(NPBF)
    xT = [np.ascontiguousarray(x[k // NQ, (k % NQ) * TL:(k % NQ + 1) * TL, :].T) for k in cores]
    cT = [np.ascontiguousarray(ctx[b].T) for b in range(cfg.B)]
    consts = [ret_consts(cfg, k % NQ) for k in cores]
    ropes = [rope_tables(cfg, k % NQ) for k in cores]
    nc_A = _get("A", build_A, cfg)
    nc_B = _get("B", build_B, cfg)
    for l in range(cfg.DEPTH):
        ml = mods_all[l]
        common = []
        for k in cores:
            b = k // NQ
            mods = np.stack([fm(ml[b, 0:D], KC), fm(ml[b, D:2 * D], KC), fm(ml[b, 2 * D:3 * D], KC),
                             fm(ml[2, 0:D], KC), fm(ml[2, D:2 * D], KC), fm(ml[2, 2 * D:3 * D], KC)], -1)
            dec = np.concatenate([np.asarray(ret_decay_f[l], f32), np.asarray(ret_decay_b[l], f32)])
            common.append({
                "ident": ident, "mods": np.ascontiguousarray(mods), "norm_g": fm(np.asarray(norm_g[l], f32), KC),
                "decay": np.ascontiguousarray(np.broadcast_to(dec[None, :], (128, 2 * H))),
                "rope": ropes[k], "na_gain": np.ascontiguousarray(np.stack([np.asarray(na_q_gain[l], f32), np.asarray(na_k_gain[l], f32)], -1)),
                "rconst": consts[k][0], "rvec": consts[k][1]})
        wl = np.asarray(w_in[l], f32)
        sel = []
        for nm in ("ret_k", "ret_v", "na_k", "na_v", "cv_a", "cv_b"):
            o, n = cfg.off[nm]
            sel.append(wl[:, o:o + n])
        wA = np.ascontiguousarray(np.concatenate(sel, axis=1))
        in_maps = [dict(common[k], xT=xT[k], wA=wA) for k in cores]
        resA = run_bass_kernel_spmd(nc_A, in_maps, core_ids=cores).results
        w_br = np.ascontiguousarray(np.concatenate([np.asarray(w_ret_o[l], f32), np.asarray(w_na_o[l], f32), np.asarray(w_cv_o[l], f32)], 0))
        wo = np.asarray(w_out[l], f32)
        NCT = cfg.CW // 128
        cvp = np.concatenate([np.asarray(cv_dw[l], f32).T, np.asarray(cv_db[l], f32)[:, None], np.asarray(cv_ln_g[l], f32)[:, None],
                              np.asarray(cv_ln_b[l], f32)[:, None]], axis=1)
        cvp = np.ascontiguousarray(cvp.reshape(NCT, 128, CONV_K + 3).transpose(1, 0, 2))
        rpb = np.asarray(na_rpb[l], f32)
        in_maps = []
        for k in cores:
            b, q = k // NQ, k % NQ
            Sall = np.stack([resA[b * NQ + r]["Fout"] for r in range(NQ)])
            nkh = np.zeros((cfg.NAW, 512), NPBF); nvh = np.zeros((512, cfg.NAW), NPBF); uh = np.zeros((cfg.CW, 30), f32)
            if q > 0:
                p = resA[k - 1]
                nkh[:, 0:256] = p["nk_e"][:, 256:512]; nvh[0:256] = p["nv_e"][256:512]; uh[:, 0:15] = p["u_e"][:, 512 - 15:512]
            if q < NQ - 1:
                p = resA[k + 1]
                nkh[:, 256:512] = p["nk_e"][:, 0:256]; nvh[256:512] = p["nv_e"][0:256]; uh[:, 15:30] = p["u_e"][:, 0:15]
            in_maps.append(dict(common[k], xT=xT[k], cT=cT[b], w_in=wl, w_br=w_br, w_out=wo, Sall=np.ascontiguousarray(Sall),
                                nk_halo=nkh, nv_halo=nvh, u_halo=uh, na_bias=na_bias_table(cfg, q, rpb), cv_par=cvp))
        resB = run_bass_kernel_spmd(nc_B, in_maps, core_ids=cores).results
        xT = [resB[k]["xoT"] for k in cores]
        if getattr(cfg, "debug", False):
            DBGOUT[l] = resB
        if l < cfg.DEPTH - 1:
            cT = [resB[b * NQ]["coT"] for b in range(cfg.B)]
    out = np.empty((cfg.B, cfg.SEQ, D), f32)
    for k in cores:
        out[k // NQ, (k % NQ) * TL:(k % NQ + 1) * TL, :] = xT[k].T
    return out


def setup_global(C, st, ins):
    cfg, P, nc = C.cfg, C.P, C.nc
    L = Layer()
    KC, H = cfg.KC, cfg.H
    L.ps = []
    for i in range(7):
        t = st.enter_context(nc.psum_tensor(C.name("ps"), [128, 512], F32))
        L.ps.append((t, Buf(f"ps{i}")))
    t = st.enter_context(nc.psum_tensor(C.name("psb"), [128, 1024], BF16))
    L.psb = (t, Buf("psb"))
    L.ones_bf, _ = C.sb(st, "ones_bf", [128, 128], BF16)
    L.ones_f, _ = C.sb(st, "ones_f", [128, 128], F32)
    L.ident, _ = C.sb(st, "ident", [128, 128], BF16)
    L.Bconst = Buf("const")
    P.op("pool", lambda e: e.memset(L.ones_bf[:], 1.0), writes=[L.Bconst])
    P.op("pool", lambda e: e.memset(L.ones_f[:], 1.0), writes=[L.Bconst])
    P.dma("sp", L.ident[:], ins["ident"][:, :], writes=[L.Bconst])
    L.modsAll, _ = C.sb(st, "modsAll", [128, cfg.DEPTH, 3 * KC, 2], F32)
    L.Bmods = Buf("modsAll")
    L.ng, _ = C.sb(st, "ng", [128, KC], F32)
    L.G, _ = C.sb(st, "G", [128, 2, KC], F32)
    L.SH, _ = C.sb(st, "SH", [128, 2, KC], F32)
    L.GT, _ = C.sb(st, "GT", [128, 2, KC], F32)
    L.Bpar = Buf("par")
    L.lg, _ = C.sb(st, "lg", [128, 2 * H], F32)
    L.lgt, _ = C.sb(st, "lgt", [128, 2 * H], F32)
    L.lgt2, _ = C.sb(st, "lgt2", [128, 2 * H], F32)
    L.Blg = Buf("lg")
    return L


def stage_ada_fused(C, L, cv, w_ada, b_ada_fm):
    cfg, P = C.cfg, C.P
    KC, D = cfg.KC, cfg.D
    with contextlib.ExitStack() as st:
        c, bcv = C.sb(st, "adac", [128, KC, 2], F32)
        P.dma("sp", c[:], cv[:, :, :], writes=[bcv])
        P.op("act", lambda e: e.activation(out=c[:], in_=c[:], func=AF.Silu), reads=[bcv], writes=[bcv])
        bfm, bb = C.sb(st, "adab", [128, cfg.DEPTH, 3 * KC], F32)
        P.dma("sp", bfm[:], b_ada_fm[:, :, :], writes=[bb])
        wbs = [C.sb(st, "adaw", [128, KC, 512], F32) for _ in range(2)]
        i = 0
        for l in range(cfg.DEPTH):
            wv = w_ada[l].rearrange("(c p) n -> p c n", p=128)
            for c0 in range(0, 3 * D, 512):
                (w, bw) = wbs[i % 2]
                ps, bps = L.ps[i % 2]
                i += 1
                half = KC // 2
                P.dma("sp", w[:, 0:half, :], wv[:, 0:half, c0:c0 + 512], writes=[bw])
                P.dma("sp", w[:, half:KC, :], wv[:, half:KC, c0:c0 + 512], writes=[bw])

                def mm(e, w=w, ps=ps):
                    for jj in range(4):
                        for kc in range(KC):
                            ins_ = e.matmul(ps[:, jj * 2:jj * 2 + 2], lhsT=w[:, kc, jj * 128:(jj + 1) * 128], rhs=c[:, kc, :],
                                            start=(kc == 0), stop=(kc == KC - 1))
                    return ins_
                P.op("pe", mm, reads=[bw, bcv], writes=[bps])
                for jj in range(4):
                    j = c0 // 128 + jj
                    P.op("act", lambda e, l=l, j=j, jj=jj, ps=ps: e.activation(out=L.modsAll[:, l, j, :], in_=ps[:, jj * 2:jj * 2 + 2], func=AF.Identity,
                                                                             bias=bfm[:, l, j:j + 1]), reads=[bps, bb], writes=[L.Bmods])
        P.barrier()


def setup_layer(C, L, l, norm_g_l, decay_l):
    cfg, P = C.cfg, C.P
    KC = cfg.KC
    P.dma("sp", L.ng[:], norm_g_l, writes=[L.Bpar])
    for s_ in range(2):
        P.op("dve", lambda e, s_=s_: e.scalar_tensor_tensor(out=L.G[:, s_, :], in0=L.modsAll[:, l, KC:2 * KC, s_], scalar=1.0,
                                                            in1=L.ng[:], op0=ALU.add, op1=ALU.mult), reads=[L.Bpar, L.Bmods], writes=[L.Bpar])
        P.op("dve", lambda e, s_=s_: e.tensor_copy(out=L.SH[:, s_, :], in_=L.modsAll[:, l, 0:KC, s_]), reads=[L.Bmods], writes=[L.Bpar])
        P.op("dve", lambda e, s_=s_: e.tensor_copy(out=L.GT[:, s_, :], in_=L.modsAll[:, l, 2 * KC:3 * KC, s_]), reads=[L.Bmods], writes=[L.Bpar])
    tmp, tmp2 = L.lgt, L.lgt2
    P.dma("sp", L.lg[:], decay_l, writes=[L.Blg])
    P.op("act", lambda e: e.activation(out=tmp[:], in_=L.lg[:], func=AF.Exp, scale=-1.0), reads=[L.Blg], writes=[L.Blg])
    P.op("dve", lambda e: e.tensor_scalar(out=tmp2[:], in0=tmp[:], scalar1=0.2, scalar2=-0.25, op0=ALU.mult, op1=ALU.add),
         reads=[L.Blg], writes=[L.Blg])
    for cst in (1.0 / 3.0, -0.5, 1.0):
        P.op("dve", lambda e: e.tensor_tensor(out=tmp2[:], in0=tmp2[:], in1=tmp[:], op=ALU.mult), reads=[L.Blg], writes=[L.Blg])
        P.op("dve", lambda e, cst=cst: e.tensor_scalar(out=tmp2[:], in0=tmp2[:], scalar1=cst, scalar2=None, op0=ALU.add),
             reads=[L.Blg], writes=[L.Blg])
    P.op("dve", lambda e: e.tensor_tensor(out=tmp2[:], in0=tmp2[:], in1=tmp[:], op=ALU.mult), reads=[L.Blg], writes=[L.Blg])
    P.op("dve", lambda e: e.tensor_scalar(out=L.lg[:], in0=tmp2[:], scalar1=-1.0, scalar2=None, op0=ALU.mult),
         reads=[L.Blg], writes=[L.Blg])
    P.barrier()


def stage_merge(C, L, S, w_br, tiles):
    cfg, P = C.cfg, C.P
    D = cfg.D
    sgT, BsgT = S["sgT"]; mT, BmT = S["mT"]

    def setup_m(st2):
        env = {}
        env["sg"] = Rot([C.sb(st2, "msg", [128, 3, 512], BF16) for _ in range(3)])
        env["t"] = Rot([C.sb(st2, "mt", [128, 3, 512], F32) for _ in range(2)])
        env["o"] = Rot([C.sb(st2, "mo", [128, 512], BF16) for _ in range(3)])
        return env

    def epi_m(env, col0, sub, pss):
        nt = sub[1] - sub[0]
        sg, bsg = env["sg"].next()
        t, bt = env["t"].next()
        o, bo = env["o"].next()
        P.dma("sp", sg[:, :, 0:nt], sgT.rearrange("(b d) t -> d b t", b=3)[col0:col0 + 128, :, sub[0]:sub[1]], reads=[BsgT], writes=[bsg])
        for b in range(3):
            ps, bps = pss[b]
            P.op("dve", lambda e, b=b, ps=ps: e.tensor_tensor(out=t[:, b, 0:nt], in0=ps[:, 0:nt], in1=sg[:, b, 0:nt], op=ALU.mult),
                 reads=[bps, bsg], writes=[bt])
        P.op("dve", lambda e: e.tensor_tensor(out=t[:, 0, 0:nt], in0=t[:, 0, 0:nt], in1=t[:, 1, 0:nt], op=ALU.add), reads=[bt], writes=[bt])
        P.op("dve", lambda e: e.tensor_tensor(out=o[:, 0:nt], in0=t[:, 0, 0:nt], in1=t[:, 2, 0:nt], op=ALU.add), reads=[bt], writes=[bo])
        P.dma("sp", mT[col0:col0 + 128, sub[0]:sub[1]], o[:, 0:nt], reads=[bo], writes=[BmT])
    groups = [dict(cols=[(c, 256)], mode="fm", epi=epi_m) for c in range(0, D, 256)]
    stage_gemm(C, L, S["ogT"][0], S["ogT"][1], [(0, cfg.RV), (cfg.RV, cfg.NAW), (cfg.RV + cfg.NAW, cfg.CW)], w_br, tiles, groups, setup_m)


def stage_final(C, L, S, w_out, tiles, xsrc, Bxs, csrc, Bcs, xdst, Bxd, cdst, Bcd):
    cfg, P = C.cfg, C.P
    D, TL = cfg.D, cfg.TL

    def setup_f(st2):
        return {"x": Rot([C.sb(st2, "fx", [128, 512], F32) for _ in range(3)])}

    def epi_f(env, col0, sub, pss):
        nt = sub[1] - sub[0]
        ps, bps = pss[0]
        x, bx = env["x"].next()
        kc = col0 // 128
        if sub[0] >= TL:
            src, Bsrc, dst, Bdst, s_, c0 = csrc, Bcs, cdst, Bcd, 1, sub[0] - TL
        else:
            src, Bsrc, dst, Bdst, s_, c0 = xsrc, Bxs, xdst, Bxd, 0, sub[0]
        P.dma("sp", x[:, 0:nt], src[col0:col0 + 128, c0:c0 + nt], reads=[Bsrc], writes=[bx])
        P.op("dve", lambda e: e.scalar_tensor_tensor(out=x[:, 0:nt], in0=ps[:, 0:nt], scalar=L.GT[:, s_, kc:kc + 1], in1=x[:, 0:nt],
                                                     op0=ALU.mult, op1=ALU.add), reads=[bps, bx, L.Bpar], writes=[bx])
        P.dma("sp", dst[col0:col0 + 128, c0:c0 + nt], x[:, 0:nt], reads=[bx], writes=[Bdst])
    groups = [dict(cols=[(c, 256)], mode="fm", epi=epi_f) for c in range(0, D, 256)]
    stage_gemm(C, L, S["mT"][0], S["mT"][1], [(0, D)], w_out, tiles, groups, setup_f)


def build_F(cfg):
    nc = bass.Bass("TRN2", target_bir_lowering=False)
    C = Ctx(nc, cfg)
    NQ, TL, T, D, Lyr, KC, H = cfg.NQ, cfg.TL, cfg.T, cfg.D, cfg.DEPTH, cfg.KC, cfg.H
    NCL = TL // 128
    NV = 2 + 2 * NCL + 10
    NCT = cfg.CW // 128
    d = lambda name, shape, dt=F32: nc.dram_tensor(name, list(shape), dt, kind="ExternalInput").ap()
    ident = d("ident", [128, 128], BF16)
    cv = d("cv", [128, KC, 2]); w_ada = d("w_ada", [Lyr, D, 3 * D]); b_ada_fm = d("b_ada_fm", [128, Lyr, 3 * KC])
    norm_g_all = d("norm_g", [Lyr, 128, KC]); decay_all = d("decay", [Lyr, 128, 2 * H]); na_gain_all = d("na_gain", [Lyr, 128, 2])
    rope_all = d("rope", [NQ, 2, 128, TL]); rconst = d("rconst", [6, 128, 128]); rvec_all = d("rvec", [NQ, 128, NV])
    na_bias_all = d("na_bias", [Lyr, NQ, 5, H, 6, 128, 128]); cv_par_all = d("cv_par", [Lyr, 128, NCT, CONV_K + 3])
    w_in = d("w_in", [Lyr, D, cfg.N_IN]); w_br = d("w_br", [Lyr, cfg.KBR, D]); w_out = d("w_out", [Lyr, D, D])
    x_in = d("xT", [D, cfg.SEQ]); c_in = d("cT", [D, cfg.CTX])
    xo = nc.dram_tensor("xoT", [D, cfg.SEQ], F32, kind="ExternalOutput").ap()
    xa, Bxa = C.dram("x_a", [D, cfg.SEQ], F32); xb, Bxb = C.dram("x_b", [D, cfg.SEQ], F32)
    ca, Bca = C.dram("c_a", [D, cfg.CTX], F32); cb, Bcb = C.dram("c_b", [D, cfg.CTX], F32)
    Fst, BFst = C.dram("Fst", [NQ, 2, H, 128, 256], F32)
    with contextlib.ExitStack() as st:
        L = setup_global(C, st, {"ident": ident})
        stage_ada_fused(C, L, cv, w_ada, b_ada_fm)
        Ss = []
        for q in range(NQ):
            S = {}
            for nm, shp, dt in (("hxT", [D, T], BF16), ("qT", [cfg.RQK, T], BF16), ("kT", [cfg.RQK, T], BF16), ("vtm", [T, cfg.RV], BF16),
                                ("rgT", [cfg.RV, T], BF16), ("nqT", [cfg.NAW, T], BF16), ("nkT", [cfg.NAW, T], BF16), ("nvtm", [T, cfg.NAW], BF16),
                                ("ngT", [cfg.NAW, T], BF16), ("uT", [cfg.CW, T], F32), ("cgT", [cfg.CW, T], BF16), ("sgT", [3 * D, T], BF16),
                                ("ogT", [cfg.KBR, T], BF16), ("mT", [D, T], BF16)):
                S[nm] = C.dram(f"{nm}_{q}", shp, dt)
            Ss.append(S)
        cur_x, Bcx = x_in, Buf("x_in")
        cur_c, Bcc = c_in, Buf("c_in")
        Bxo = Buf("xo")
        all_groups = {"ret_q", "ret_k", "ret_v", "na_q", "na_k", "na_v", "cv_ab", "silu", "gates"}
        for l in range(Lyr):
            last = (l == Lyr - 1)
            setup_layer(C, L, l, norm_g_all[l], decay_all[l])
            if last:
                nxt_x, Bnx = xo, Bxo
            else:
                nxt_x, Bnx = (xa, Bxa) if l % 2 == 0 else (xb, Bxb)
            nxt_c, Bnc = (ca, Bca) if l % 2 == 0 else (cb, Bcb)
            for q in range(NQ):
                S = Ss[q]
                insq = {"rope": rope_all[q], "na_gain": na_gain_all[l], "rconst": rconst, "rvec": rvec_all[q]}
                stage_norm(C, L, cur_x[:, q * TL:(q + 1) * TL], Bcx, cur_c, Bcc, S["hxT"][0], S["hxT"][1])
                groups, setup = inproj_groups(C, L, S, insq, all_groups)
                stage_gemm(C, L, S["hxT"][0], S["hxT"][1], [(0, D)], w_in[l], tok_tiles(cfg, True), groups, setup)
                stage_ret_passA(C, L, S, insq, Fst[q], BFst)
            for q in range(NQ):
                S = Ss[q]
                hal = {}
                if q > 0:
                    Pv = Ss[q - 1]
                    hal["kb"] = (Pv["nkT"][0][:, TL - 256:TL], Pv["nkT"][1]); hal["vb"] = (Pv["nvtm"][0][TL - 256:TL, :], Pv["nvtm"][1])
                    hal["ub"] = (Pv["uT"][0][:, TL - 15:TL], Pv["uT"][1])
                else:
                    hal["kb"] = hal["vb"] = hal["ub"] = None
                if q < NQ - 1:
                    Nx = Ss[q + 1]
                    hal["ka"] = (Nx["nkT"][0][:, 0:256], Nx["nkT"][1]); hal["va"] = (Nx["nvtm"][0][0:256, :], Nx["nvtm"][1])
                    hal["ua"] = (Nx["uT"][0][:, 0:15], Nx["uT"][1])
                else:
                    hal["ka"] = hal["va"] = hal["ua"] = None
                insq = {"rope": rope_all[q], "na_gain": na_gain_all[l], "rconst": rconst, "rvec": rvec_all[q],
                        "na_bias": na_bias_all[l, q], "cv_par": cv_par_all[l], "halo": hal}
                stage_ret(C, L, S, insq, Fst, BFst)
                stage_na(C, L, S, insq)
                stage_conv(C, L, S, insq)
                tiles = tok_tiles(cfg, with_ctx=(q == 0 and not last))
                stage_merge(C, L, S, w_br[l], tiles)
                stage_final(C, L, S, w_out[l], tiles, cur_x[:, q * TL:(q + 1) * TL], Bcx, cur_c, Bcc,
                            nxt_x[:, q * TL:(q + 1) * TL], Bnx, nxt_c, Bnc)
            cur_x, Bcx = nxt_x, Bnx
            if not last:
                cur_c, Bcc = nxt_c, Bnc
        C.P.emit()
    return nc


def run_fused(cfg, x, c, ctx, c_ctx, w_ada, b_ada, norm_g, w_in, ret_decay_f, ret_decay_b, w_ret_o,
              na_q_gain, na_k_gain, na_rpb, w_na_o, cv_dw, cv_db, cv_ln_g, cv_ln_b, w_cv_o, w_out):
    f32 = np.float32
    D, KC, H, TL, NQ, Lyr = cfg.D, cfg.KC, cfg.H, cfg.TL, cfg.NQ, cfg.DEPTH
    NCT = cfg.CW // 128
    A = lambda v: np.asarray(v, f32)
    nc_F = _get("F", build_F, cfg)
    shared = {
        "ident": np.eye(128, dtype=f32).astype(NPBF),
        "w_ada": np.ascontiguousarray(A(w_ada)[:Lyr]),
        "b_ada_fm": np.ascontiguousarray(A(b_ada)[:Lyr].reshape(Lyr, 3 * KC, 128).transpose(2, 0, 1)),
        "norm_g": np.ascontiguousarray(A(norm_g)[:Lyr].reshape(Lyr, KC, 128).transpose(0, 2, 1)),
        "decay": np.ascontiguousarray(np.broadcast_to(np.concatenate([A(ret_decay_f)[:Lyr], A(ret_decay_b)[:Lyr]], -1)[:, None, :], (Lyr, 128, 2 * H))),
        "na_gain": np.ascontiguousarray(np.stack([A(na_q_gain)[:Lyr], A(na_k_gain)[:Lyr]], -1)),
        "rope": np.stack([rope_tables(cfg, q) for q in range(NQ)]),
        "rconst": ret_consts(cfg, 0)[0],
        "rvec": np.stack([ret_consts(cfg, q)[1] for q in range(NQ)]),
        "na_bias": np.stack([np.stack([na_bias_table(cfg, q, A(na_rpb)[l]) for q in range(NQ)]) for l in range(Lyr)]),
        "w_in": np.ascontiguousarray(A(w_in)[:Lyr]),
        "w_br": np.ascontiguousarray(np.concatenate([A(w_ret_o)[:Lyr], A(w_na_o)[:Lyr], A(w_cv_o)[:Lyr]], 1)),
        "w_out": np.ascontiguousarray(A(w_out)[:Lyr]),
    }
    cvp = np.concatenate([A(cv_dw)[:Lyr].transpose(0, 2, 1), A(cv_db)[:Lyr][:, :, None], A(cv_ln_g)[:Lyr][:, :, None], A(cv_ln_b)[:Lyr][:, :, None]], axis=2)
    shared["cv_par"] = np.ascontiguousarray(cvp.reshape(Lyr, NCT, 128, CONV_K + 3).transpose(0, 2, 1, 3))
    in_maps = []
    for b in range(cfg.B):
        cvec = np.stack([A(c)[b], A(c_ctx)], -1)
        m = dict(shared)
        m["cv"] = np.ascontiguousarray(cvec.reshape(KC, 128, 2).transpose(1, 0, 2))
        m["xT"] = np.ascontiguousarray(A(x)[b].T)
        m["cT"] = np.ascontiguousarray(A(ctx)[b].T)
        in_maps.append(m)
    res = run_bass_kernel_spmd(nc_F, in_maps, core_ids=list(range(cfg.B))).results
    return np.stack([np.ascontiguousarray(res[b]["xoT"].T) for b in range(cfg.B)])


MODE = "unfused"


def kernel(**inputs):
    cfg = Cfg()
    if MODE == "fused":
        return run_fused(cfg, **inputs)
    return run_model(cfg, **inputs)
```

```python
import contextlib
import numpy as np
import ml_dtypes
import concourse.bass as bass
import concourse.mybir as mybir
from concourse.bass_utils import run_bass_kernel_spmd

F32 = mybir.dt.float32
BF16 = mybir.dt.bfloat16
AF = mybir.ActivationFunctionType
ALU = mybir.AluOpType
NPBF = ml_dtypes.bfloat16

GRID_W = 64
EPS = 1e-6
CONV_K = 31
BIGD = 1.0e7
NEG = -30000.0


class Cfg:
    def __init__(self, D=4096, H=8, SEQ=8192, CTX=256, DEPTH=4, B=2, NQ=4):
        self.D, self.H, self.SEQ, self.CTX, self.DEPTH, self.B = D, H, SEQ, CTX, DEPTH, B
        self.NQ = NQ
        self.TL = SEQ // self.NQ
        self.T = self.TL + CTX
        self.KC = D // 128
        self.RQK = H * 128
        self.RV = H * 256
        self.NAW = H * 128
        self.CW = H * 128
        self.NP = self.TL // 128
        self.ROWS = SEQ // GRID_W
        self.LR = self.TL // GRID_W
        o = 0
        self.off = {}
        for name, n in (("ret_q", self.RQK), ("ret_k", self.RQK), ("ret_v", self.RV), ("ret_g", self.RV),
                        ("na_q", self.NAW), ("na_k", self.NAW), ("na_v", self.NAW), ("na_g", self.NAW),
                        ("cv_a", self.CW), ("cv_b", self.CW), ("cv_g", self.CW),
                        ("gate_ret", D), ("gate_na", D), ("gate_cv", D)):
            self.off[name] = (o, n)
            o += n
        self.N_IN = o
        self.KBR = self.RV + self.NAW + self.CW


class Buf:
    __slots__ = ("name", "w", "r")

    def __init__(self, name=""):
        self.name = name
        self.w = None
        self.r = []


class Prog:
    ENG = ("pe", "act", "dve", "pool", "sp")
    NDMA = 24

    def __init__(self, nc):
        self.nc = nc
        self.ops = {e: [] for e in self.ENG}
        self.cnt = {}
        self.known = {e: {} for e in self.ENG}
        self.dma_rr = {e: 0 for e in self.ENG}
        self.n_ops = 0

    def _need(self, eng, ticket, waits):
        if ticket is None:
            return
        k, v = ticket
        if k == ("e", eng) and eng == "pe":
            return
        if self.known[eng].get(k, 0) >= v:
            return
        if waits.get(k, 0) < v:
            waits[k] = v

    def _deps(self, eng, reads, writes):
        waits = {}
        for b in reads:
            self._need(eng, b.w, waits)
        for b in writes:
            self._need(eng, b.w, waits)
            for t in b.r:
                self._need(eng, t, waits)
        return waits

    def _commit(self, eng, waits):
        for k, v in waits.items():
            self.known[eng][k] = v
        return list(waits.items())

    def _mark(self, ticket, reads, writes):
        for b in reads:
            if b not in writes:
                b.r.append(ticket)
                if len(b.r) > 48:
                    b.r = b.r[-48:]
        for b in writes:
            b.w = ticket
            b.r = []

    def op(self, eng, fn, reads=(), writes=()):
        reads = list(reads); writes = list(writes)
        waits = self._commit(eng, self._deps(eng, reads, writes))
        k = ("e", eng)
        v = self.cnt.get(k, 0) + 1
        self.cnt[k] = v
        self.ops[eng].append((waits, fn, k, 1))
        self._mark((k, v), reads, writes)
        self.n_ops += 1

    def dma(self, q, out, in_, reads=(), writes=(), **kw):
        reads = list(reads); writes = list(writes)
        waits = self._deps(q, reads, writes)
        i = self.dma_rr[q]
        self.dma_rr[q] = (i + 1) % self.NDMA
        k = ("d", q, i)
        prev = self.cnt.get(k, 0)
        if prev > 0 and self.known[q].get(k, 0) < prev:
            waits[k] = max(waits.get(k, 0), prev)
        waits = self._commit(q, waits)
        self.cnt[k] = prev + 16

        def fn(e, out=out, in_=in_, kw=kw):
            return e.dma_start(out=out, in_=in_, **kw)
        self.ops[q].append((waits, fn, k, 16))
        self._mark((k, prev + 16), reads, writes)
        self.n_ops += 1

    def barrier(self):
        for eng in self.ENG:
            waits = {}
            for k, v in self.cnt.items():
                if k == ("e", eng):
                    continue
                if self.known[eng].get(k, 0) < v:
                    waits[k] = v
            waits = self._commit(eng, waits)
            if waits:
                self.ops[eng].append((waits, None, None, 0))

    def emit(self):
        nc = self.nc
        self.barrier()
        keys = sorted(self.cnt.keys(), key=str)
        with contextlib.ExitStack() as st:
            sems = {}
            for k in keys:
                sems[k] = st.enter_context(nc.semaphore("s_" + "_".join(str(x) for x in k)))
            block = st.enter_context(nc.Block())

            def run(name, e):
                for waits, fn, k, inc in self.ops[name]:
                    for wk, wv in waits:
                        e.wait_ge(sems[wk], wv)
                    if fn is not None:
                        fn(e).then_inc(sems[k], inc)

            @block.tensor
            def _(e):
                run("pe", e)

            @block.scalar
            def _(e):
                run("act", e)

            @block.vector
            def _(e):
                run("dve", e)

            @block.gpsimd
            def _(e):
                run("pool", e)

            @block.sync
            def _(e):
                run("sp", e)


DBG_NAMES = ("hxT", "ogT", "mT", "qT", "kT", "nqT", "nkT")
DBGOUT = {}


class Ctx:
    def __init__(self, nc, cfg):
        self.nc = nc
        self.cfg = cfg
        self.P = Prog(nc)
        self.uid = 0
        self.bufs = {}

    def name(self, s):
        self.uid += 1
        return f"{s}_{self.uid}"

    def sb(self, st, name, shape, dt):
        t = st.enter_context(self.nc.sbuf_tensor(self.name(name), list(shape), dt))
        return t, Buf(name)

    def dram(self, name, shape, dt, kind="Internal"):
        if getattr(self.cfg, "debug", False) and name in DBG_NAMES:
            kind = "ExternalOutput"
        t = self.nc.dram_tensor(name, list(shape), dt, kind=kind).ap()
        b = Buf(name)
        self.bufs[name] = b
        return t, b


def bc(ap, shape, axis):
    return ap.unsqueeze(axis).broadcast_to(list(shape))


class Layer:
    pass


def setup_common(C, st, ins):
    cfg, P, nc = C.cfg, C.P, C.nc
    L = Layer()
    KC, H = cfg.KC, cfg.H
    L.ps = []
    for i in range(7):
        t = st.enter_context(nc.psum_tensor(C.name("ps"), [128, 512], F32))
        L.ps.append((t, Buf(f"ps{i}")))
    t = st.enter_context(nc.psum_tensor(C.name("psb"), [128, 1024], BF16))
    L.psb = (t, Buf("psb"))
    L.ones_bf, b1 = C.sb(st, "ones_bf", [128, 128], BF16)
    L.ones_f, b2 = C.sb(st, "ones_f", [128, 128], F32)
    L.ident, b3 = C.sb(st, "ident", [128, 128], BF16)
    L.Bconst = Buf("const")
    P.op("pool", lambda e: e.memset(L.ones_bf[:], 1.0), writes=[L.Bconst])
    P.op("pool", lambda e: e.memset(L.ones_f[:], 1.0), writes=[L.Bconst])
    P.dma("sp", L.ident[:], ins["ident"][:, :], writes=[L.Bconst])
    L.mods, _ = C.sb(st, "mods", [128, KC, 6], F32)
    L.ng, _ = C.sb(st, "ng", [128, KC], F32)
    L.G, _ = C.sb(st, "G", [128, 2, KC], F32)
    L.SH, _ = C.sb(st, "SH", [128, 2, KC], F32)
    L.GT, _ = C.sb(st, "GT", [128, 2, KC], F32)
    L.Bpar = Buf("par")
    P.dma("sp", L.mods[:], ins["mods"][:, :, :], writes=[L.Bpar])
    P.dma("sp", L.ng[:], ins["norm_g"][:, :], writes=[L.Bpar])
    for s in range(2):
        P.op("dve", lambda e, s=s: e.scalar_tensor_tensor(out=L.G[:, s, :], in0=L.mods[:, :, 3 * s + 1], scalar=1.0,
                                                          in1=L.ng[:], op0=ALU.add, op1=ALU.mult),
             reads=[L.Bpar], writes=[L.Bpar])
        P.op("dve", lambda e, s=s: e.tensor_copy(out=L.SH[:, s, :], in_=L.mods[:, :, 3 * s]), reads=[L.Bpar], writes=[L.Bpar])
        P.op("dve", lambda e, s=s: e.tensor_copy(out=L.GT[:, s, :], in_=L.mods[:, :, 3 * s + 2]), reads=[L.Bpar], writes=[L.Bpar])
    L.lg, _ = C.sb(st, "lg", [128, 2 * H], F32)
    tmp, _ = C.sb(st, "lgt", [128, 2 * H], F32)
    tmp2, _ = C.sb(st, "lgt2", [128, 2 * H], F32)
    L.Blg = Buf("lg")
    P.dma("sp", L.lg[:], ins["decay"][:, :], writes=[L.Blg])
    P.op("act", lambda e: e.activation(out=tmp[:], in_=L.lg[:], func=AF.Exp, scale=-1.0), reads=[L.Blg], writes=[L.Blg])
    P.op("dve", lambda e: e.tensor_scalar(out=tmp2[:], in0=tmp[:], scalar1=0.2, scalar2=-0.25, op0=ALU.mult, op1=ALU.add),
         reads=[L.Blg], writes=[L.Blg])
    for cst in (1.0 / 3.0, -0.5, 1.0):
        P.op("dve", lambda e: e.tensor_tensor(out=tmp2[:], in0=tmp2[:], in1=tmp[:], op=ALU.mult), reads=[L.Blg], writes=[L.Blg])
        P.op("dve", lambda e, cst=cst: e.tensor_scalar(out=tmp2[:], in0=tmp2[:], scalar1=cst, scalar2=None, op0=ALU.add),
             reads=[L.Blg], writes=[L.Blg])
    P.op("dve", lambda e: e.tensor_tensor(out=tmp2[:], in0=tmp2[:], in1=tmp[:], op=ALU.mult), reads=[L.Blg], writes=[L.Blg])
    P.op("dve", lambda e: e.tensor_scalar(out=L.lg[:], in0=tmp2[:], scalar1=-1.0, scalar2=None, op0=ALU.mult),
         reads=[L.Blg], writes=[L.Blg])
    return L


def stage_norm(C, L, xT, Bx, cT, Bc, hxT, Bhx, do_ctx=True, do_lat=True):
    cfg, P, nc = C.cfg, C.P, C.nc
    KC, D = cfg.KC, cfg.D
    BLK = 256
    with contextlib.ExitStack() as st:
        xs = [C.sb(st, "nx", [128, KC, BLK], F32) for _ in range(2)]
        sq = [C.sb(st, "nsq", [128, KC, BLK], BF16) for _ in range(2)]
        ho = [C.sb(st, "nho", [128, KC, BLK], BF16) for _ in range(2)]
        rs = [C.sb(st, "nrs", [128, BLK], F32) for _ in range(2)]
        blocks = [(0, t0, xT, Bx, t0) for t0 in range(0, cfg.TL, BLK)] if do_lat else []
        if do_ctx:
            blocks += [(1, t0, cT, Bc, cfg.TL + t0) for t0 in range(0, cfg.CTX, BLK)]
        for bi, (s, t0, src, Bsrc, o0) in enumerate(blocks):
            (x, bx), (q, bq), (h, bh), (r, br) = xs[bi % 2], sq[bi % 2], ho[bi % 2], rs[bi % 2]
            ps, bps = L.ps[bi % 2]
            P.dma("sp", x[:], src.rearrange("(c p) t -> p c t", p=128)[:, :, t0:t0 + BLK], reads=[Bsrc], writes=[bx])
            P.op("act", lambda e, x=x, q=q: e.activation(out=q[:], in_=x[:], func=AF.Square), reads=[bx], writes=[bq])

            def mm(e, q=q, ps=ps):
                for kc in range(KC):
                    ins_ = e.matmul(ps[:, 0:BLK], lhsT=L.ones_bf[:], rhs=q[:, kc, :], start=(kc == 0), stop=(kc == KC - 1))
                return ins_
            P.op("pe", mm, reads=[bq, L.Bconst], writes=[bps])
            P.op("act", lambda e, r=r, ps=ps: e.activation(out=r[:], in_=ps[:, 0:BLK], func=AF.Sqrt, bias=EPS, scale=1.0 / D),
                 reads=[bps], writes=[br])
            P.op("dve", lambda e, r=r: e.reciprocal(out=r[:], in_=r[:]), reads=[br], writes=[br])
            P.op("dve", lambda e, x=x, r=r: e.tensor_tensor(out=x[:], in0=x[:], in1=bc(r[:], [128, KC, BLK], 1), op=ALU.mult),
                 reads=[bx, br], writes=[bx])
            def modul(e, x=x, h=h, s=s):
                for kc in range(KC):
                    ins_ = e.activation(out=h[:, kc, :], in_=x[:, kc, :], func=AF.Identity, scale=L.G[:, s, kc:kc + 1], bias=L.SH[:, s, kc:kc + 1])
                return ins_
            P.op("act", modul, reads=[bx, L.Bpar], writes=[bh])
            P.dma("sp", hxT.rearrange("(c p) t -> p c t", p=128)[:, :, o0:o0 + BLK], h[:], reads=[bh], writes=[Bhx])
        P.barrier()


def stage_gemm(C, L, aT, Ba, Ka_segs, w_ap, tiles, groups, epi_setup=None):
    cfg, P, nc = C.cfg, C.P, C.nc
    Ktot = sum(n for _, n in Ka_segs)
    KCt = Ktot // 128
    nseg = len(Ka_segs)
    GW = 256
    with contextlib.ExitStack() as st:
        maxT = max(sum(t1 - t0 for t0, t1 in tl) for tl in tiles)
        a_sb, Basb = C.sb(st, "ga", [128, KCt, maxT], BF16)
        NWB = 3
        wbs = [C.sb(st, "gw", [128, KCt, GW], BF16) for _ in range(NWB)]
        env = epi_setup(st) if epi_setup else None
        av = aT.rearrange("(c p) t -> p c t", p=128)
        wv = w_ap.rearrange("(c p) n -> p c n", p=128)
        wi = 0
        pi = 0
        npsum = 6 if nseg == 1 else 6
        for tl in tiles:
            o = 0
            suboff = []
            for (t0, t1) in tl:
                n = t1 - t0
                half = KCt // 2 if KCt >= 2 else KCt
                P.dma("sp", a_sb[:, 0:half, o:o + n], av[:, 0:half, t0:t1], reads=[Ba], writes=[Basb])
                if half < KCt:
                    P.dma("sp", a_sb[:, half:KCt, o:o + n], av[:, half:KCt, t0:t1], reads=[Ba], writes=[Basb])
                suboff.append(o)
                o += n
            for g in groups:
                wb, bwb = wbs[wi % NWB]
                wi += 1
                co = 0
                colmap = []
                for (c0, n) in g["cols"]:
                    P.dma("pool", wb[:, :, co:co + n], wv[:, :, c0:c0 + n], writes=[bwb])
                    colmap.append((c0, n, co))
                    co += n
                if g["mode"] == "fm":
                    for (c0, n, cof) in colmap:
                        for cj in range(n // 128):
                            col0 = c0 + cj * 128
                            wo = cof + cj * 128
                            for si, (t0, t1) in enumerate(tl):
                                nt = t1 - t0
                                pss = []
                                for sg in range(nseg):
                                    pss.append(L.ps[pi % npsum]); pi += 1
                                so = suboff[si]

                                def mm(e, pss=pss, wb=wb, wo=wo, so=so, nt=nt):
                                    ins_ = None
                                    for sg, (k0, nk) in enumerate(Ka_segs):
                                        kc0 = k0 // 128
                                        nkc = nk // 128
                                        for kk in range(nkc):
                                            ins_ = e.matmul(pss[sg][0][:, 0:nt], lhsT=wb[:, kc0 + kk, wo:wo + 128],
                                                            rhs=a_sb[:, kc0 + kk, so:so + nt],
                                                            start=(kk == 0), stop=(kk == nkc - 1))
                                    return ins_
                                P.op("pe", mm, reads=[bwb, Basb], writes=[b for _, b in pss])
                                g["epi"](env, col0, (t0, t1), pss)
                else:
                    ncols = co
                    for si, (t0, t1) in enumerate(tl):
                        for tk in range(t0, t1, 128):
                            ps, bps = L.ps[pi % npsum]; pi += 1
                            so = suboff[si] + (tk - t0)

                            def mm(e, ps=ps, wb=wb, so=so, ncols=ncols):
                                for kc in range(KCt):
                                    ins_ = e.matmul(ps[:, 0:ncols], lhsT=a_sb[:, kc, so:so + 128], rhs=wb[:, kc, 0:ncols],
                                                    start=(kc == 0), stop=(kc == KCt - 1))
                                return ins_
                            P.op("pe", mm, reads=[bwb, Basb], writes=[bps])
                            g["epi"](env, colmap, tk, ps, bps)
        P.barrier()


class Rot:
    def __init__(self, items):
        self.items = items
        self.i = 0

    def next(self):
        x = self.items[self.i % len(self.items)]
        self.i += 1
        return x


def inproj_groups(C, L, S, ins, which):
    cfg, P, nc = C.cfg, C.P, C.nc
    off = cfg.off
    TL = cfg.TL

    def setup(st):
        env = {}
        env["o_bf"] = Rot([C.sb(st, "eob", [128, 512], BF16) for _ in range(4)])
        env["o_f"] = Rot([C.sb(st, "eof", [128, 512], F32) for _ in range(3)])
        env["t_f"] = Rot([C.sb(st, "etf", [128, 512], F32) for _ in range(3)])
        env["t_f2"] = Rot([C.sb(st, "etg", [128, 512], F32) for _ in range(2)])
        env["sq"] = Rot([C.sb(st, "esq", [128, 512], BF16) for _ in range(2)])
        env["ahold"] = {}
        if "ret_q" in which or "ret_k" in which:
            env["rope"], env["Brope"] = C.sb(st, "rope", [128, 2, TL], F32)
            P.dma("sp", env["rope"][:], ins["rope"].rearrange("f p t -> p f t"), writes=[env["Brope"]])
        if "na_q" in which or "na_k" in which:
            env["gains"], env["Bgains"] = C.sb(st, "gains", [128, 2], F32)
            P.dma("sp", env["gains"][:], ins["na_gain"][:, :], writes=[env["Bgains"]])
            P.op("dve", lambda e: e.tensor_scalar(out=env["gains"][:, 0:1], in0=env["gains"][:, 0:1], scalar1=128.0 ** -0.5,
                                                  scalar2=None, op0=ALU.mult), reads=[env["Bgains"]], writes=[env["Bgains"]])
        return env

    def store(env, dst, Bdst, row0, sub, tile, btile, nt):
        P.dma("sp", dst[row0:row0 + 128, sub[0]:sub[1]], tile[:, 0:nt], reads=[btile], writes=[Bdst])

    def epi_act(func, dstname, base):
        dst, Bdst = S[dstname]

        def f(env, col0, sub, pss):
            ps, bps = pss[0]
            nt = sub[1] - sub[0]
            o, bo = env["o_bf"].next()
            P.op("act", lambda e: e.activation(out=o[:, 0:nt], in_=ps[:, 0:nt], func=func), reads=[bps], writes=[bo])
            store(env, dst, Bdst, col0 - base, sub, o, bo, nt)
        return f

    def epi_rope(dstname, base, tab):
        dst, Bdst = S[dstname]

        def f(env, col0, sub, pss):
            ps, bps = pss[0]
            nt = sub[1] - sub[0]
            o, bo = env["o_bf"].next()
            sc = 1.0 if tab == 0 else 128.0 ** -0.5
            if sub[0] >= TL:
                P.op("act", lambda e: e.activation(out=o[:, 0:nt], in_=ps[:, 0:nt], func=AF.Copy, scale=sc), reads=[bps], writes=[bo])
            else:
                x, bx = env["t_f"].next()
                t1, bt1 = env["o_f"].next()
                t2, bt2 = env["t_f2"].next()
                rp = env["rope"]
                P.op("act", lambda e: e.activation(out=x[:, 0:nt], in_=ps[:, 0:nt], func=AF.Copy, scale=sc), reads=[bps], writes=[bx])
                P.op("dve", lambda e: e.tensor_tensor(out=t1[:, 0:nt], in0=x[:, 0:nt], in1=rp[:, 0, sub[0]:sub[1]], op=ALU.mult),
                     reads=[bx, env["Brope"]], writes=[bt1])
                P.op("dve", lambda e: e.tensor_tensor(out=t2[0:64, 0:nt], in0=x[64:128, 0:nt], in1=rp[64:128, 1, sub[0]:sub[1]], op=ALU.mult),
                     reads=[bx, env["Brope"]], writes=[bt2])
                P.op("dve", lambda e: e.tensor_tensor(out=t2[64:128, 0:nt], in0=x[0:64, 0:nt], in1=rp[0:64, 1, sub[0]:sub[1]], op=ALU.mult),
                     reads=[bx, env["Brope"]], writes=[bt2])
                P.op("dve", lambda e: e.tensor_tensor(out=o[:, 0:nt], in0=t1[:, 0:nt], in1=t2[:, 0:nt], op=ALU.add),
                     reads=[bt1, bt2], writes=[bo])
            store(env, dst, Bdst, col0 - base, sub, o, bo, nt)
        return f

    def epi_norm(dstname, base, gi):
        dst, Bdst = S[dstname]

        def f(env, col0, sub, pss):
            ps, bps = pss[0]
            nt = sub[1] - sub[0]
            o, bo = env["o_bf"].next()
            q, bq = env["sq"].next()
            r, br = env["t_f"].next()
            ps2, bps2 = L.ps[6]
            P.op("act", lambda e: e.activation(out=q[:, 0:nt], in_=ps[:, 0:nt], func=AF.Square), reads=[bps], writes=[bq])
            P.op("pe", lambda e: e.matmul(ps2[:, 0:nt], lhsT=L.ones_bf[:], rhs=q[:, 0:nt], start=True, stop=True),
                 reads=[bq, L.Bconst], writes=[bps2])
            P.op("act", lambda e: e.activation(out=r[:, 0:nt], in_=ps2[:, 0:nt], func=AF.Sqrt, bias=EPS, scale=1.0 / 128), reads=[bps2], writes=[br])
            P.op("dve", lambda e: e.reciprocal(out=r[:, 0:nt], in_=r[:, 0:nt]), reads=[br], writes=[br])
            P.op("dve", lambda e: e.scalar_tensor_tensor(out=o[:, 0:nt], in0=ps[:, 0:nt], scalar=env["gains"][:, gi:gi + 1], in1=r[:, 0:nt],
                                                         op0=ALU.mult, op1=ALU.mult), reads=[bps, br, env["Bgains"]], writes=[bo])
            store(env, dst, Bdst, col0 - base, sub, o, bo, nt)
        return f

    def epi_conv_ab():
        dst, Bdst = S["uT"]
        a0 = off["cv_a"][0]
        b0 = off["cv_b"][0]

        def f(env, col0, sub, pss):
            ps, bps = pss[0]
            nt = sub[1] - sub[0]
            if col0 < b0:
                key = (col0 - a0, sub)
                x, bx = env["o_f"].next()
                env["ahold"][key] = (x, bx)
                P.op("act", lambda e: e.activation(out=x[:, 0:nt], in_=ps[:, 0:nt], func=AF.Copy), reads=[bps], writes=[bx])
            else:
                x, bx = env["ahold"].pop((col0 - b0, sub))
                sg, bsg = env["t_f"].next()
                P.op("act", lambda e: e.activation(out=sg[:, 0:nt], in_=ps[:, 0:nt], func=AF.Sigmoid), reads=[bps], writes=[bsg])
                P.op("dve", lambda e: e.tensor_tensor(out=sg[:, 0:nt], in0=sg[:, 0:nt], in1=x[:, 0:nt], op=ALU.mult),
                     reads=[bsg, bx], writes=[bsg])
                P.dma("sp", dst[col0 - b0:col0 - b0 + 128, sub[0]:sub[1]], sg[:, 0:nt], reads=[bsg], writes=[Bdst])
        return f

    def epi_tm(dstname, base):
        dst, Bdst = S[dstname]

        def f(env, colmap, tk, ps, bps):
            for (c0, n, cof) in colmap:
                o, bo = env["o_bf"].next()
                P.op("act", lambda e, o=o, cof=cof, n=n: e.activation(out=o[:, 0:n], in_=ps[:, cof:cof + n], func=AF.Copy), reads=[bps], writes=[bo])
                P.dma("sp", dst[tk:tk + 128, c0 - base:c0 - base + n], o[:, 0:n], reads=[bo], writes=[Bdst])
        return f

    groups = []

    def add_fm(name, epi):
        c0, n = off[name]
        for c in range(c0, c0 + n, 256):
            groups.append(dict(cols=[(c, min(256, c0 + n - c))], mode="fm", epi=epi))

    def add_tm(name, epi):
        c0, n = off[name]
        for c in range(c0, c0 + n, 256):
            groups.append(dict(cols=[(c, min(256, c0 + n - c))], mode="tm", epi=epi))

    if "ret_q" in which:
        add_fm("ret_q", epi_rope("qT", off["ret_q"][0], 0))
    if "ret_k" in which:
        add_fm("ret_k", epi_rope("kT", off["ret_k"][0], 2))
    if "ret_v" in which:
        add_tm("ret_v", epi_tm("vtm", off["ret_v"][0]))
    if "na_q" in which:
        add_fm("na_q", epi_norm("nqT", off["na_q"][0], 0))
    if "na_k" in which:
        add_fm("na_k", epi_norm("nkT", off["na_k"][0], 1))
    if "na_v" in which:
        add_tm("na_v", epi_tm("nvtm", off["na_v"][0]))
    if "cv_ab" in which:
        e = epi_conv_ab()
        for i in range(cfg.CW // 128):
            groups.append(dict(cols=[(off["cv_a"][0] + i * 128, 128), (off["cv_b"][0] + i * 128, 128)], mode="fm", epi=e))
    if "silu" in which:
        add_fm("ret_g", epi_act(AF.Silu, "rgT", off["ret_g"][0]))
        add_fm("na_g", epi_act(AF.Silu, "ngT", off["na_g"][0]))
        add_fm("cv_g", epi_act(AF.Silu, "cgT", off["cv_g"][0]))
    if "gates" in which:
        g0 = off["gate_ret"][0]
        for nm in ("gate_ret", "gate_na", "gate_cv"):
            add_fm(nm, epi_act(AF.Sigmoid, "sgT", g0))
    return groups, setup


def ret_tables(C, L, st, ins):
    cfg, P = C.cfg, C.P
    H = cfg.H
    NCL = cfg.TL // 128
    R = {}
    R["B"] = Buf("rtab")
    cst, bcst = C.sb(st, "rcst", [128, 6, 128], F32)
    P.dma("sp", cst[:], ins["rconst"].rearrange("f p i -> p f i"), writes=[bcst])
    cv, bcv = C.sb(st, "rcv", [128, 2 + 2 * NCL + 10], F32)
    P.dma("sp", cv[:], ins["rvec"][:, :], writes=[bcv])
    R["mask"], _ = C.sb(st, "rmask", [128, H, 128], F32)
    R["qdf"], _ = C.sb(st, "rqdf", [128, H, 128], BF16)
    R["qdb"], _ = C.sb(st, "rqdb", [128, H, 128], BF16)
    R["vec"], _ = C.sb(st, "rvecs", [128, H, 2, 2 + 2 * NCL + 10], F32)
    R["gC"], _ = C.sb(st, "rgC", [128, H, 2], F32)
    tmpa, bta = C.sb(st, "rtmpa", [128, 128], F32)
    tmpb, btb = C.sb(st, "rtmpb", [128, 128], F32)
    c128, bc128 = C.sb(st, "rc128", [128, 1], F32)
    P.op("pool", lambda e: e.memset(c128[:], 128.0), writes=[bc128])
    for h in range(H):
        lf = L.lg[:, h:h + 1]
        lb = L.lg[:, H + h:H + h + 1]
        P.op("act", lambda e, lf=lf: e.activation(out=tmpa[:], in_=cst[:, 0, :], func=AF.Exp, scale=lf), reads=[bcst, L.Blg], writes=[bta])
        P.op("dve", lambda e: e.tensor_tensor(out=tmpa[:], in0=tmpa[:], in1=cst[:, 2, :], op=ALU.mult), reads=[bta, bcst], writes=[bta])
        P.op("act", lambda e, lb=lb: e.activation(out=tmpb[:], in_=cst[:, 1, :], func=AF.Exp, scale=lb), reads=[bcst, L.Blg], writes=[btb])
        P.op("dve", lambda e: e.tensor_tensor(out=tmpb[:], in0=tmpb[:], in1=cst[:, 3, :], op=ALU.mult), reads=[btb, bcst], writes=[btb])
        P.op("dve", lambda e, h=h: e.tensor_tensor(out=R["mask"][:, h, :], in0=tmpa[:], in1=tmpb[:], op=ALU.add), reads=[bta, btb], writes=[R["B"]])
        P.op("act", lambda e, h=h, lf=lf: e.activation(out=R["qdf"][:, h, :], in_=cst[:, 4, :], func=AF.Exp, scale=lf), reads=[bcst, L.Blg], writes=[R["B"]])
        P.op("act", lambda e, h=h, lb=lb: e.activation(out=R["qdb"][:, h, :], in_=cst[:, 5, :], func=AF.Exp, scale=lb), reads=[bcst, L.Blg], writes=[R["B"]])
        P.op("act", lambda e, h=h, lf=lf: e.activation(out=R["vec"][:, h, 0, :], in_=cv[:], func=AF.Exp, scale=lf), reads=[bcv, L.Blg], writes=[R["B"]])
        P.op("act", lambda e, h=h, lb=lb: e.activation(out=R["vec"][:, h, 1, :], in_=cv[:], func=AF.Exp, scale=lb), reads=[bcv, L.Blg], writes=[R["B"]])
        P.op("act", lambda e, h=h, lf=lf: e.activation(out=R["gC"][:, h, 0:1], in_=c128[:], func=AF.Exp, scale=lf), reads=[bc128, L.Blg], writes=[R["B"]])
        P.op("act", lambda e, h=h, lb=lb: e.activation(out=R["gC"][:, h, 1:2], in_=c128[:], func=AF.Exp, scale=lb), reads=[bc128, L.Blg], writes=[R["B"]])
    R["NCL"] = NCL
    return R


def k_tokmajor(C, L, kT_sb, bk, c0, nch, outs):
    P = C.P
    psb, bpsb = L.psb
    for g0 in range(0, nch, 4):
        n = min(4, nch - g0)

        def tr(e, g0=g0, n=n):
            for i in range(n):
                c = c0 + g0 + i
                ins_ = e.transpose(out=psb[:, i * 128:(i + 1) * 128], in_=kT_sb[:, c * 128:(c + 1) * 128], identity=L.ident[:])
            return ins_
        P.op("pe", tr, reads=[bk, L.Bconst], writes=[bpsb])
        for oi, (dst, bd, scf) in enumerate(outs):
            for i in range(n):
                c = c0 + g0 + i
                eng = "act" if (oi + i) % 2 == 0 else "dve"
                if eng == "act":
                    P.op("act", lambda e, dst=dst, c=c, i=i, scf=scf: e.activation(out=dst[:, c, :], in_=psb[:, i * 128:(i + 1) * 128], func=AF.Identity, scale=scf(c)),
                         reads=[bpsb], writes=[bd])
                else:
                    P.op("dve", lambda e, dst=dst, c=c, i=i, scf=scf: e.tensor_scalar(out=dst[:, c, :], in0=psb[:, i * 128:(i + 1) * 128], scalar1=scf(c), scalar2=None, op0=ALU.mult),
                         reads=[bpsb], writes=[bd])


def stage_ret_passA(C, L, S, ins, Fout, BFout):
    cfg, P = C.cfg, C.P
    H, TL = cfg.H, cfg.TL
    NCL = TL // 128
    with contextlib.ExitStack() as st:
        R = ret_tables(C, L, st, ins)
        kT, BkT = S["kT"]
        vtm, Bvtm = S["vtm"]
        ks = [C.sb(st, "pak", [128, TL], BF16) for _ in range(2)]
        vs = [C.sb(st, "pav", [128, NCL, 256], BF16) for _ in range(2)]
        kf = [C.sb(st, "pakf", [128, NCL, 128], BF16) for _ in range(2)]
        kb = [C.sb(st, "pakb", [128, NCL, 128], BF16) for _ in range(2)]
        so = [C.sb(st, "paso", [128, 2, 256], F32) for _ in range(2)]
        for h in range(H):
            (k, bk), (v, bv), (kfx, bkf), (kbx, bkb), (o, bo) = ks[h % 2], vs[h % 2], kf[h % 2], kb[h % 2], so[h % 2]
            P.dma("sp", k[:], kT[h * 128:(h + 1) * 128, 0:TL], reads=[BkT], writes=[bk])
            P.dma("sp", v[:], vtm[0:TL, h * 256:(h + 1) * 256].rearrange("(c p) v -> p c v", p=128), reads=[Bvtm], writes=[bv])
            k_tokmajor(C, L, k, bk, 0, NCL, [
                (kfx, bkf, lambda c, h=h: R["vec"][:, h, 0, 2 + c:3 + c]),
                (kbx, bkb, lambda c, h=h: R["vec"][:, h, 1, 2 + NCL + c:3 + NCL + c])])
            for d, (kx, bkx) in enumerate(((kfx, bkf), (kbx, bkb))):
                ps, bps = L.ps[(2 * h + d) % 4]

                def mm(e, kx=kx, v=v, ps=ps):
                    for c in range(NCL):
                        ins_ = e.matmul(ps[:, 0:256], lhsT=kx[:, c, :], rhs=v[:, c, :], start=(c == 0), stop=(c == NCL - 1))
                    return ins_
                P.op("pe", mm, reads=[bkx, bv, R["B"]], writes=[bps])
                P.op("act", lambda e, o=o, d=d, ps=ps: e.activation(out=o[:, d, :], in_=ps[:, 0:256], func=AF.Copy), reads=[bps], writes=[bo])
            P.dma("sp", Fout[:, h, :, :].rearrange("d p v -> p d v"), o[:], reads=[bo], writes=[BFout])
        P.barrier()


def stage_ret(C, L, S, ins, Sall, BSall):
    cfg, P = C.cfg, C.P
    H, TL, T = cfg.H, cfg.TL, cfg.T
    NCL = TL // 128
    NCC = cfg.CTX // 128
    NCH = NCL + NCC
    with contextlib.ExitStack() as st:
        R = ret_tables(C, L, st, ins)
        VD = 2 + 2 * NCL
        qT, BqT = S["qT"]; kT, BkT = S["kT"]; vtm, Bvtm = S["vtm"]; rgT, BrgT = S["rgT"]; ogT, BogT = S["ogT"]
        NB = 2
        qs = [C.sb(st, "rq", [128, T], BF16) for _ in range(NB)]
        ks = [C.sb(st, "rk", [128, T], BF16) for _ in range(NB)]
        vs = [C.sb(st, "rv", [128, NCH, 256], BF16) for _ in range(NB)]
        gs = [C.sb(st, "rg", [128, 2, T], BF16) for _ in range(NB)]
        qfs = [C.sb(st, "rqf", [128, T], BF16) for _ in range(1)] * NB
        qbs = [C.sb(st, "rqb", [128, T], BF16) for _ in range(1)] * NB
        kfs = [C.sb(st, "rkf", [128, NCH, 128], BF16) for _ in range(NB)]
        kbs = [C.sb(st, "rkb", [128, NCH, 128], BF16) for _ in range(NB)]
        sbin = [C.sb(st, "rsbin", [128, NCH, 256], BF16) for _ in range(1)] * NB
        ogs = [C.sb(st, "rog", [128, 2, T], BF16) for _ in range(1)] * NB
        oraw, boraw = C.sb(st, "roraw", [128, 2, T], F32)
        sqb, bsqb = C.sb(st, "rsqb", [128, 2, T], BF16)
        rsb, brsb = C.sb(st, "rrsb", [128, T], F32)
        sall, bsall = C.sb(st, "rsall", [128, 4, 2, 256], F32)
        sfP = [C.sb(st, "rsf", [128, 256], F32) for _ in range(2)]
        sbP = [C.sb(st, "rsb", [128, 256], F32) for _ in range(2)]
        sfbR = Rot([C.sb(st, "rsfb", [128, 256], BF16) for _ in range(3)])
        sctx, bsctx = C.sb(st, "rsctx", [128, 2, 256], F32)
        smr = Rot([C.sb(st, "rsm", [128, 128], BF16) for _ in range(3)])
        psr = Rot([L.ps[i] for i in range(6)])
        for h in range(H):
            i2 = h % NB
            (q, bq), (k, bk), (v, bv), (g, bg) = qs[i2], ks[i2], vs[i2], gs[i2]
            (qf, bqf), (qb, bqb), (kf, bkf), (kb, bkb) = qfs[i2], qbs[i2], kfs[i2], kbs[i2]
            (sbi, bsbi), (og, bog) = sbin[i2], ogs[i2]
            P.dma("sp", q[:], qT[h * 128:(h + 1) * 128, :], reads=[BqT], writes=[bq])
            P.dma("sp", k[:], kT[h * 128:(h + 1) * 128, :], reads=[BkT], writes=[bk])
            P.dma("sp", v[:], vtm[:, h * 256:(h + 1) * 256].rearrange("(c p) v -> p c v", p=128), reads=[Bvtm], writes=[bv])
            P.dma("sp", g[:], rgT[h * 256:(h + 1) * 256, :].rearrange("(a p) t -> p a t", p=128), reads=[BrgT], writes=[bg])
            P.dma("sp", sall[:], Sall[:, :, h, :, :].rearrange("r d p v -> p r d v"), reads=[BSall], writes=[bsall])
            P.op("dve", lambda e, q=q, qf=qf, h=h: e.tensor_tensor(out=qf[:].rearrange("p (c i) -> p c i", i=128), in0=q[:].rearrange("p (c i) -> p c i", i=128),
                                                                   in1=bc(R["qdf"][:, h, :], [128, NCH, 128], 1), op=ALU.mult), reads=[bq, R["B"]], writes=[bqf])
            P.op("pool", lambda e, q=q, qb=qb, h=h: e.tensor_tensor(out=qb[:].rearrange("p (c i) -> p c i", i=128), in0=q[:].rearrange("p (c i) -> p c i", i=128),
                                                                    in1=bc(R["qdb"][:, h, :], [128, NCH, 128], 1), op=ALU.mult), reads=[bq, R["B"]], writes=[bqb])
            k_tokmajor(C, L, k, bk, 0, NCH, [
                (kf, bkf, lambda c, h=h: R["vec"][:, h, 0, 0:1]),
                (kb, bkb, lambda c, h=h: R["vec"][:, h, 1, 1:2])])
            gCf = R["gC"][:, h, 0:1]
            gCb = R["gC"][:, h, 1:2]
            for seq in ("ctx", "lat"):
                cs = list(range(NCL, NCH)) if seq == "ctx" else list(range(0, NCL))
                (sf, bsf), (sbk, bsb) = sfP[0], sbP[0]
                if seq == "ctx":
                    P.op("pool", lambda e, sf=sf: e.memset(sf[:], 0.0), writes=[bsf])
                    P.op("pool", lambda e, sbk=sbk: e.memset(sbk[:], 0.0), writes=[bsb])
                else:
                    for d, (sx, bsx) in enumerate(((sf, bsf), (sbk, bsb))):
                        vo = VD + 5 * d
                        P.op("dve", lambda e, sx=sx, d=d, vo=vo, h=h: e.tensor_scalar(out=sx[:], in0=sctx[:, d, :], scalar1=R["vec"][:, h, d, vo + 4:vo + 5],
                                                                                      scalar2=None, op0=ALU.mult), reads=[bsctx, R["B"]], writes=[bsx])
                        for r in range(4):
                            P.op("dve", lambda e, sx=sx, d=d, r=r, vo=vo, h=h: e.scalar_tensor_tensor(out=sx[:], in0=sall[:, r, d, :], scalar=R["vec"][:, h, d, vo + r:vo + r + 1],
                                                                                                      in1=sx[:], op0=ALU.mult, op1=ALU.add), reads=[bsall, R["B"]], writes=[bsx])
                pp = 0
                for c in reversed(cs):
                    (sbk, bsb), (sbn, bsbn) = sbP[pp], sbP[1 - pp]
                    pp = 1 - pp
                    P.op("act", lambda e, c=c, sbi=sbi, sbk=sbk: e.activation(out=sbi[:, c, :], in_=sbk[:], func=AF.Copy), reads=[bsb], writes=[bsbi])
                    ps, bps = psr.next()
                    P.op("pe", lambda e, c=c, ps=ps, kb=kb, v=v: e.matmul(ps[:, 0:256], lhsT=kb[:, c, :], rhs=v[:, c, :], start=True, stop=True),
                         reads=[bkb, bv], writes=[bps])
                    P.op("dve", lambda e, ps=ps, gCb=gCb, sbk=sbk, sbn=sbn: e.scalar_tensor_tensor(out=sbn[:], in0=sbk[:], scalar=gCb, in1=ps[:, 0:256], op0=ALU.mult, op1=ALU.add),
                         reads=[bps, R["B"], bsb], writes=[bsbn])
                (sbk, bsb) = sbP[pp]
                if seq == "ctx":
                    P.op("act", lambda e, sbk=sbk: e.activation(out=sctx[:, 1, :], in_=sbk[:], func=AF.Copy), reads=[bsb], writes=[bsctx])
                pf = 0
                for c in cs:
                    tsl = slice(c * 128, (c + 1) * 128)
                    (sf, bsf), (sfn, bsfn) = sfP[pf], sfP[1 - pf]
                    pf = 1 - pf
                    sfb, bsfb = sfbR.next()
                    P.op("act", lambda e, sfb=sfb, sf=sf: e.activation(out=sfb[:], in_=sf[:], func=AF.Copy), reads=[bsf], writes=[bsfb])
                    ps, bps = psr.next()
                    P.op("pe", lambda e, ps=ps, k=k, q=q, tsl=tsl: e.matmul(ps[:, 0:128], lhsT=k[:, tsl], rhs=q[:, tsl], start=True, stop=True),
                         reads=[bk, bq], writes=[bps])
                    sm, bsm = smr.next()
                    P.op("dve", lambda e, ps=ps, sm=sm, h=h: e.tensor_tensor(out=sm[:], in0=ps[:, 0:128], in1=R["mask"][:, h, :], op=ALU.mult),
                         reads=[bps, R["B"]], writes=[bsm])
                    po, bpo = psr.next()

                    def mmo(e, po=po, v=v, sm=sm, qf=qf, qb=qb, sbi=sbi, c=c, tsl=tsl, sfb=sfb):
                        for hv in range(2):
                            vsl = slice(hv * 128, (hv + 1) * 128)
                            e.matmul(po[:, vsl], lhsT=v[:, c, vsl], rhs=sm[:], start=True, stop=False)
                            e.matmul(po[:, vsl], lhsT=sfb[:, vsl], rhs=qf[:, tsl], start=False, stop=False)
                            ins_ = e.matmul(po[:, vsl], lhsT=sbi[:, c, vsl], rhs=qb[:, tsl], start=False, stop=True)
                        return ins_
                    P.op("pe", mmo, reads=[bv, bsm, bsfb, bqf, bqb, bsbi], writes=[bpo])
                    pu, bpu = psr.next()
                    P.op("pe", lambda e, pu=pu, kf=kf, v=v, c=c: e.matmul(pu[:, 0:256], lhsT=kf[:, c, :], rhs=v[:, c, :], start=True, stop=True),
                         reads=[bkf, bv], writes=[bpu])
                    P.op("dve", lambda e, pu=pu, gCf=gCf, sf=sf, sfn=sfn: e.scalar_tensor_tensor(out=sfn[:], in0=sf[:], scalar=gCf, in1=pu[:, 0:256], op0=ALU.mult, op1=ALU.add),
                         reads=[bpu, R["B"], bsf], writes=[bsfn])
                    P.op("act", lambda e, po=po, tsl=tsl: e.activation(out=oraw[:, :, tsl], in_=po[:, 0:256].rearrange("p (a i) -> p a i", i=128), func=AF.Copy),
                         reads=[bpo], writes=[boraw])
                (sf, bsf) = sfP[pf]
                if seq == "ctx":
                    P.op("act", lambda e, sf=sf: e.activation(out=sctx[:, 0, :], in_=sf[:], func=AF.Copy), reads=[bsf], writes=[bsctx])
            P.op("act", lambda e: e.activation(out=sqb[:], in_=oraw[:], func=AF.Square), reads=[boraw], writes=[bsqb])
            for t0 in range(0, T, 512):
                n_ = min(512, T - t0)
                pr, bpr = psr.next()

                def mmr(e, pr=pr, t0=t0, n_=n_):
                    e.matmul(pr[:, 0:n_], lhsT=L.ones_bf[:], rhs=sqb[:, 0, t0:t0 + n_], start=True, stop=False)
                    return e.matmul(pr[:, 0:n_], lhsT=L.ones_bf[:], rhs=sqb[:, 1, t0:t0 + n_], start=False, stop=True)
                P.op("pe", mmr, reads=[bsqb, L.Bconst], writes=[bpr])
                P.op("act", lambda e, pr=pr, t0=t0, n_=n_: e.activation(out=rsb[:, t0:t0 + n_], in_=pr[:, 0:n_], func=AF.Sqrt, bias=EPS, scale=1.0 / 256),
                     reads=[bpr], writes=[brsb])
            P.op("dve", lambda e: e.reciprocal(out=rsb[:], in_=rsb[:]), reads=[brsb], writes=[brsb])
            P.op("dve", lambda e: e.tensor_tensor(out=oraw[:], in0=oraw[:], in1=bc(rsb[:], [128, 2, T], 1), op=ALU.mult), reads=[boraw, brsb], writes=[boraw])
            P.op("pool", lambda e, og=og, g=g: e.tensor_tensor(out=og[:], in0=oraw[:], in1=g[:], op=ALU.mult), reads=[boraw, bg], writes=[bog])
            P.dma("sp", ogT[h * 256:(h + 1) * 256, :].rearrange("(a p) t -> p a t", p=128), og[:], reads=[bog], writes=[BogT])
        P.barrier()


NA_SLOT_KEYS = (768, 640, 576, 576, 704)


def na_pair_info(NP, m):
    if m == 0:
        return 0, 0
    if m == 1:
        return 1, 1
    if m == NP - 1:
        return 4, m - 1
    if m == NP - 2:
        return 3, m
    return 2, m


def stage_na(C, L, S, ins):
    cfg, P = C.cfg, C.P
    H, TL, T, NP = cfg.H, cfg.TL, cfg.T, cfg.NP
    TE = TL + 512
    NCE = TE // 128
    NCC = cfg.CTX // 128
    with contextlib.ExitStack() as st:
        nqT, BnqT = S["nqT"]; nkT, BnkT = S["nkT"]; nvtm, Bnvtm = S["nvtm"]; ngT, BngT = S["ngT"]; ogT, BogT = S["ogT"]
        NB = 2
        qs = [C.sb(st, "nq", [128, T], BF16) for _ in range(NB)]
        ke = [C.sb(st, "nke", [128, TE + cfg.CTX], BF16) for _ in range(NB)]
        ve = [C.sb(st, "nve", [128, NCE + NCC, 128], BF16) for _ in range(NB)]
        gs = [C.sb(st, "ngs", [128, T], BF16) for _ in range(NB)]
        ogs = [C.sb(st, "nog", [128, T], BF16) for _ in range(NB)]
        bias = [C.sb(st, "nbias", [128, 5, 6, 128], F32) for _ in range(NB)]
        er = Rot([C.sb(st, "ne", [128, 4, 128], BF16) for _ in range(4)])
        tr = Rot([C.sb(st, "nt", [128, 4, 128], F32) for _ in range(4)])
        onums = [C.sb(st, "nonum", [128, T], F32) for _ in range(NB)]
        odens = [C.sb(st, "noden", [128, T], F32) for _ in range(NB)]
        pss = Rot([L.ps[i] for i in range(3)])
        pacc = Rot([(L.ps[3], L.ps[4]), (L.ps[5], L.ps[6])])
        for h in range(H):
            i2 = h % NB
            (q, bq), (k, bk), (v, bv), (g, bg), (og, bog), (bi, bbi) = qs[i2], ke[i2], ve[i2], gs[i2], ogs[i2], bias[i2]
            (onum, bonum), (oden, boden) = onums[i2], odens[i2]
            hs = slice(h * 128, (h + 1) * 128)
            P.dma("sp", q[:], nqT[hs, :], reads=[BnqT], writes=[bq])
            P.dma("sp", g[:], ngT[hs, :], reads=[BngT], writes=[bg])
            P.dma("sp", k[:, 256:256 + TL], nkT[hs, 0:TL], reads=[BnkT], writes=[bk])
            P.dma("sp", k[:, TE:TE + cfg.CTX], nkT[hs, TL:T], reads=[BnkT], writes=[bk])
            hal = ins.get("halo")
            if hal is None:
                P.dma("sp", k[:, 0:256], ins["nk_halo"][hs, 0:256], writes=[bk])
                P.dma("sp", k[:, 256 + TL:TE], ins["nk_halo"][hs, 256:512], writes=[bk])
            else:
                for key, dsl in (("kb", slice(0, 256)), ("ka", slice(256 + TL, TE))):
                    if hal[key] is None:
                        P.op("pool", lambda e, k=k, dsl=dsl: e.memset(k[:, dsl], 0.0), writes=[bk])
                    else:
                        P.dma("sp", k[:, dsl], hal[key][0][hs, :], reads=[hal[key][1]], writes=[bk])
            P.dma("sp", v[:, 2:2 + TL // 128, :], nvtm[0:TL, hs].rearrange("(c p) d -> p c d", p=128), reads=[Bnvtm], writes=[bv])
            P.dma("sp", v[:, NCE:NCE + NCC, :], nvtm[TL:T, hs].rearrange("(c p) d -> p c d", p=128), reads=[Bnvtm], writes=[bv])
            if hal is None:
                P.dma("sp", v[:, 0:2, :], ins["nv_halo"][0:256, hs].rearrange("(c p) d -> p c d", p=128), writes=[bv])
                P.dma("sp", v[:, 2 + TL // 128:NCE, :], ins["nv_halo"][256:512, hs].rearrange("(c p) d -> p c d", p=128), writes=[bv])
            else:
                for key, c0_ in (("vb", 0), ("va", 2 + TL // 128)):
                    if hal[key] is None:
                        P.op("pool", lambda e, v=v, c0_=c0_: e.memset(v[:, c0_:c0_ + 2, :], 0.0), writes=[bv])
                    else:
                        P.dma("sp", v[:, c0_:c0_ + 2, :], hal[key][0][:, hs].rearrange("(c p) d -> p c d", p=128), reads=[hal[key][1]], writes=[bv])
            for s_ in range(5):
                P.dma("sp", bi[:, s_, :, :], ins["na_bias"][s_, h, :, :, :].rearrange("c p q -> p c q"), writes=[bbi])
            units = [("lat", m) for m in range(NP)] + [("ctx", j) for j in range(NCC)]
            for kind, m in units:
                if kind == "lat":
                    qsl = slice(m * 128, (m + 1) * 128)
                    slot, sc = na_pair_info(NP, m)
                    nk = NA_SLOT_KEYS[slot]
                    chunks = []
                    for j in range((nk + 127) // 128):
                        n = min(128, nk - j * 128)
                        chunks.append(((sc + j) * 128, sc + j, n, j))
                else:
                    qsl = slice(TL + m * 128, TL + (m + 1) * 128)
                    chunks = []
                for j in range(NCC):
                    chunks.append((TE + j * 128, NCE + j, 128, None))
                (pn, bpn), (pd, bpd) = pacc.next()
                nchunks = len(chunks)
                gps = []
                for b0 in range(0, nchunks, 4):
                    grp = chunks[b0:b0 + 4]
                    ps, bps = pss.next()

                    def mms(e, ps=ps, grp=grp, k=k, q=q, qsl=qsl):
                        for i, (kc0, vc, n, bj) in enumerate(grp):
                            ins_ = e.matmul(ps[0:n, i * 128:(i + 1) * 128], lhsT=k[:, kc0:kc0 + n], rhs=q[:, qsl], start=True, stop=True)
                        return ins_
                    P.op("pe", mms, reads=[bk, bq], writes=[bps])
                    gps.append((grp, ps, bps))
                gex = []
                for grp, ps, bps in gps:
                    ex, bex = er.next()
                    i = 0
                    while i < len(grp):
                        j = i
                        while j + 1 < len(grp) and (grp[j + 1][3] is None) == (grp[i][3] is None) and grp[j + 1][2] == grp[i][2]:
                            j += 1
                        n = grp[i][2]
                        cnt = j - i + 1
                        psv = ps[0:n, i * 128:(j + 1) * 128].rearrange("p (c q) -> p c q", q=128)
                        if grp[i][3] is not None:
                            tt, btt = tr.next()
                            bj0 = grp[i][3]
                            P.op("dve", lambda e, tt=tt, psv=psv, n=n, cnt=cnt, bj0=bj0, slot=slot, bi=bi: e.tensor_tensor(
                                out=tt[0:n, 0:cnt, :], in0=psv, in1=bi[0:n, slot, bj0:bj0 + cnt, :], op=ALU.add), reads=[bps, bbi], writes=[btt])
                            P.op("act", lambda e, ex=ex, tt=tt, n=n, cnt=cnt, i=i: e.activation(out=ex[0:n, i:i + cnt, :], in_=tt[0:n, 0:cnt, :], func=AF.Exp),
                                 reads=[btt], writes=[bex])
                        else:
                            P.op("act", lambda e, ex=ex, psv=psv, n=n, cnt=cnt, i=i: e.activation(out=ex[0:n, i:i + cnt, :], in_=psv, func=AF.Exp),
                                 reads=[bps], writes=[bex])
                        i = j + 1
                    gex.append((grp, ex, bex))
                done = 0
                for grp, ex, bex in gex:
                    def mmv(e, grp=grp, ex=ex, v=v, pn=pn, pd=pd, done=done, nchunks=nchunks):
                        for i, (kc0, vc, n, bj) in enumerate(grp):
                            first = (done + i == 0)
                            last = (done + i == nchunks - 1)
                            e.matmul(pn[:, 0:128], lhsT=v[0:n, vc, :], rhs=ex[0:n, i, :], start=first, stop=last)
                            ins_ = e.matmul(pd[:, 0:128], lhsT=L.ones_bf[0:n, :], rhs=ex[0:n, i, :], start=first, stop=last)
                        return ins_
                    P.op("pe", mmv, reads=[bex, bv, L.Bconst], writes=[bpn, bpd])
                    done += len(grp)
                P.op("act", lambda e, pn=pn, onum=onum, qsl=qsl: e.activation(out=onum[:, qsl], in_=pn[:, 0:128], func=AF.Copy), reads=[bpn], writes=[bonum])
                P.op("dve", lambda e, pd=pd, oden=oden, qsl=qsl: e.tensor_copy(out=oden[:, qsl], in_=pd[:, 0:128]), reads=[bpd], writes=[boden])
            P.op("dve", lambda e, oden=oden: e.reciprocal(out=oden[:], in_=oden[:]), reads=[boden], writes=[boden])
            P.op("dve", lambda e, onum=onum, oden=oden: e.tensor_tensor(out=onum[:], in0=onum[:], in1=oden[:], op=ALU.mult), reads=[bonum, boden], writes=[bonum])
            P.op("pool", lambda e, onum=onum, og=og, g=g: e.tensor_tensor(out=og[:], in0=onum[:], in1=g[:], op=ALU.mult), reads=[bonum, bg], writes=[bog])
            P.dma("sp", ogT[cfg.RV + h * 128:cfg.RV + (h + 1) * 128, :], og[:], reads=[bog], writes=[BogT])
        P.barrier()


def stage_conv(C, L, S, ins):
    cfg, P = C.cfg, C.P
    TL, T, CW = cfg.TL, cfg.T, cfg.CW
    NCT = CW // 128
    uT, BuT = S["uT"]; cgT, BcgT = S["cgT"]; ogT, BogT = S["ogT"]
    R0 = cfg.RV + cfg.NAW
    with contextlib.ExitStack() as st:
        par, bpar = C.sb(st, "cpar", [128, NCT, CONV_K + 3], F32)
        P.dma("sp", par[:], ins["cv_par"][:, :, :], writes=[bpar])
        BLKMAX = 512
        ue = [C.sb(st, "cue", [128, NCT, BLKMAX + 30], F32) for _ in range(2)]
        acc = [C.sb(st, "cacc", [128, NCT, BLKMAX], F32) for _ in range(2)]
        sqt = [C.sb(st, "csq", [128, NCT, BLKMAX], F32) for _ in range(2)]
        mean = [C.sb(st, "cmean", [128, BLKMAX], F32) for _ in range(2)]
        rstd = [C.sb(st, "crstd", [128, BLKMAX], F32) for _ in range(2)]
        cg = [C.sb(st, "ccg", [128, NCT, BLKMAX], BF16) for _ in range(2)]
        ob = [C.sb(st, "cob", [128, NCT, BLKMAX], BF16) for _ in range(2)]
        blocks = [("lat", t0, min(t0 + BLKMAX, TL)) for t0 in range(0, TL, BLKMAX)]
        blocks += [("ctx", TL + t0, min(TL + t0 + BLKMAX, T)) for t0 in range(0, cfg.CTX, BLKMAX)]
        uv = uT.rearrange("(c p) t -> p c t", p=128)
        hal = ins.get("halo")
        if hal is None:
            hv = ins["u_halo"].rearrange("(c p) t -> p c t", p=128)
            hvb = (hv[:, :, 0:15], None); hva = (hv[:, :, 15:30], None)
        else:
            hvb = None if hal["ub"] is None else (hal["ub"][0].rearrange("(c p) t -> p c t", p=128), hal["ub"][1])
            hva = None if hal["ua"] is None else (hal["ua"][0].rearrange("(c p) t -> p c t", p=128), hal["ua"][1])
        for bi_, (kind, t0, t1) in enumerate(blocks):
            n = t1 - t0
            (u, bu), (a, ba), (sq, bsq), (mn, bmn), (rs, brs), (cgx, bcg), (o, bo) = [x[bi_ % 2] for x in (ue, acc, sqt, mean, rstd, cg, ob)]
            lo = TL if kind == "ctx" else 0
            hi = T if kind == "ctx" else TL
            s0 = max(t0 - 15, lo); s1 = min(t1 + 15, hi)
            P.dma("sp", u[:, :, 15 - (t0 - s0):15 + n + (s1 - t1)], uv[:, :, s0:s1], reads=[BuT], writes=[bu])
            if t0 - 15 < lo:
                if kind == "lat" and hvb is not None:
                    P.dma("sp", u[:, :, 0:15], hvb[0], reads=([hvb[1]] if hvb[1] is not None else []), writes=[bu])
                else:
                    P.op("pool", lambda e, u=u: e.memset(u[:, :, 0:15], 0.0), writes=[bu])
            if t1 + 15 > hi:
                if kind == "lat" and hva is not None:
                    P.dma("sp", u[:, :, 15 + n:30 + n], hva[0], reads=([hva[1]] if hva[1] is not None else []), writes=[bu])
                else:
                    P.op("pool", lambda e, u=u, n=n: e.memset(u[:, :, 15 + n:30 + n], 0.0), writes=[bu])
            P.dma("sp", cgx[:, :, 0:n], cgT.rearrange("(c p) t -> p c t", p=128)[:, :, t0:t1], reads=[BcgT], writes=[bcg])
            for ct in range(NCT):
                eng = "dve"
                P.op(eng, lambda e, ct=ct, a=a, u=u, n=n: e.tensor_scalar(out=a[:, ct, 0:n], in0=u[:, ct, 0:n], scalar1=par[:, ct, 0:1], scalar2=par[:, ct, CONV_K:CONV_K + 1],
                                                                         op0=ALU.mult, op1=ALU.add), reads=[bu, bpar], writes=[ba])
                for kk in range(1, CONV_K):
                    P.op(eng, lambda e, ct=ct, a=a, u=u, n=n, kk=kk: e.scalar_tensor_tensor(out=a[:, ct, 0:n], in0=u[:, ct, kk:kk + n], scalar=par[:, ct, kk:kk + 1],
                                                                                       in1=a[:, ct, 0:n], op0=ALU.mult, op1=ALU.add), reads=[bu, bpar], writes=[ba])
            ps, bps = L.ps[bi_ % 2]

            def mm1(e, ps=ps, a=a, n=n):
                for ct in range(NCT):
                    ins_ = e.matmul(ps[:, 0:n], lhsT=L.ones_f[:], rhs=a[:, ct, 0:n], start=(ct == 0), stop=(ct == NCT - 1))
                return ins_
            P.op("pe", mm1, reads=[ba, L.Bconst], writes=[bps])
            P.op("act", lambda e, mn=mn, ps=ps, n=n: e.activation(out=mn[:, 0:n], in_=ps[:, 0:n], func=AF.Copy, scale=1.0 / CW), reads=[bps], writes=[bmn])
            P.op("dve", lambda e, a=a, mn=mn, n=n: e.tensor_tensor(out=a[:, :, 0:n], in0=a[:, :, 0:n], in1=bc(mn[:, 0:n], [128, NCT, n], 1), op=ALU.subtract),
                 reads=[ba, bmn], writes=[ba])
            P.op("act", lambda e, sq=sq, a=a, n=n: e.activation(out=sq[:, :, 0:n], in_=a[:, :, 0:n], func=AF.Square), reads=[ba], writes=[bsq])
            ps2, bps2 = L.ps[2 + bi_ % 2]

            def mm2(e, ps2=ps2, sq=sq, n=n):
                for ct in range(NCT):
                    ins_ = e.matmul(ps2[:, 0:n], lhsT=L.ones_f[:], rhs=sq[:, ct, 0:n], start=(ct == 0), stop=(ct == NCT - 1))
                return ins_
            P.op("pe", mm2, reads=[bsq, L.Bconst], writes=[bps2])
            P.op("act", lambda e, rs=rs, ps2=ps2, n=n: e.activation(out=rs[:, 0:n], in_=ps2[:, 0:n], func=AF.Sqrt, bias=EPS, scale=1.0 / CW), reads=[bps2], writes=[brs])
            P.op("dve", lambda e, rs=rs, n=n: e.reciprocal(out=rs[:, 0:n], in_=rs[:, 0:n]), reads=[brs], writes=[brs])
            P.op("dve", lambda e, a=a, rs=rs, n=n: e.tensor_tensor(out=a[:, :, 0:n], in0=a[:, :, 0:n], in1=bc(rs[:, 0:n], [128, NCT, n], 1), op=ALU.mult),
                 reads=[ba, brs], writes=[ba])
            for ct in range(NCT):
                P.op("act", lambda e, a=a, ct=ct, n=n: e.activation(out=a[:, ct, 0:n], in_=a[:, ct, 0:n], func=AF.Silu, scale=par[:, ct, CONV_K + 1:CONV_K + 2],
                                                                    bias=par[:, ct, CONV_K + 2:CONV_K + 3]), reads=[ba, bpar], writes=[ba])
            P.op("pool", lambda e, o=o, a=a, cgx=cgx, n=n: e.tensor_tensor(out=o[:, :, 0:n], in0=a[:, :, 0:n], in1=cgx[:, :, 0:n], op=ALU.mult),
                 reads=[ba, bcg], writes=[bo])
            P.dma("sp", ogT[R0:R0 + CW, t0:t1].rearrange("(c p) t -> p c t", p=128), o[:, :, 0:n], reads=[bo], writes=[BogT])
        P.barrier()


def tok_tiles(cfg, with_ctx=True):
    subs = [(t0, min(t0 + 512, cfg.TL)) for t0 in range(0, cfg.TL, 512)]
    if with_ctx:
        subs += [(cfg.TL, cfg.T)]
    tiles = []
    cur = []
    tot = 0
    for s in subs:
        n = s[1] - s[0]
        if tot + n > 1280 and cur:
            tiles.append(cur); cur = []; tot = 0
        cur.append(s); tot += n
    if cur:
        tiles.append(cur)
    return tiles


def common_inputs(nc, cfg):
    ins = {}
    d = lambda name, shape, dt=F32: nc.dram_tensor(name, list(shape), dt, kind="ExternalInput").ap()
    NCL = cfg.TL // 128
    ins["ident"] = d("ident", [128, 128], BF16)
    ins["mods"] = d("mods", [128, cfg.KC, 6])
    ins["norm_g"] = d("norm_g", [128, cfg.KC])
    ins["decay"] = d("decay", [128, 2 * cfg.H])
    ins["rope"] = d("rope", [2, 128, cfg.TL])
    ins["na_gain"] = d("na_gain", [128, 2])
    ins["rconst"] = d("rconst", [6, 128, 128])
    ins["rvec"] = d("rvec", [128, 2 + 2 * NCL + 10])
    return ins


def build_A(cfg):
    nc = bass.Bass("TRN2", target_bir_lowering=False)
    C = Ctx(nc, cfg)
    ins = common_inputs(nc, cfg)
    TL = cfg.TL
    cols = [cfg.off["ret_k"], cfg.off["ret_v"], cfg.off["na_k"], cfg.off["na_v"], cfg.off["cv_a"], cfg.off["cv_b"]]
    ncol = sum(n for _, n in cols)
    xT = nc.dram_tensor("xT", [cfg.D, TL], F32, kind="ExternalInput").ap()
    wA = nc.dram_tensor("wA", [cfg.D, ncol], F32, kind="ExternalInput").ap()
    Fout = nc.dram_tensor("Fout", [2, cfg.H, 128, 256], F32, kind="ExternalOutput").ap()
    nk_e = nc.dram_tensor("nk_e", [cfg.NAW, 512], BF16, kind="ExternalOutput").ap()
    nv_e = nc.dram_tensor("nv_e", [512, cfg.NAW], BF16, kind="ExternalOutput").ap()
    u_e = nc.dram_tensor("u_e", [cfg.CW, 512], F32, kind="ExternalOutput").ap()
    Bx, Bw = Buf("xT"), Buf("wA")
    cfgA = Cfg(cfg.D, cfg.H, cfg.SEQ, cfg.CTX, cfg.DEPTH, cfg.B)
    o = 0
    cfgA.off = dict(cfg.off)
    for nm in ("ret_k", "ret_v", "na_k", "na_v", "cv_a", "cv_b"):
        cfgA.off[nm] = (o, cfg.off[nm][1]); o += cfg.off[nm][1]
    C.cfg = cfgA
    with contextlib.ExitStack() as st:
        L = setup_common(C, st, ins)
        S = {}
        hxA = nc.dram_tensor("hxA", [cfg.D, TL], BF16, kind="ExternalOutput").ap()
        S["hxT"] = (hxA, Buf("hxA"))
        S["kT"] = C.dram("kT", [cfg.RQK, cfg.T], BF16)
        S["vtm"] = C.dram("vtm", [cfg.T, cfg.RV], BF16)
        S["nkT"] = (nk_e, Buf("nk_e"))
        S["nvtm"] = (nv_e, Buf("nv_e"))
        S["uT"] = (u_e, Buf("u_e"))
        stage_norm(C, L, xT, Bx, None, None, S["hxT"][0], S["hxT"][1], do_ctx=False)
        groups, setup = inproj_groups(C, L, S, ins, {"ret_k", "ret_v"})
        stage_gemm(C, L, S["hxT"][0], S["hxT"][1], [(0, cfg.D)], wA, tok_tiles(cfg, False), groups, setup)
        stage_ret_passA(C, L, S, ins, Fout, Buf("Fout"))
        hx_e, Bhe = C.dram("hx_e", [cfg.D, 512], BF16)
        with contextlib.ExitStack() as st2:
            t, bt = C.sb(st2, "edge", [128, cfg.KC, 512], BF16)
            hv = S["hxT"][0].rearrange("(c p) t -> p c t", p=128)
            C.P.dma("sp", t[:, :, 0:256], hv[:, :, 0:256], reads=[S["hxT"][1]], writes=[bt])
            C.P.dma("sp", t[:, :, 256:512], hv[:, :, TL - 256:TL], reads=[S["hxT"][1]], writes=[bt])
            C.P.dma("sp", hx_e.rearrange("(c p) t -> p c t", p=128), t[:], reads=[bt], writes=[Bhe])
            C.P.barrier()
        groups, setup = inproj_groups(C, L, S, ins, {"na_k", "na_v", "cv_ab"})
        stage_gemm(C, L, hx_e, Bhe, [(0, cfg.D)], wA, [[(0, 512)]], groups, setup)
        C.P.emit()
    return nc


def build_B(cfg):
    nc = bass.Bass("TRN2", target_bir_lowering=False)
    C = Ctx(nc, cfg)
    ins = common_inputs(nc, cfg)
    TL, T, D = cfg.TL, cfg.T, cfg.D
    d = lambda name, shape, dt=F32: nc.dram_tensor(name, list(shape), dt, kind="ExternalInput").ap()
    xT = d("xT", [D, TL]); cT = d("cT", [D, cfg.CTX])
    hx_in = d("hx_in", [D, TL], BF16)
    w_in = d("w_in", [D, cfg.N_IN]); w_br = d("w_br", [cfg.KBR, D]); w_out = d("w_out", [D, D])
    ins["Sall"] = d("Sall", [4, 2, cfg.H, 128, 256])
    ins["nk_halo"] = d("nk_halo", [cfg.NAW, 512], BF16)
    ins["nv_halo"] = d("nv_halo", [512, cfg.NAW], BF16)
    ins["u_halo"] = d("u_halo", [cfg.CW, 30])
    ins["na_bias"] = d("na_bias", [5, cfg.H, 6, 128, 128])
    ins["cv_par"] = d("cv_par", [128, cfg.CW // 128, CONV_K + 3])
    xo = nc.dram_tensor("xoT", [D, TL], F32, kind="ExternalOutput").ap()
    co = nc.dram_tensor("coT", [D, cfg.CTX], F32, kind="ExternalOutput").ap()
    Bx, Bc, Bxo, Bco = Buf("xT"), Buf("cT"), Buf("xo"), Buf("co")
    with contextlib.ExitStack() as st:
        L = setup_common(C, st, ins)
        S = {}
        S["hxT"] = C.dram("hxT", [D, T], BF16)
        S["qT"] = C.dram("qT", [cfg.RQK, T], BF16)
        S["kT"] = C.dram("kT", [cfg.RQK, T], BF16)
        S["vtm"] = C.dram("vtm", [T, cfg.RV], BF16)
        S["rgT"] = C.dram("rgT", [cfg.RV, T], BF16)
        S["nqT"] = C.dram("nqT", [cfg.NAW, T], BF16)
        S["nkT"] = C.dram("nkT", [cfg.NAW, T], BF16)
        S["nvtm"] = C.dram("nvtm", [T, cfg.NAW], BF16)
        S["ngT"] = C.dram("ngT", [cfg.NAW, T], BF16)
        S["uT"] = C.dram("uT", [cfg.CW, T], F32)
        S["cgT"] = C.dram("cgT", [cfg.CW, T], BF16)
        S["sgT"] = C.dram("sgT", [3 * D, T], BF16)
        S["ogT"] = C.dram("ogT", [cfg.KBR, T], BF16)
        S["mT"] = C.dram("mT", [D, T], BF16)
        for r0 in range(0, D, D // 4):
            C.P.dma("sp", S["hxT"][0][r0:r0 + D // 4, 0:TL], hx_in[r0:r0 + D // 4, :], writes=[S["hxT"][1]])
        stage_norm(C, L, xT, Bx, cT, Bc, S["hxT"][0], S["hxT"][1], do_lat=False)
        groups, setup = inproj_groups(C, L, S, ins, {"ret_q", "ret_k", "ret_v", "na_q", "na_k", "na_v", "cv_ab", "silu", "gates"})
        tiles = tok_tiles(cfg, True)
        stage_gemm(C, L, S["hxT"][0], S["hxT"][1], [(0, D)], w_in, tiles, groups, setup)
        stage_ret(C, L, S, ins, ins["Sall"], Buf("Sall"))
        stage_na(C, L, S, ins)
        stage_conv(C, L, S, ins)
        P = C.P
        sgT, BsgT = S["sgT"]; mT, BmT = S["mT"]

        def setup_m(st2):
            env = {}
            env["sg"] = Rot([C.sb(st2, "msg", [128, 3, 512], BF16) for _ in range(3)])
            env["t"] = Rot([C.sb(st2, "mt", [128, 3, 512], F32) for _ in range(2)])
            env["o"] = Rot([C.sb(st2, "mo", [128, 512], BF16) for _ in range(3)])
            return env

        def epi_m(env, col0, sub, pss):
            nt = sub[1] - sub[0]
            sg, bsg = env["sg"].next()
            t, bt = env["t"].next()
            o, bo = env["o"].next()
            P.dma("sp", sg[:, :, 0:nt], sgT.rearrange("(b d) t -> d b t", b=3)[col0:col0 + 128, :, sub[0]:sub[1]], reads=[BsgT], writes=[bsg])
            for b in range(3):
                ps, bps = pss[b]
                P.op("dve", lambda e, b=b, ps=ps: e.tensor_tensor(out=t[:, b, 0:nt], in0=ps[:, 0:nt], in1=sg[:, b, 0:nt], op=ALU.mult),
                     reads=[bps, bsg], writes=[bt])
            P.op("dve", lambda e: e.tensor_tensor(out=t[:, 0, 0:nt], in0=t[:, 0, 0:nt], in1=t[:, 1, 0:nt], op=ALU.add), reads=[bt], writes=[bt])
            P.op("dve", lambda e: e.tensor_tensor(out=o[:, 0:nt], in0=t[:, 0, 0:nt], in1=t[:, 2, 0:nt], op=ALU.add), reads=[bt], writes=[bo])
            P.dma("sp", mT[col0:col0 + 128, sub[0]:sub[1]], o[:, 0:nt], reads=[bo], writes=[BmT])
        groups = [dict(cols=[(c, 256)], mode="fm", epi=epi_m) for c in range(0, D, 256)]
        stage_gemm(C, L, S["ogT"][0], S["ogT"][1], [(0, cfg.RV), (cfg.RV, cfg.NAW), (cfg.RV + cfg.NAW, cfg.CW)], w_br, tiles, groups, setup_m)

        def setup_f(st2):
            env = {}
            env["x"] = Rot([C.sb(st2, "fx", [128, 512], F32) for _ in range(3)])
            return env

        def epi_f(env, col0, sub, pss):
            nt = sub[1] - sub[0]
            ps, bps = pss[0]
            x, bx = env["x"].next()
            kc = col0 // 128
            if sub[0] >= TL:
                src, Bsrc, dst, Bdst, s, c0 = cT, Bc, co, Bco, 1, sub[0] - TL
            else:
                src, Bsrc, dst, Bdst, s, c0 = xT, Bx, xo, Bxo, 0, sub[0]
            P.dma("sp", x[:, 0:nt], src[col0:col0 + 128, c0:c0 + nt], reads=[Bsrc], writes=[bx])
            P.op("dve", lambda e: e.scalar_tensor_tensor(out=x[:, 0:nt], in0=ps[:, 0:nt], scalar=L.GT[:, s, kc:kc + 1], in1=x[:, 0:nt],
                                                         op0=ALU.mult, op1=ALU.add), reads=[bps, bx, L.Bpar], writes=[bx])
            P.dma("sp", dst[col0:col0 + 128, c0:c0 + nt], x[:, 0:nt], reads=[bx], writes=[Bdst])
        groups = [dict(cols=[(c, 256)], mode="fm", epi=epi_f) for c in range(0, D, 256)]
        stage_gemm(C, L, S["mT"][0], S["mT"][1], [(0, D)], w_out, tiles, groups, setup_f)
        C.P.emit()
    return nc


def build_ada(cfg):
    nc = bass.Bass("TRN2", target_bir_lowering=False)
    C = Ctx(nc, cfg)
    P = C.P
    D, KC, Lyr = cfg.D, cfg.KC, cfg.DEPTH
    NCOL = 3 * D // 8
    cv = nc.dram_tensor("cv", [128, KC, 3], F32, kind="ExternalInput").ap()
    wa = nc.dram_tensor("wa", [Lyr, D, NCOL], F32, kind="ExternalInput").ap()
    ba = nc.dram_tensor("ba", [Lyr, 3, NCOL], F32, kind="ExternalInput").ap()
    out = nc.dram_tensor("out", [Lyr, 3, NCOL], F32, kind="ExternalOutput").ap()
    CB = min(512, NCOL)
    with contextlib.ExitStack() as st:
        ps = [(st.enter_context(nc.psum_tensor(C.name("ps"), [128, 512], F32)), Buf("ps")) for _ in range(2)]
        c, bcv = C.sb(st, "c", [128, KC, 3], F32)
        P.dma("sp", c[:], cv[:, :, :], writes=[bcv])
        P.op("act", lambda e: e.activation(out=c[:], in_=c[:], func=AF.Silu), reads=[bcv], writes=[bcv])
        wbs = [C.sb(st, "w", [128, KC, CB], F32) for _ in range(2)]
        obs = [C.sb(st, "o", [3, CB], F32) for _ in range(2)]
        bbs = [C.sb(st, "b", [3, CB], F32) for _ in range(2)]
        Bout = Buf("out")
        i = 0
        for l in range(Lyr):
            for c0 in range(0, NCOL, CB):
                (w, bw), (o, bo), (b, bb), (p, bp) = wbs[i % 2], obs[i % 2], bbs[i % 2], ps[i % 2]
                i += 1
                half = KC // 2
                P.dma("sp", w[:, 0:half, :], wa[l].rearrange("(c p) n -> p c n", p=128)[:, 0:half, c0:c0 + CB], writes=[bw])
                P.dma("sp", w[:, half:KC, :], wa[l].rearrange("(c p) n -> p c n", p=128)[:, half:KC, c0:c0 + CB], writes=[bw])
                P.dma("sp", b[:], ba[l, :, c0:c0 + CB], writes=[bb])

                def mm(e, w=w, p=p):
                    for kc in range(KC):
                        ins_ = e.matmul(p[0:3, 0:CB], lhsT=c[:, kc, :], rhs=w[:, kc, :], start=(kc == 0), stop=(kc == KC - 1))
                    return ins_
                P.op("pe", mm, reads=[bw, bcv], writes=[bp])
                P.op("dve", lambda e, o=o, p=p, b=b: e.tensor_tensor(out=o[:], in0=p[0:3, 0:CB], in1=b[:], op=ALU.add), reads=[bp, bb], writes=[bo])
                P.dma("sp", out[l, :, c0:c0 + CB], o[:], reads=[bo], writes=[Bout])
        P.emit()
    return nc


def rope_tables(cfg, q):
    t = np.arange(cfg.TL) + q * cfg.TL
    row = (t // GRID_W).astype(np.float32)
    col = (t % GRID_W).astype(np.float32)
    n_freq = 32
    inv = (10000.0 ** (-np.arange(n_freq, dtype=np.float32) / n_freq)).astype(np.float32)
    ang = np.concatenate([row[:, None] * inv, col[:, None] * inv], axis=-1).astype(np.float32)
    cos = np.cos(ang).T.astype(np.float32)
    sin = np.sin(ang).T.astype(np.float32)
    cosf = np.concatenate([cos, cos], 0)
    sinf = np.concatenate([sin, -sin], 0)
    return np.stack([cosf, sinf]).astype(np.float32)


def ret_consts(cfg, q):
    i = np.arange(128, dtype=np.float32)
    J, I = np.meshgrid(i, i, indexing="ij")
    relF = np.maximum(I - J, 0); relB = np.maximum(J - I, 0)
    indF = (I >= J).astype(np.float32); indB = (J >= I).astype(np.float32)
    qdf = np.broadcast_to(i[None, :] + 1.0, (128, 128)); qdb = np.broadcast_to(128.0 - i[None, :], (128, 128))
    rconst = np.stack([relF, relB, indF, indB, qdf, qdb]).astype(np.float32)
    NCL = cfg.TL // 128
    TL = cfg.TL
    vec = np.zeros((128, 2 + 2 * NCL + 10), np.float32)
    vec[:, 0] = 127.0 - i
    vec[:, 1] = i
    for c in range(NCL):
        vec[:, 2 + c] = TL - 1 - (128 * c + i)
        vec[:, 2 + NCL + c] = 128 * c + i
    o = 2 + 2 * NCL
    for r in range(4):
        vec[:, o + r] = TL * (q - 1 - r) if r < q else BIGD
        vec[:, o + 5 + r] = TL * (r - q - 1) if r > q else BIGD
    vec[:, o + 4] = TL * q
    vec[:, o + 9] = TL * (3 - q)
    return rconst, vec


def na_bias_table(cfg, q, rpb):
    NP, H = cfg.NP, cfg.H
    rows = cfg.ROWS
    out = np.full((5, H, 768, 128), NEG, np.float32)
    qi = np.arange(128)
    for m in sorted(set([0, 1, 2, NP - 2, NP - 1])):
        if m < 0 or m >= NP:
            continue
        slot, sc = na_pair_info(NP, m)
        nk = NA_SLOT_KEYS[slot]
        kk = np.arange(nk)
        ext_tok = 128 * sc + kk
        gr = q * cfg.LR + ext_tok // 64 - 4
        kc = ext_tok % 64
        r = q * cfg.LR + 2 * m + qi // 64
        c = qi % 64
        rs = np.clip(r - 4, 0, rows - 8)
        cs = np.clip(c - 8, 0, GRID_W - 16)
        valid = ((gr[:, None] >= rs[None, :]) & (gr[:, None] < rs[None, :] + 8) & (kc[:, None] >= cs[None, :]) & (kc[:, None] < cs[None, :] + 16)
                 & (gr[:, None] >= 0) & (gr[:, None] < rows))
        ri = np.clip(gr[:, None] - r[None, :] + 7, 0, 14)
        ci = np.clip(kc[:, None] - c[None, :] + 15, 0, 30)
        for h in range(H):
            vals = rpb[h][ri, ci]
            out[slot, h, :nk, :] = np.where(valid, vals, np.float32(NEG))
    return out.reshape(5, H, 6, 128, 128)


def fm(v, KC):
    return np.ascontiguousarray(v.reshape(KC, 128).T)


_CACHE = {}


def _get(name, builder, cfg):
    key = (name, cfg.D, cfg.H, cfg.SEQ, cfg.DEPTH)
    if key not in _CACHE:
        _CACHE[key] = builder(cfg)
    return _CACHE[key]


def run_model(cfg, x, c, ctx, c_ctx, w_ada, b_ada, norm_g, w_in, ret_decay_f, ret_decay_b, w_ret_o,
              na_q_gain, na_k_gain, na_rpb, w_na_o, cv_dw, cv_db, cv_ln_g, cv_ln_b, w_cv_o, w_out):
    f32 = np.float32
    D, KC, H, TL, NQ = cfg.D, cfg.KC, cfg.H, cfg.TL, cfg.NQ
    cores = list(range(8))
    x = np.asarray(x, f32); ctx = np.asarray(ctx, f32)
    w_ada = np.asarray(w_ada, f32)[:cfg.DEPTH]; b_ada = np.asarray(b_ada, f32)[:cfg.DEPTH]
    nc_ada = _get("ada", build_ada, cfg)
    cvec = np.stack([np.asarray(c[0], f32), np.asarray(c[1], f32), np.asarray(c_ctx, f32)], -1)
    cvl = np.ascontiguousarray(cvec.reshape(KC, 128, 3).transpose(1, 0, 2))
    NCOL = 3 * D // 8
    in_maps = []
    for k in cores:
        in_maps.append({"cv": cvl,
                        "wa": np.ascontiguousarray(np.asarray(w_ada, f32)[:, :, k * NCOL:(k + 1) * NCOL]),
                        "ba": np.ascontiguousarray(np.broadcast_to(np.asarray(b_ada, f32)[:, None, k * NCOL:(k + 1) * NCOL], (cfg.DEPTH, 3, NCOL)))})
    res = run_bass_kernel_spmd(nc_ada, in_maps, core_ids=cores)
    mods_all = np.concatenate([r["out"] for r in res.results], axis=-1)
    ident = np.eye(128, dtype=f32).astype(NPBF)
    xT = [np.ascontiguousarray(x[k // NQ, (k % NQ) * TL:(k % NQ + 1) * TL, :].T) for k in cores]
    cT = [np.ascontiguousarray(ctx[b].T) for b in range(cfg.B)]
    consts = [ret_consts(cfg, k % NQ) for k in cores]
    ropes = [rope_tables(cfg, k % NQ) for k in cores]
    nc_A = _get("A", build_A, cfg)
    nc_B = _get("B", build_B, cfg)
    for l in range(cfg.DEPTH):
        ml = mods_all[l]
        common = []
        for k in cores:
            b = k // NQ
            mods = np.stack([fm(ml[b, 0:D], KC), fm(ml[b, D:2 * D], KC), fm(ml[b, 2 * D:3 * D], KC),
                             fm(ml[2, 0:D], KC), fm(ml[2, D:2 * D], KC), fm(ml[2, 2 * D:3 * D], KC)], -1)
            dec = np.concatenate([np.asarray(ret_decay_f[l], f32), np.asarray(ret_decay_b[l], f32)])
            common.append({
                "ident": ident, "mods": np.ascontiguousarray(mods), "norm_g": fm(np.asarray(norm_g[l], f32), KC),
                "decay": np.ascontiguousarray(np.broadcast_to(dec[None, :], (128, 2 * H))),
                "rope": ropes[k], "na_gain": np.ascontiguousarray(np.stack([np.asarray(na_q_gain[l], f32), np.asarray(na_k_gain[l], f32)], -1)),
                "rconst": consts[k][0], "rvec": consts[k][1]})
        wl = np.asarray(w_in[l], f32)
        sel = []
        for nm in ("ret_k", "ret_v", "na_k", "na_v", "cv_a", "cv_b"):
            o, n = cfg.off[nm]
            sel.append(wl[:, o:o + n])
        wA = np.ascontiguousarray(np.concatenate(sel, axis=1))
        in_maps = [dict(common[k], xT=xT[k], wA=wA) for k in cores]
        resA = run_bass_kernel_spmd(nc_A, in_maps, core_ids=cores).results
        w_br = np.ascontiguousarray(np.concatenate([np.asarray(w_ret_o[l], f32), np.asarray(w_na_o[l], f32), np.asarray(w_cv_o[l], f32)], 0))
        wo = np.asarray(w_out[l], f32)
        NCT = cfg.CW // 128
        cvp = np.concatenate([np.asarray(cv_dw[l], f32).T, np.asarray(cv_db[l], f32)[:, None], np.asarray(cv_ln_g[l], f32)[:, None],
                              np.asarray(cv_ln_b[l], f32)[:, None]], axis=1)
        cvp = np.ascontiguousarray(cvp.reshape(NCT, 128, CONV_K + 3).transpose(1, 0, 2))
        rpb = np.asarray(na_rpb[l], f32)
        in_maps = []
        for k in cores:
            b, q = k // NQ, k % NQ
            Sall = np.stack([resA[b * NQ + r]["Fout"] for r in range(NQ)])
            nkh = np.zeros((cfg.NAW, 512), NPBF); nvh = np.zeros((512, cfg.NAW), NPBF); uh = np.zeros((cfg.CW, 30), f32)
            if q > 0:
                p = resA[k - 1]
                nkh[:, 0:256] = p["nk_e"][:, 256:512]; nvh[0:256] = p["nv_e"][256:512]; uh[:, 0:15] = p["u_e"][:, 512 - 15:512]
            if q < NQ - 1:
                p = resA[k + 1]
                nkh[:, 256:512] = p["nk_e"][:, 0:256]; nvh[256:512] = p["nv_e"][0:256]; uh[:, 15:30] = p["u_e"][:, 0:15]
            in_maps.append(dict(common[k], xT=xT[k], cT=cT[b], hx_in=resA[k]["hxA"], w_in=wl, w_br=w_br, w_out=wo, Sall=np.ascontiguousarray(Sall),
                                nk_halo=nkh, nv_halo=nvh, u_halo=uh, na_bias=na_bias_table(cfg, q, rpb), cv_par=cvp))
        resB = run_bass_kernel_spmd(nc_B, in_maps, core_ids=cores).results
        xT = [resB[k]["xoT"] for k in cores]
        if getattr(cfg, "debug", False):
            DBGOUT[l] = resB
        if l < cfg.DEPTH - 1:
            cT = [resB[b * NQ]["coT"] for b in range(cfg.B)]
    out = np.empty((cfg.B, cfg.SEQ, D), f32)
    for k in cores:
        out[k // NQ, (k % NQ) * TL:(k % NQ + 1) * TL, :] = xT[k].T
    return out


def setup_global(C, st, ins):
    cfg, P, nc = C.cfg, C.P, C.nc
    L = Layer()
    KC, H = cfg.KC, cfg.H
    L.ps = []
    for i in range(7):
        t = st.enter_context(nc.psum_tensor(C.name("ps"), [128, 512], F32))
        L.ps.append((t, Buf(f"ps{i}")))
    t = st.enter_context(nc.psum_tensor(C.name("psb"), [128, 1024], BF16))
    L.psb = (t, Buf("psb"))
    L.ones_bf, _ = C.sb(st, "ones_bf", [128, 128], BF16)
    L.ones_f, _ = C.sb(st, "ones_f", [128, 128], F32)
    L.ident, _ = C.sb(st, "ident", [128, 128], BF16)
    L.Bconst = Buf("const")
    P.op("pool", lambda e: e.memset(L.ones_bf[:], 1.0), writes=[L.Bconst])
    P.op("pool", lambda e: e.memset(L.ones_f[:], 1.0), writes=[L.Bconst])
    P.dma("sp", L.ident[:], ins["ident"][:, :], writes=[L.Bconst])
    L.modsAll, _ = C.sb(st, "modsAll", [128, cfg.DEPTH, 3 * KC, 2], F32)
    L.Bmods = Buf("modsAll")
    L.ng, _ = C.sb(st, "ng", [128, KC], F32)
    L.G, _ = C.sb(st, "G", [128, 2, KC], F32)
    L.SH, _ = C.sb(st, "SH", [128, 2, KC], F32)
    L.GT, _ = C.sb(st, "GT", [128, 2, KC], F32)
    L.Bpar = Buf("par")
    L.lg, _ = C.sb(st, "lg", [128, 2 * H], F32)
    L.lgt, _ = C.sb(st, "lgt", [128, 2 * H], F32)
    L.lgt2, _ = C.sb(st, "lgt2", [128, 2 * H], F32)
    L.Blg = Buf("lg")
    return L


def stage_ada_fused(C, L, cv, w_ada, b_ada_fm):
    cfg, P = C.cfg, C.P
    KC, D = cfg.KC, cfg.D
    with contextlib.ExitStack() as st:
        c, bcv = C.sb(st, "adac", [128, KC, 2], F32)
        P.dma("sp", c[:], cv[:, :, :], writes=[bcv])
        P.op("act", lambda e: e.activation(out=c[:], in_=c[:], func=AF.Silu), reads=[bcv], writes=[bcv])
        bfm, bb = C.sb(st, "adab", [128, cfg.DEPTH, 3 * KC], F32)
        P.dma("sp", bfm[:], b_ada_fm[:, :, :], writes=[bb])
        wbs = [C.sb(st, "adaw", [128, KC, 512], F32) for _ in range(2)]
        i = 0
        for l in range(cfg.DEPTH):
            wv = w_ada[l].rearrange("(c p) n -> p c n", p=128)
            for c0 in range(0, 3 * D, 512):
                (w, bw) = wbs[i % 2]
                ps, bps = L.ps[i % 2]
                i += 1
                half = KC // 2
                P.dma("sp", w[:, 0:half, :], wv[:, 0:half, c0:c0 + 512], writes=[bw])
                P.dma("sp", w[:, half:KC, :], wv[:, half:KC, c0:c0 + 512], writes=[bw])

                def mm(e, w=w, ps=ps):
                    for jj in range(4):
                        for kc in range(KC):
                            ins_ = e.matmul(ps[:, jj * 2:jj * 2 + 2], lhsT=w[:, kc, jj * 128:(jj + 1) * 128], rhs=c[:, kc, :],
                                            start=(kc == 0), stop=(kc == KC - 1))
                    return ins_
                P.op("pe", mm, reads=[bw, bcv], writes=[bps])
                for jj in range(4):
                    j = c0 // 128 + jj
                    P.op("act", lambda e, l=l, j=j, jj=jj, ps=ps: e.activation(out=L.modsAll[:, l, j, :], in_=ps[:, jj * 2:jj * 2 + 2], func=AF.Identity,
                                                                             bias=bfm[:, l, j:j + 1]), reads=[bps, bb], writes=[L.Bmods])
        P.barrier()


def setup_layer(C, L, l, norm_g_l, decay_l):
    cfg, P = C.cfg, C.P
    KC = cfg.KC
    P.dma("sp", L.ng[:], norm_g_l, writes=[L.Bpar])
    for s_ in range(2):
        P.op("dve", lambda e, s_=s_: e.scalar_tensor_tensor(out=L.G[:, s_, :], in0=L.modsAll[:, l, KC:2 * KC, s_], scalar=1.0,
                                                            in1=L.ng[:], op0=ALU.add, op1=ALU.mult), reads=[L.Bpar, L.Bmods], writes=[L.Bpar])
        P.op("dve", lambda e, s_=s_: e.tensor_copy(out=L.SH[:, s_, :], in_=L.modsAll[:, l, 0:KC, s_]), reads=[L.Bmods], writes=[L.Bpar])
        P.op("dve", lambda e, s_=s_: e.tensor_copy(out=L.GT[:, s_, :], in_=L.modsAll[:, l, 2 * KC:3 * KC, s_]), reads=[L.Bmods], writes=[L.Bpar])
    tmp, tmp2 = L.lgt, L.lgt2
    P.dma("sp", L.lg[:], decay_l, writes=[L.Blg])
    P.op("act", lambda e: e.activation(out=tmp[:], in_=L.lg[:], func=AF.Exp, scale=-1.0), reads=[L.Blg], writes=[L.Blg])
    P.op("dve", lambda e: e.tensor_scalar(out=tmp2[:], in0=tmp[:], scalar1=0.2, scalar2=-0.25, op0=ALU.mult, op1=ALU.add),
         reads=[L.Blg], writes=[L.Blg])
    for cst in (1.0 / 3.0, -0.5, 1.0):
        P.op("dve", lambda e: e.tensor_tensor(out=tmp2[:], in0=tmp2[:], in1=tmp[:], op=ALU.mult), reads=[L.Blg], writes=[L.Blg])
        P.op("dve", lambda e, cst=cst: e.tensor_scalar(out=tmp2[:], in0=tmp2[:], scalar1=cst, scalar2=None, op0=ALU.add),
             reads=[L.Blg], writes=[L.Blg])
    P.op("dve", lambda e: e.tensor_tensor(out=tmp2[:], in0=tmp2[:], in1=tmp[:], op=ALU.mult), reads=[L.Blg], writes=[L.Blg])
    P.op("dve", lambda e: e.tensor_scalar(out=L.lg[:], in0=tmp2[:], scalar1=-1.0, scalar2=None, op0=ALU.mult),
         reads=[L.Blg], writes=[L.Blg])
    P.barrier()


def stage_merge(C, L, S, w_br, tiles):
    cfg, P = C.cfg, C.P
    D = cfg.D
    sgT, BsgT = S["sgT"]; mT, BmT = S["mT"]

    def setup_m(st2):
        env = {}
        env["sg"] = Rot([C.sb(st2, "msg", [128, 3, 512], BF16) for _ in range(3)])
        env["t"] = Rot([C.sb(st2, "mt", [128, 3, 512], F32) for _ in range(2)])
        env["o"] = Rot([C.sb(st2, "mo", [128, 512], BF16) for _ in range(3)])
        return env

    def epi_m(env, col0, sub, pss):
        nt = sub[1] - sub[0]
        sg, bsg = env["sg"].next()
        t, bt = env["t"].next()
        o, bo = env["o"].next()
        P.dma("sp", sg[:, :, 0:nt], sgT.rearrange("(b d) t -> d b t", b=3)[col0:col0 + 128, :, sub[0]:sub[1]], reads=[BsgT], writes=[bsg])
        for b in range(3):
            ps, bps = pss[b]
            P.op("dve", lambda e, b=b, ps=ps: e.tensor_tensor(out=t[:, b, 0:nt], in0=ps[:, 0:nt], in1=sg[:, b, 0:nt], op=ALU.mult),
                 reads=[bps, bsg], writes=[bt])
        P.op("dve", lambda e: e.tensor_tensor(out=t[:, 0, 0:nt], in0=t[:, 0, 0:nt], in1=t[:, 1, 0:nt], op=ALU.add), reads=[bt], writes=[bt])
        P.op("dve", lambda e: e.tensor_tensor(out=o[:, 0:nt], in0=t[:, 0, 0:nt], in1=t[:, 2, 0:nt], op=ALU.add), reads=[bt], writes=[bo])
        P.dma("sp", mT[col0:col0 + 128, sub[0]:sub[1]], o[:, 0:nt], reads=[bo], writes=[BmT])
    groups = [dict(cols=[(c, 256)], mode="fm", epi=epi_m) for c in range(0, D, 256)]
    stage_gemm(C, L, S["ogT"][0], S["ogT"][1], [(0, cfg.RV), (cfg.RV, cfg.NAW), (cfg.RV + cfg.NAW, cfg.CW)], w_br, tiles, groups, setup_m)


def stage_final(C, L, S, w_out, tiles, xsrc, Bxs, csrc, Bcs, xdst, Bxd, cdst, Bcd):
    cfg, P = C.cfg, C.P
    D, TL = cfg.D, cfg.TL

    def setup_f(st2):
        return {"x": Rot([C.sb(st2, "fx", [128, 512], F32) for _ in range(3)])}

    def epi_f(env, col0, sub, pss):
        nt = sub[1] - sub[0]
        ps, bps = pss[0]
        x, bx = env["x"].next()
        kc = col0 // 128
        if sub[0] >= TL:
            src, Bsrc, dst, Bdst, s_, c0 = csrc, Bcs, cdst, Bcd, 1, sub[0] - TL
        else:
            src, Bsrc, dst, Bdst, s_, c0 = xsrc, Bxs, xdst, Bxd, 0, sub[0]
        P.dma("sp", x[:, 0:nt], src[col0:col0 + 128, c0:c0 + nt], reads=[Bsrc], writes=[bx])
        P.op("dve", lambda e: e.scalar_tensor_tensor(out=x[:, 0:nt], in0=ps[:, 0:nt], scalar=L.GT[:, s_, kc:kc + 1], in1=x[:, 0:nt],
                                                     op0=ALU.mult, op1=ALU.add), reads=[bps, bx, L.Bpar], writes=[bx])
        P.dma("sp", dst[col0:col0 + 128, c0:c0 + nt], x[:, 0:nt], reads=[bx], writes=[Bdst])
    groups = [dict(cols=[(c, 256)], mode="fm", epi=epi_f) for c in range(0, D, 256)]
    stage_gemm(C, L, S["mT"][0], S["mT"][1], [(0, D)], w_out, tiles, groups, setup_f)


def build_F(cfg):
    nc = bass.Bass("TRN2", target_bir_lowering=False)
    C = Ctx(nc, cfg)
    NQ, TL, T, D, Lyr, KC, H = cfg.NQ, cfg.TL, cfg.T, cfg.D, cfg.DEPTH, cfg.KC, cfg.H
    NCL = TL // 128
    NV = 2 + 2 * NCL + 10
    NCT = cfg.CW // 128
    d = lambda name, shape, dt=F32: nc.dram_tensor(name, list(shape), dt, kind="ExternalInput").ap()
    ident = d("ident", [128, 128], BF16)
    cv = d("cv", [128, KC, 2]); w_ada = d("w_ada", [Lyr, D, 3 * D]); b_ada_fm = d("b_ada_fm", [128, Lyr, 3 * KC])
    norm_g_all = d("norm_g", [Lyr, 128, KC]); decay_all = d("decay", [Lyr, 128, 2 * H]); na_gain_all = d("na_gain", [Lyr, 128, 2])
    rope_all = d("rope", [NQ, 2, 128, TL]); rconst = d("rconst", [6, 128, 128]); rvec_all = d("rvec", [NQ, 128, NV])
    na_bias_all = d("na_bias", [Lyr, NQ, 5, H, 6, 128, 128]); cv_par_all = d("cv_par", [Lyr, 128, NCT, CONV_K + 3])
    w_in = d("w_in", [Lyr, D, cfg.N_IN]); w_br = d("w_br", [Lyr, cfg.KBR, D]); w_out = d("w_out", [Lyr, D, D])
    x_in = d("xT", [D, cfg.SEQ]); c_in = d("cT", [D, cfg.CTX])
    xo = nc.dram_tensor("xoT", [D, cfg.SEQ], F32, kind="ExternalOutput").ap()
    xa, Bxa = C.dram("x_a", [D, cfg.SEQ], F32); xb, Bxb = C.dram("x_b", [D, cfg.SEQ], F32)
    ca, Bca = C.dram("c_a", [D, cfg.CTX], F32); cb, Bcb = C.dram("c_b", [D, cfg.CTX], F32)
    Fst, BFst = C.dram("Fst", [NQ, 2, H, 128, 256], F32)
    with contextlib.ExitStack() as st:
        L = setup_global(C, st, {"ident": ident})
        stage_ada_fused(C, L, cv, w_ada, b_ada_fm)
        Ss = []
        for q in range(NQ):
            S = {}
            for nm, shp, dt in (("hxT", [D, T], BF16), ("qT", [cfg.RQK, T], BF16), ("kT", [cfg.RQK, T], BF16), ("vtm", [T, cfg.RV], BF16),
                                ("rgT", [cfg.RV, T], BF16), ("nqT", [cfg.NAW, T], BF16), ("nkT", [cfg.NAW, T], BF16), ("nvtm", [T, cfg.NAW], BF16),
                                ("ngT", [cfg.NAW, T], BF16), ("uT", [cfg.CW, T], F32), ("cgT", [cfg.CW, T], BF16), ("sgT", [3 * D, T], BF16),
                                ("ogT", [cfg.KBR, T], BF16), ("mT", [D, T], BF16)):
                S[nm] = C.dram(f"{nm}_{q}", shp, dt)
            Ss.append(S)
        cur_x, Bcx = x_in, Buf("x_in")
        cur_c, Bcc = c_in, Buf("c_in")
        Bxo = Buf("xo")
        all_groups = {"ret_q", "ret_k", "ret_v", "na_q", "na_k", "na_v", "cv_ab", "silu", "gates"}
        for l in range(Lyr):
            last = (l == Lyr - 1)
            setup_layer(C, L, l, norm_g_all[l], decay_all[l])
            if last:
                nxt_x, Bnx = xo, Bxo
            else:
                nxt_x, Bnx = (xa, Bxa) if l % 2 == 0 else (xb, Bxb)
            nxt_c, Bnc = (ca, Bca) if l % 2 == 0 else (cb, Bcb)
            for q in range(NQ):
                S = Ss[q]
                insq = {"rope": rope_all[q], "na_gain": na_gain_all[l], "rconst": rconst, "rvec": rvec_all[q]}
                stage_norm(C, L, cur_x[:, q * TL:(q + 1) * TL], Bcx, cur_c, Bcc, S["hxT"][0], S["hxT"][1])
                groups, setup = inproj_groups(C, L, S, insq, all_groups)
                stage_gemm(C, L, S["hxT"][0], S["hxT"][1], [(0, D)], w_in[l], tok_tiles(cfg, True), groups, setup)
                stage_ret_passA(C, L, S, insq, Fst[q], BFst)
            for q in range(NQ):
                S = Ss[q]
                hal = {}
                if q > 0:
                    Pv = Ss[q - 1]
                    hal["kb"] = (Pv["nkT"][0][:, TL - 256:TL], Pv["nkT"][1]); hal["vb"] = (Pv["nvtm"][0][TL - 256:TL, :], Pv["nvtm"][1])
                    hal["ub"] = (Pv["uT"][0][:, TL - 15:TL], Pv["uT"][1])
                else:
                    hal["kb"] = hal["vb"] = hal["ub"] = None
                if q < NQ - 1:
                    Nx = Ss[q + 1]
                    hal["ka"] = (Nx["nkT"][0][:, 0:256], Nx["nkT"][1]); hal["va"] = (Nx["nvtm"][0][0:256, :], Nx["nvtm"][1])
                    hal["ua"] = (Nx["uT"][0][:, 0:15], Nx["uT"][1])
                else:
                    hal["ka"] = hal["va"] = hal["ua"] = None
                insq = {"rope": rope_all[q], "na_gain": na_gain_all[l], "rconst": rconst, "rvec": rvec_all[q],
                        "na_bias": na_bias_all[l, q], "cv_par": cv_par_all[l], "halo": hal}
                stage_ret(C, L, S, insq, Fst, BFst)
                stage_na(C, L, S, insq)
                stage_conv(C, L, S, insq)
                tiles = tok_tiles(cfg, with_ctx=(q == 0 and not last))
                stage_merge(C, L, S, w_br[l], tiles)
                stage_final(C, L, S, w_out[l], tiles, cur_x[:, q * TL:(q + 1) * TL], Bcx, cur_c, Bcc,
                            nxt_x[:, q * TL:(q + 1) * TL], Bnx, nxt_c, Bnc)
            cur_x, Bcx = nxt_x, Bnx
            if not last:
                cur_c, Bcc = nxt_c, Bnc
        C.P.emit()
    return nc


def run_fused(cfg, x, c, ctx, c_ctx, w_ada, b_ada, norm_g, w_in, ret_decay_f, ret_decay_b, w_ret_o,
              na_q_gain, na_k_gain, na_rpb, w_na_o, cv_dw, cv_db, cv_ln_g, cv_ln_b, w_cv_o, w_out):
    f32 = np.float32
    D, KC, H, TL, NQ, Lyr = cfg.D, cfg.KC, cfg.H, cfg.TL, cfg.NQ, cfg.DEPTH
    NCT = cfg.CW // 128
    A = lambda v: np.asarray(v, f32)
    nc_F = _get("F", build_F, cfg)
    shared = {
        "ident": np.eye(128, dtype=f32).astype(NPBF),
        "w_ada": np.ascontiguousarray(A(w_ada)[:Lyr]),
        "b_ada_fm": np.ascontiguousarray(A(b_ada)[:Lyr].reshape(Lyr, 3 * KC, 128).transpose(2, 0, 1)),
        "norm_g": np.ascontiguousarray(A(norm_g)[:Lyr].reshape(Lyr, KC, 128).transpose(0, 2, 1)),
        "decay": np.ascontiguousarray(np.broadcast_to(np.concatenate([A(ret_decay_f)[:Lyr], A(ret_decay_b)[:Lyr]], -1)[:, None, :], (Lyr, 128, 2 * H))),
        "na_gain": np.ascontiguousarray(np.stack([A(na_q_gain)[:Lyr], A(na_k_gain)[:Lyr]], -1)),
        "rope": np.stack([rope_tables(cfg, q) for q in range(NQ)]),
        "rconst": ret_consts(cfg, 0)[0],
        "rvec": np.stack([ret_consts(cfg, q)[1] for q in range(NQ)]),
        "na_bias": np.stack([np.stack([na_bias_table(cfg, q, A(na_rpb)[l]) for q in range(NQ)]) for l in range(Lyr)]),
        "w_in": np.ascontiguousarray(A(w_in)[:Lyr]),
        "w_br": np.ascontiguousarray(np.concatenate([A(w_ret_o)[:Lyr], A(w_na_o)[:Lyr], A(w_cv_o)[:Lyr]], 1)),
        "w_out": np.ascontiguousarray(A(w_out)[:Lyr]),
    }
    cvp = np.concatenate([A(cv_dw)[:Lyr].transpose(0, 2, 1), A(cv_db)[:Lyr][:, :, None], A(cv_ln_g)[:Lyr][:, :, None], A(cv_ln_b)[:Lyr][:, :, None]], axis=2)
    shared["cv_par"] = np.ascontiguousarray(cvp.reshape(Lyr, NCT, 128, CONV_K + 3).transpose(0, 2, 1, 3))
    in_maps = []
    for b in range(cfg.B):
        cvec = np.stack([A(c)[b], A(c_ctx)], -1)
        m = dict(shared)
        m["cv"] = np.ascontiguousarray(cvec.reshape(KC, 128, 2).transpose(1, 0, 2))
        m["xT"] = np.ascontiguousarray(A(x)[b].T)
        m["cT"] = np.ascontiguousarray(A(ctx)[b].T)
        in_maps.append(m)
    res = run_bass_kernel_spmd(nc_F, in_maps, core_ids=list(range(cfg.B))).results
    return np.stack([np.ascontiguousarray(res[b]["xoT"].T) for b in range(cfg.B)])


MODE = "unfused"


def kernel(**inputs):
    cfg = Cfg()
    if MODE == "fused":
        return run_fused(cfg, **inputs)
    return run_model(cfg, **inputs)
```
